# Optimizing a Trainium2 kernel written in Bass

```python
import math
import jax, jax.numpy as jnp
from jax import lax
import numpy as np

D_MODEL = 1024
BATCH = 16
SEQ = 4096
DEPTH = 1

D_RNN = 1024
RNN_HEADS = 16
RNN_HEAD_DIM = D_RNN // RNN_HEADS
CONV_WIDTH = 4
LRU_C = 8.0
D_SG = 1024
SG_GROUPS = 8
SG_GROUP_DIM = D_SG // SG_GROUPS
SG_CHUNK = 128
N_EXPERTS = 32
TOP_K = 4
D_EXPERT = 1024
SWIGLU_LIMIT = 7.0
SWIGLU_ALPHA = 1.702
MOE_BLOCK = 256
EPS = 1e-6
N_MOD = 6
IN_SPLITS = (D_RNN, 2 * D_RNN, 2 * D_RNN + D_SG, 2 * D_RNN + 2 * D_SG, 2 * D_RNN + 2 * D_SG + D_MODEL)
D_IN = 2 * D_RNN + 2 * D_SG + 2 * D_MODEL

kernel_name = "hybrid_rglru_gmlp_moe_adaln"


def rmsnorm(x, g):
    x32 = x.astype(jnp.float32)
    y = x32 * lax.rsqrt(jnp.mean(x32 * x32, axis=-1, keepdims=True) + EPS)
    return (y * g.astype(jnp.float32)).astype(x.dtype)


def modulate(h, shift, scale):
    return h * (1.0 + scale[:, None, :]) + shift[:, None, :]


def causal_depthwise_conv(x, w, b):
    C = x.shape[-1]
    y = lax.conv_general_dilated(x, w[:, None, :].astype(x.dtype), window_strides=(1,),
                                 padding=[(CONV_WIDTH - 1, 0)],
                                 dimension_numbers=('NWC', 'WIO', 'NWC'),
                                 feature_group_count=C)
    return y + b


def rg_lru(x, w_a, b_a, w_x, b_x, lam):
    B, S, _ = x.shape
    x32 = x.astype(jnp.float32)
    xh = x32.reshape(B, S, RNN_HEADS, RNN_HEAD_DIM)
    r = jax.nn.sigmoid(jnp.einsum('bshi,hij->bshj', xh, w_a.astype(jnp.float32)).reshape(B, S, D_RNN)
                       + b_a.astype(jnp.float32))
    i = jax.nn.sigmoid(jnp.einsum('bshi,hij->bshj', xh, w_x.astype(jnp.float32)).reshape(B, S, D_RNN)
                       + b_x.astype(jnp.float32))
    log_a = -LRU_C * r * jax.nn.softplus(-lam.astype(jnp.float32))
    a = jnp.exp(log_a)
    mult = jnp.sqrt(-jnp.expm1(2.0 * log_a))
    u = mult * (i * x32)

    def combine(left, right):
        a_l, h_l = left
        a_r, h_r = right
        return a_l * a_r, a_r * h_l + h_r

    _, h = lax.associative_scan(combine, (a, u), axis=1)
    return h.astype(x.dtype)


def spatial_gating(u, v, ln_g, ln_b, w_s, b_s):
    B, S, _ = v.shape
    v32 = v.astype(jnp.float32)
    mu = jnp.mean(v32, axis=-1, keepdims=True)
    var = jnp.mean(jnp.square(v32 - mu), axis=-1, keepdims=True)
    vn = ((v32 - mu) * lax.rsqrt(var + EPS) * ln_g.astype(jnp.float32) + ln_b.astype(jnp.float32)).astype(v.dtype)
    vc = vn.reshape(B, S // SG_CHUNK, SG_CHUNK, SG_GROUPS, SG_GROUP_DIM)
    causal = jnp.tril(jnp.ones((SG_CHUNK, SG_CHUNK), dtype=bool))
    w = jnp.where(causal[None], w_s, jnp.zeros((), w_s.dtype))
    sv = jnp.einsum('gts,bnsgc->bntgc', w, vc) + b_s.T[None, None, :, :, None]
    return u * sv.reshape(B, S, D_SG)


def hybrid_mixer(h, w_in, conv_w, conv_b, lru_wa, lru_ba, lru_wx, lru_bx, lru_lam,
                 sg_ln_g, sg_ln_b, sg_ws, sg_bs, w_br_rnn, w_br_sg, w_out):
    z = h @ w_in
    rnn_x, rnn_gate, sg_u, sg_v, g_rnn, g_sg = jnp.split(z, IN_SPLITS, axis=-1)
    y_rnn = rg_lru(causal_depthwise_conv(rnn_x, conv_w, conv_b), lru_wa, lru_ba, lru_wx, lru_bx, lru_lam)
    y_rnn = y_rnn * jax.nn.gelu(rnn_gate)
    y_sg = spatial_gating(jax.nn.gelu(sg_u), jax.nn.gelu(sg_v), sg_ln_g, sg_ln_b, sg_ws, sg_bs)
    m = jax.nn.sigmoid(g_rnn) * (y_rnn @ w_br_rnn) + jax.nn.sigmoid(g_sg) * (y_sg @ w_br_sg)
    return m @ w_out


def moe_ffn(h, w_router, b_router, w_gu, b_gu, w_down, b_down):
    B, S, D = h.shape
    N = B * S
    t = h.reshape(N, D)
    logits = (t @ w_router + b_router).astype(jnp.float32)
    top_val, top_idx = lax.top_k(logits, TOP_K)
    top_w = jax.nn.softmax(top_val, axis=-1)
    A = N * TOP_K
    cap = A + N_EXPERTS * MOE_BLOCK
    n_blocks = cap // MOE_BLOCK
    flat_e = top_idx.reshape(A).astype(jnp.int32)
    flat_tok = (jnp.arange(A, dtype=jnp.int32) // TOP_K)
    flat_w = top_w.reshape(A)
    order = jnp.argsort(flat_e)
    sorted_e = flat_e[order]
    counts = jnp.bincount(flat_e, length=N_EXPERTS).astype(jnp.int32)
    padded = ((counts + MOE_BLOCK - 1) // MOE_BLOCK) * MOE_BLOCK
    start = jnp.cumsum(counts) - counts
    padded_end = jnp.cumsum(padded)
    padded_start = padded_end - padded
    dest = padded_start[sorted_e] + jnp.arange(A, dtype=jnp.int32) - start[sorted_e]
    slot_tok = jnp.zeros((cap,), jnp.int32).at[dest].set(flat_tok[order])
    slot_w = jnp.zeros((cap,), t.dtype).at[dest].set(flat_w[order].astype(t.dtype))
    block_e = jnp.searchsorted(padded_end, jnp.arange(n_blocks, dtype=jnp.int32) * MOE_BLOCK, side='right')
    block_e = jnp.minimum(block_e, N_EXPERTS - 1).astype(jnp.int32)

    def expert_block(args):
        tok, w, e = args
        xb = t[tok]
        gu = xb @ w_gu[e] + b_gu[e]
        gate, up = gu[:, :D_EXPERT], gu[:, D_EXPERT:]
        gate = jnp.minimum(gate, SWIGLU_LIMIT)
        up = jnp.clip(up, -SWIGLU_LIMIT, SWIGLU_LIMIT)
        act = (up + 1.0) * (gate * jax.nn.sigmoid(SWIGLU_ALPHA * gate))
        return (act @ w_down[e] + b_down[e]) * w[:, None]

    ys = lax.map(expert_block, (slot_tok.reshape(n_blocks, MOE_BLOCK),
                                slot_w.reshape(n_blocks, MOE_BLOCK), block_e))
    out = jax.ops.segment_sum(ys.reshape(cap, D), slot_tok, num_segments=N)
    return out.reshape(B, S, D)


def setup_inputs(seed: int = 0) -> dict:
    key = jax.random.key(seed)
    ks = jax.random.split(key, 32)
    f32 = jnp.float32
    L = DEPTH
    nrm = lambda k, shape, s: jax.random.normal(k, shape, f32) * s
    a0 = jax.random.uniform(ks[10], (L, D_RNN), f32, 0.9, 0.999)
    s0 = a0 ** (1.0 / LRU_C)
    lam = jnp.log(s0) - jnp.log1p(-s0)
    return {
        "x": nrm(ks[0], (BATCH, SEQ, D_MODEL), 1.0),
        "c": nrm(ks[1], (BATCH, D_MODEL), 1.0),
        "ada_w": nrm(ks[2], (L, D_MODEL, N_MOD * D_MODEL), 0.5 * D_MODEL ** -0.5),
        "ada_b": nrm(ks[3], (L, N_MOD * D_MODEL), 0.02),
        "norm1_g": 1.0 + nrm(ks[4], (L, D_MODEL), 0.02),
        "w_in": nrm(ks[5], (L, D_MODEL, D_IN), D_MODEL ** -0.5),
        "conv_w": nrm(ks[6], (L, CONV_WIDTH, D_RNN), CONV_WIDTH ** -0.5),
        "conv_b": nrm(ks[7], (L, D_RNN), 0.02),
        "lru_wa": nrm(ks[8], (L, RNN_HEADS, RNN_HEAD_DIM, RNN_HEAD_DIM), RNN_HEAD_DIM ** -0.5),
        "lru_ba": nrm(ks[9], (L, D_RNN), 0.1),
        "lru_wx": nrm(ks[11], (L, RNN_HEADS, RNN_HEAD_DIM, RNN_HEAD_DIM), RNN_HEAD_DIM ** -0.5),
        "lru_bx": nrm(ks[12], (L, D_RNN), 0.1),
        "lru_lam": lam,
        "sg_ln_g": 1.0 + nrm(ks[13], (L, D_SG), 0.02),
        "sg_ln_b": nrm(ks[14], (L, D_SG), 0.02),
        "sg_ws": nrm(ks[15], (L, SG_GROUPS, SG_CHUNK, SG_CHUNK), SG_CHUNK ** -0.5),
        "sg_bs": 1.0 + nrm(ks[16], (L, SG_GROUPS, SG_CHUNK), 0.02),
        "w_br_rnn": nrm(ks[17], (L, D_RNN, D_MODEL), D_RNN ** -0.5),
        "w_br_sg": nrm(ks[18], (L, D_SG, D_MODEL), D_SG ** -0.5),
        "w_out": nrm(ks[19], (L, D_MODEL, D_MODEL), D_MODEL ** -0.5),
        "norm2_g": 1.0 + nrm(ks[20], (L, D_MODEL), 0.02),
        "w_router": nrm(ks[21], (L, D_MODEL, N_EXPERTS), D_MODEL ** -0.5),
        "b_router": nrm(ks[22], (L, N_EXPERTS), 0.01),
        "w_gu": nrm(ks[23], (L, N_EXPERTS, D_MODEL, 2 * D_EXPERT), D_MODEL ** -0.5),
        "b_gu": nrm(ks[24], (L, N_EXPERTS, 2 * D_EXPERT), 0.02),
        "w_down": nrm(ks[25], (L, N_EXPERTS, D_EXPERT, D_MODEL), D_EXPERT ** -0.5),
        "b_down": nrm(ks[26], (L, N_EXPERTS, D_MODEL), 0.02),
        "final_g": 1.0 + nrm(ks[27], (D_MODEL,), 0.02),
    }


def reference(x, c, ada_w, ada_b, norm1_g, w_in, conv_w, conv_b, lru_wa, lru_ba, lru_wx, lru_bx,
              lru_lam, sg_ln_g, sg_ln_b, sg_ws, sg_bs, w_br_rnn, w_br_sg, w_out, norm2_g,
              w_router, b_router, w_gu, b_gu, w_down, b_down, final_g):
    for l in range(DEPTH):
        mod = jax.nn.silu(c) @ ada_w[l] + ada_b[l]
        sh1, sc1, gt1, sh2, sc2, gt2 = jnp.split(mod, N_MOD, axis=-1)
        h = modulate(rmsnorm(x, norm1_g[l]), sh1, sc1)
        x = x + gt1[:, None, :] * hybrid_mixer(
            h, w_in[l], conv_w[l], conv_b[l], lru_wa[l], lru_ba[l], lru_wx[l], lru_bx[l], lru_lam[l],
            sg_ln_g[l], sg_ln_b[l], sg_ws[l], sg_bs[l], w_br_rnn[l], w_br_sg[l], w_out[l])
        h = modulate(rmsnorm(x, norm2_g[l]), sh2, sc2)
        x = x + gt2[:, None, :] * moe_ffn(h, w_router[l], b_router[l], w_gu[l], b_gu[l], w_down[l], b_down[l])
    return rmsnorm(x, final_g)
```

```python
from contextlib import ExitStack
import numpy as np
import concourse.bass as bass
import concourse.mybir as mybir
from concourse.bass_utils import run_bass_kernel_spmd

F32 = mybir.dt.float32
BF16 = mybir.dt.bfloat16
AF = mybir.ActivationFunctionType
ALU = mybir.AluOpType
ENGS = ("pe", "act", "dve", "pool", "sp")

CFG_FULL = dict(D=1024, DE=1024, NE=32, SEQ=4096, NSEQ=2, NCORES=8)
EPS = 1e-6
LIMIT = 7.0
ALPHA = 1.702
TOPK = 4
USE_GELU_TANH_LUT = False


class Prog:
    def __init__(self, nc):
        self.nc = nc
        self.tracks = {}
        self.issue = {e: [] for e in ENGS}
        self.last_write = {}
        self.readers = {}
        self.waited = {e: {} for e in ENGS}
        self.n_dma_tracks = 0

    def dma_track(self, name=""):
        self.n_dma_tracks += 1
        return "dma:%d:%s" % (self.n_dma_tracks, name)

    def op(self, eng, fn, reads=(), writes=(), track=None):
        track = track or eng
        tl = self.tracks.setdefault(track, [])
        idx = len(tl)
        deps = {}

        def add(d):
            t, i = d
            if t == track and t.startswith("dma:"):
                return
            if deps.get(t, -1) < i:
                deps[t] = i
        for k in reads:
            lw = self.last_write.get(k)
            if lw is not None:
                add(lw)
        for k in writes:
            lw = self.last_write.get(k)
            if lw is not None and lw[0] != track:
                add(lw)
            for r in self.readers.get(k, ()):
                if r[0] != track:
                    add(r)
        waits = []
        wd = self.waited[eng]
        for t, i in deps.items():
            if t.startswith("dma:"):
                i = len(self.tracks[t]) - 1
            if wd.get(t, -1) >= i:
                continue
            if t == eng and eng == "pe":
                continue
            wd[t] = i
            self.tracks[t][i]["need"] = True
            waits.append((t, i))
        rec = dict(eng=eng, track=track, fn=fn, waits=waits, need=track.startswith("dma:"))
        tl.append(rec)
        self.issue[eng].append(rec)
        for k in reads:
            self.readers.setdefault(k, []).append((track, idx))
        for k in writes:
            self.last_write[k] = (track, idx)
            self.readers[k] = []
        return rec

    def wait_all(self, eng, tracks):
        waits = []
        for t in tracks:
            tl = self.tracks.get(t)
            if not tl:
                continue
            i = len(tl) - 1
            tl[i]["need"] = True
            waits.append((t, i))
        self.issue[eng].append(dict(eng=eng, track=eng, fn=None, waits=waits, need=False))

    def barrier(self):
        tr = list(self.tracks.keys())
        for e in ENGS:
            self.wait_all(e, tr)
            for t in tr:
                self.waited[e][t] = len(self.tracks[t]) - 1

    def emit(self, stack):
        nc = self.nc
        sems, cum = {}, {}
        for n, (t, tl) in enumerate(self.tracks.items()):
            sems[t] = stack.enter_context(nc.semaphore("s%d" % n))
            c, step, arr = 0, (16 if t.startswith("dma:") else 1), []
            for rec in tl:
                if rec["need"]:
                    c += step
                arr.append(c)
            cum[t] = arr
        block = stack.enter_context(nc.Block())
        engobj = {"pe": "tensor", "act": "scalar", "dve": "vector", "pool": "gpsimd", "sp": "sync"}

        def make(engname):
            recs = self.issue[engname]

            def body(e):
                for rec in recs:
                    for (t, i) in rec["waits"]:
                        e.wait_ge(sems[t], cum[t][i])
                    if rec["fn"] is None:
                        continue
                    ins = rec["fn"](e)
                    if rec["need"]:
                        t = rec["track"]
                        ins.then_inc(sems[t], 16 if t.startswith("dma:") else 1)
            return body
        for engname in ENGS:
            if self.issue[engname]:
                getattr(block, engobj[engname])(make(engname))


class Arena:
    def __init__(self, nc, st, name, dt, nelem):
        self.t = st.enter_context(nc.sbuf_tensor(name, [128, nelem], dt))
        self.n, self.off, self.hi = nelem, 0, 0

    def reset(self):
        self.off = 0

    def alloc(self, shape):
        n = 1
        for d in shape[1:]:
            n *= d
        o = self.off
        self.off += n
        self.hi = max(self.hi, self.off)
        assert self.off <= self.n, ("arena overflow", self.off, self.n)
        ap = self.t[:shape[0], o:o + n]
        if len(shape) == 3:
            ap = ap.rearrange("p (a b) -> p a b", a=shape[1])
        elif len(shape) == 4:
            ap = ap.rearrange("p (a b c) -> p a b c", a=shape[1], b=shape[2])
        return ap


def build_nc(cfg):
    D, DE, NE, SEQ, NSEQ = cfg["D"], cfg["DE"], cfg["NE"], cfg["SEQ"], cfg["NSEQ"]
    KC, CE = D // 128, DE // 128
    NTOK = NSEQ * SEQ
    T1 = 256
    S1 = T1 // 128
    NT1 = SEQ // T1
    T2 = 512
    G2 = min(1024, NTOK)
    NG = NTOK // G2
    CB = min(512, D)
    NCB = D // CB
    D6 = 6 * D
    NSUBS = NTOK // 128
    KCM = max(KC, CE)
    PW = 256

    nc = bass.Bass("TRN2", target_bir_lowering=False)
    st = ExitStack()
    P = Prog(nc)

    def din(name, shape, dt=F32):
        return nc.dram_tensor(name, list(shape), dt, kind="ExternalInput").ap()

    def dscr(name, shape, dt):
        return nc.dram_tensor(name, list(shape), dt, kind="Internal").ap()

    x_d = din("x", [NTOK, D])
    cT_d = din("cT", [128, KC, NSEQ])
    adaw_d = din("ada_w", [128, KC, D6])
    adabT_d = din("ada_bT", [128, 6 * KC])
    adabg_d = din("ada_bg", [128, 2, D])
    g1T_d = din("g1T", [128, KC])
    g2T_d = din("g2T", [128, KC])
    win_d = din("w_in", [128, KC, D6])
    convwT_d = din("conv_wT", [128, KC, 4])
    convbT_d = din("conv_bT", [128, KC])
    lruwa_d = din("lru_wa", [KC, 2, 64, 64])
    lruwx_d = din("lru_wx", [KC, 2, 64, 64])
    baT_d = din("lru_baT", [128, KC])
    bxT_d = din("lru_bxT", [128, KC])
    lamT_d = din("lamT", [128, KC])
    lng_d = din("ln_g_b", [128, D])
    lnb_d = din("ln_b_b", [128, D])
    wsT_d = din("sg_wsT", [128, KC, 128])
    bsb_d = din("sg_bs_b", [128, KC, 128])
    wbr_d = din("w_br", [3, 128, KC, D])
    wr_d = din("w_router", [128, KC, NE])
    brb_d = din("b_router_b", [128, NE])
    wgu_d = din("w_gu", [NE, 128, KC, 2 * DE])
    bguT_d = din("b_guT", [128, NE, 2 * CE])
    wdn_d = din("w_down", [NE, 128, CE, D])
    bdn_d = din("b_down", [NE, D])
    fgb_d = din("final_g_b", [128, D])
    out_d = nc.dram_tensor("out", [NTOK, D], F32, kind="ExternalOutput").ap()

    wmix_s = dscr("wmix_s", [9, 128, KC, D], BF16)
    wgu_s = dscr("wgu_s", [NE, 128, KC, 2 * DE], BF16)
    wdn_s = dscr("wdn_s", [NE, 128, CE, D], BF16)
    x1_s = dscr("x1_s", [NTOK, D], F32)
    h2T_s = dscr("h2T_s", [128, KC, NTOK], BF16)

    def sb(name, shape, dt=F32):
        return st.enter_context(nc.sbuf_tensor(name, list(shape), dt))

    A32 = Arena(nc, st, "arena32", F32, cfg.get("A32", 15104))
    A16 = Arena(nc, st, "arena16", BF16, cfg.get("A16", 40960))

    def ar(name, shape, dt=F32):
        return (A32 if dt == F32 else A16).alloc(list(shape))

    NPS = 5
    ps = [st.enter_context(nc.psum_tensor("ps%d" % i, [128, 512], F32)) for i in range(NPS)]
    pst = [st.enter_context(nc.psum_tensor("pst%d" % i, [128, 512], BF16)) for i in range(2)]
    psm = st.enter_context(nc.psum_tensor("psm", [128, 512], F32))
    psmk = "psm"
    psc = [0]
    pstc = [0]

    def next_ps():
        i = psc[0] % NPS
        psc[0] += 1
        return ps[i], ("ps", i)

    def next_pst():
        i = pstc[0] % 2
        pstc[0] += 1
        return pst[i], ("pst", i)

    def mm(out, lhsT, rhs, start, stop, reads, writes):
        P.op("pe", lambda e: e.matmul(out, lhsT, rhs, start=start, stop=stop), reads, writes)

    def tr(out, in_, ident, reads, writes):
        P.op("pe", lambda e: e.transpose(out, in_, ident), reads, writes)

    def act(out, in_, func, reads, writes, bias=None, scale=None, accum_out=None, eng="act"):
        kw = {}
        if bias is not None:
            kw["bias"] = bias
        if scale is not None:
            kw["scale"] = scale
        if accum_out is not None:
            kw["accum_out"] = accum_out
        P.op("act", lambda e: e.activation(out, in_, func, **kw), reads, writes)

    def ts(eng, out, in0, s1, s2, op0, op1, reads, writes):
        if op1 is None:
            P.op(eng, lambda e: e.tensor_scalar(out, in0, s1, None, op0), reads, writes)
        else:
            P.op(eng, lambda e: e.tensor_scalar(out, in0, s1, s2, op0, op1), reads, writes)

    def tt(eng, out, in0, in1, op, reads, writes):
        P.op(eng, lambda e: e.tensor_tensor(out, in0, in1, op), reads, writes)

    def stt(out, in0, scalar, in1, op0, op1, reads, writes):
        P.op("dve", lambda e: e.scalar_tensor_tensor(out, in0, scalar, in1, op0, op1), reads, writes)

    def cp(eng, out, in_, reads, writes):
        if eng == "act":
            P.op("act", lambda e: e.copy(out, in_), reads, writes)
        else:
            P.op(eng, lambda e: e.tensor_copy(out, in_), reads, writes)

    def dma(q, out, in_, reads, writes, track):
        P.op(q, lambda e: e.dma_start(out=out, in_=in_), reads, writes, track=track)

    ctrack = P.dma_track("const")
    consts = {}

    def cload(name, dram, shape, dt=F32, q="sp", arena=False):
        t = ar("c_" + name, shape, dt) if arena else sb("c_" + name, shape, dt)
        dma(q, t[:], dram, [], ["c_" + name], ctrack)
        consts[name] = t
        return t

    cT = cload("cT", cT_d, [128, KC, NSEQ])
    adabT = cload("adabT", adabT_d, [128, 6 * KC])
    adabg = cload("adabg", adabg_d, [128, 2, D], arena=True)
    g1T = cload("g1T", g1T_d, [128, KC])
    g2T = cload("g2T", g2T_d, [128, KC])
    convwT = cload("convwT", convwT_d, [128, KC, 4])
    convbT = cload("convbT", convbT_d, [128, KC])
    baT = cload("baT", baT_d, [128, KC])
    bxT = cload("bxT", bxT_d, [128, KC])
    lamT = cload("lamT", lamT_d, [128, KC])
    lng = cload("lng", lng_d, [128, D])
    lnb = cload("lnb", lnb_d, [128, D])
    wsT32 = cload("wsT32", wsT_d, [128, KC, 128], arena=True)
    bsb = cload("bsb", bsb_d, [128, KC, 128])
    wr32 = cload("wr32", wr_d, [128, KC, NE], arena=True)
    brb = cload("brb", brb_d, [128, NE])
    bguT = cload("bguT", bguT_d, [128, NE, 2 * CE])
    bdn = cload("bdn", bdn_d, [NE, D])
    fgb = cload("fgb", fgb_d, [128, D])
    wbd32 = ar("wbd32", [128, 2, KC, 128])
    P.op("pool", lambda e: e.memset(wbd32[:], 0.0), [], ["c_wbd32"])
    for gi, src in enumerate((lruwa_d, lruwx_d)):
        for half in range(2):
            dma("sp", wbd32[half * 64:(half + 1) * 64, gi, :, half * 64:(half + 1) * 64],
                src[:, half].rearrange("k i j -> i k j"), [], ["c_wbd32"], ctrack)

    ident_bf = sb("ident_bf", [128, 128], BF16)
    ident32 = sb("ident32", [128, 128], F32)
    ones32 = ar("ones32", [128, 128], F32)
    P.op("pool", lambda e: e.memset(ones32[:], 1.0), [], ["ones32"])
    P.op("pool", lambda e: e.affine_select(out=ident32[:], in_=ones32[:], pattern=[[-1, 128]],
                                           compare_op=ALU.is_equal, fill=0.0, base=0, channel_multiplier=1),
         ["ones32"], ["ident32"])
    cp("dve", ident_bf[:], ident32[:], ["ident32"], ["ident_bf"])

    wbd = sb("wbd", [128, 2, KC, 128], BF16)
    cp("dve", wbd[:], wbd32[:], ["c_wbd32"], ["wbd"])
    wsT = sb("wsT", [128, KC, 128], BF16)
    wsTm = ar("wsTm", [128, KC, 128], F32)
    P.op("pool", lambda e: e.affine_select(out=wsTm[:], in_=wsT32[:], pattern=[[0, KC], [1, 128]],
                                           compare_op=ALU.is_ge, fill=0.0, base=0, channel_multiplier=-1),
         ["c_wsT32"], ["wsTm"])
    cp("dve", wsT[:], wsTm[:], ["wsTm"], ["wsT"])
    wr = sb("wr", [128, KC, NE], BF16)
    cp("dve", wr[:], wr32[:], ["c_wr32"], ["wr"])

    kneg = sb("kneg", [128, KC])
    k2 = sb("k2", [128, KC])
    ktmp = sb("ktmp", [128, KC])
    act(ktmp[:], lamT[:], AF.Exp, ["c_lamT"], ["ktmp"], scale=-1.0)
    act(ktmp[:], ktmp[:], AF.Ln, ["ktmp"], ["ktmp"], bias=1.0)
    ts("dve", kneg[:], ktmp[:], -8.0, None, ALU.mult, None, ["ktmp"], ["kneg"])
    ts("dve", k2[:], ktmp[:], -16.0, None, ALU.mult, None, ["ktmp"], ["k2"])

    sc = sb("sc", [128, KC, NSEQ])
    sgt = sb("sgt", [128, KC, NSEQ])
    act(sgt[:], cT[:], AF.Sigmoid, ["c_cT"], ["sgt"])
    tt("dve", sc[:], sgt[:], cT[:], ALU.mult, ["sgt", "c_cT"], ["sc"])
    screp = ar("screp", [128, NSEQ, KC, 128])
    for b in range(NSEQ):
        for k in range(KC):
            cp("dve", screp[:, b, k, :], sc[:, k, b:b + 1].to_broadcast([128, 128]), ["sc"], ["screp"])
    modT = sb("modT", [128, NSEQ, 6 * KC])
    gtb = sb("gtb", [128, 2, NSEQ, D])
    stg = [ar("stg%d" % i, [128, KCM, PW]) for i in range(2)]
    stg_tr = [P.dma_track("stg%d" % i) for i in range(2)]
    stgc = [0]

    def stage_load(dram_ap, q="sp"):
        i = stgc[0] % 2
        stgc[0] += 1
        dma(q, stg[i][:, :dram_ap.shape[1], :dram_ap.shape[2]], dram_ap, [], [("stg", i)], stg_tr[i])
        return stg[i], ("stg", i)

    NJB = D6 // PW
    for jb in range(NJB):
        s_t, s_k = stage_load(adaw_d[:, :, jb * PW:(jb + 1) * PW])
        for jj in range(PW // 128):
            j = jb * (PW // 128) + jj
            for b in range(NSEQ):
                for k in range(KC):
                    col = b * 6 * KC + j
                    mm(psm[:, col:col + 1], s_t[:, k, jj * 128:(jj + 1) * 128], sc[:, k, b:b + 1],
                       k == 0, k == KC - 1, [s_k, "sc"], [psmk])
        m = (jb * PW) // D
        if m in (2, 5):
            which = 0 if m == 2 else 1
            c0 = (jb * PW) % D
            for b in range(NSEQ):
                pg, pgk = next_ps()
                for k in range(KC):
                    mm(pg[:, :PW], screp[:, b, k, :], s_t[:, k, :], k == 0, k == KC - 1, [s_k, "screp"], [pgk])
                tt("dve", gtb[:, which, b, c0:c0 + PW], pg[:, :PW], adabg[:, which, c0:c0 + PW], ALU.add,
                   [pgk, "c_adabg"], ["gtb"])
    for b in range(NSEQ):
        tt("dve", modT[:, b, :], psm[:, b * 6 * KC:(b + 1) * 6 * KC], adabT[:], ALU.add,
           [psmk, "c_adabT"], ["modT"])
    s1 = sb("s1", [128, NSEQ, KC])
    s2 = sb("s2", [128, NSEQ, KC])
    for b in range(NSEQ):
        stt(s1[:, b, :], modT[:, b, KC:2 * KC], 1.0, g1T[:], ALU.add, ALU.mult, ["modT", "c_g1T"], ["s1"])
        stt(s2[:, b, :], modT[:, b, 4 * KC:5 * KC], 1.0, g2T[:], ALU.add, ALU.mult, ["modT", "c_g2T"], ["s2"])

    cbf = [ar("cbf%d" % i, [128, KCM, PW], BF16) for i in range(2)]
    cbf_tr = [P.dma_track("cbf%d" % i) for i in range(2)]
    cbc = [0]

    def precast(src_ap, dst_ap, dst_key):
        kc, w = src_ap.shape[1], src_ap.shape[2]
        s_t, s_k = stage_load(src_ap)
        i = cbc[0] % 2
        cbc[0] += 1
        eng = "act" if i == 0 else "pool"
        cp(eng, cbf[i][:, :kc, :w], s_t[:, :kc, :w], [s_k], [("cbf", i)])
        dma("sp", dst_ap, cbf[i][:, :kc, :w], [("cbf", i)], [dst_key], cbf_tr[i])

    for blk in range(9):
        for c0 in range(0, D, PW):
            w = min(PW, D - c0)
            src = win_d[:, :, blk * D + c0: blk * D + c0 + w] if blk < 6 else wbr_d[blk - 6][:, :, c0:c0 + w]
            precast(src, wmix_s[blk][:, :, c0:c0 + w], ("wmix", blk))
    for e_ in range(NE):
        for c0 in range(0, 2 * DE, PW):
            w = min(PW, 2 * DE - c0)
            precast(wgu_d[e_][:, :, c0:c0 + w], wgu_s[e_][:, :, c0:c0 + w], ("wgu", e_))
        for c0 in range(0, D, PW):
            w = min(PW, D - c0)
            precast(wdn_d[e_][:, :, c0:c0 + w], wdn_s[e_][:, :, c0:c0 + w], ("wdn", e_))

    P.barrier()
    A32.reset()
    A16.reset()
    wts = sb("wts", [128, NSUBS, NE])
    hist = sb("hist", [128, KC, 3])
    hstate = sb("hstate", [128, KC])
    RING = 3
    wring = [ar("wring%d" % i, [128, KC, D], BF16) for i in range(RING)]
    wring_tr = [P.dma_track("wring%d" % i) for i in range(RING)]
    wrc = [0]

    def wload(blk):
        i = wrc[0] % RING
        wrc[0] += 1
        dma("sp", wring[i][:], wmix_s[blk], [("wmix", blk)], [("wring", i)], wring_tr[i])
        return wring[i], ("wring", i)

    X = ar("X", [128, S1, D])
    x_tr = P.dma_track("x")
    xn = ar("xn", [128, S1, D], BF16)
    hT = ar("hT", [128, KC, T1], BF16)
    yrT = ar("yrT", [128, KC, T1], BF16)
    ysT = ar("ysT", [128, KC, T1], BF16)
    mT = ar("mT", [128, KC, T1], BF16)
    tmpR = ar("tmpR", [128, KC, T1])
    junk = ar("junk", [128, D])
    ssq = sb("ssq", [128, 4])
    rstd = sb("rstd", [128, 4])
    NB = 2
    tmp = {n: [ar("t_%s%d" % (n, i), [128, T1 + (4 if n == "rx" else 0)]) for i in range(NB)]
           for n in ("rx", "cv", "r", "i", "a", "m", "u", "hs", "g", "g2", "q")}
    cvb = [ar("cvb%d" % i, [128, T1], BF16) for i in range(NB)]
    vtm = [ar("vtm%d" % i, [128, D]) for i in range(2)]
    vt2 = [ar("vt2%d" % i, [128, D]) for i in range(2)]
    bnst = sb("bnst", [128, 2 * max(1, D // 512), 6])
    mv = sb("mv", [128, 2])
    x1st_tr = P.dma_track("x1st")
    h2st_tr = P.dma_track("h2st")
    h2T_o = ar("h2T_o", [128, KC, T1], BF16)
    lg = sb("lg", [128, NE])
    top8 = sb("top8", [128, 8])
    negmx = sb("negmx", [128, 1])
    msk = sb("msk", [128, NE])
    ex = sb("ex", [128, NE])
    den = sb("den", [128, 1])

    def gelu(eng_alt, out, in_, n, reads, writes, tg, tgk):
        if USE_GELU_TANH_LUT:
            act(out, in_, AF.Gelu_apprx_tanh, reads, writes)
            return
        act(tg, in_, AF.Square, reads, [tgk])
        ts("dve", tg, tg, 0.044715, 1.0, ALU.mult, ALU.add, [tgk], [tgk])
        tt("dve", tg, tg, in_, ALU.mult, [tgk] + list(reads), [tgk])
        act(tg, tg, AF.Sigmoid, [tgk], [tgk], scale=1.5957691216057308)
        tt("dve", out, tg, in_, ALU.mult, [tgk] + list(reads), writes)

    def rmsnorm_to_T(src, src_key, nsub, dstT, dstT_key, scale_ap_fn, bias_ap_fn, sk_reads):
        for s in range(nsub):
            act(junk[:], src[:, s, :], AF.Square, [src_key], ["junk"], accum_out=ssq[:, s:s + 1])
            P.issue
            P.last_write[("ssq", s)] = P.last_write["junk"]
            P.readers[("ssq", s)] = []
        for s in range(nsub):
            act(rstd[:, s:s + 1], ssq[:, s:s + 1], AF.Sqrt, [("ssq", s)], [("rstd", s)], scale=1.0 / D, bias=EPS)
            P.op("dve", lambda e, s=s: e.reciprocal(rstd[:, s:s + 1], rstd[:, s:s + 1]), [("rstd", s)], [("rstd", s)])
            ts("dve", xn[:, s, :], src[:, s, :], rstd[:, s:s + 1], None, ALU.mult, None,
               [src_key, ("rstd", s)], [("xn", s)])
        for k in range(KC):
            pt, ptk = next_pst()
            for s in range(nsub):
                tr(pt[:, s * 128:(s + 1) * 128], xn[:, s, k * 128:(k + 1) * 128], ident_bf[:],
                   [("xn", s), "ident_bf"], [ptk])
            act(dstT[:, k, :nsub * 128], pt[:, :nsub * 128], AF.Identity, [ptk] + sk_reads, [dstT_key],
                scale=scale_ap_fn(k), bias=bias_ap_fn(k))

    for b in range(NSEQ):
        P.op("pool", lambda e: e.memset(hist[:], 0.0), [], ["hist"])
        P.op("pool", lambda e: e.memset(hstate[:], 0.0), [], ["hstate"])
        for j in range(NT1):
            tok0 = b * SEQ + j * T1
            dma("sp", X[:], x_d[tok0:tok0 + T1, :].rearrange("(s p) d -> p s d", p=128), [], ["X"], x_tr)
            rmsnorm_to_T(X, "X", S1, hT, "hT",
                         lambda k: s1[:, b, k:k + 1], lambda k: modT[:, b, k:k + 1], ["s1", "modT"])
            w0, w0k = wload(0)
            w1, w1k = wload(1)
            for c in range(KC):
                i_ = c % NB
                T = {n: tmp[n][i_] for n in tmp}
                K = {n: ("t", n, i_) for n in tmp}
                pz, pzk = next_ps()
                for k in range(KC):
                    mm(pz[:, :T1], w0[:, k, c * 128:(c + 1) * 128], hT[:, k, :], k == 0, k == KC - 1,
                       [w0k, "hT"], [pzk])
                cp("act", T["rx"][:, 0:3], hist[:, c, :], ["hist"], [K["rx"]])
                cp("act", T["rx"][:, 3:3 + T1], pz[:, :T1], [pzk], [K["rx"]])
                cp("act", hist[:, c, :], T["rx"][:, T1:T1 + 3], [K["rx"]], ["hist"])
                ts("dve", T["cv"][:, :], T["rx"][:, 0:T1], convwT[:, c, 0:1], convbT[:, c:c + 1], ALU.mult, ALU.add,
                   [K["rx"], "c_convwT", "c_convbT"], [K["cv"]])
                for kk in range(1, 4):
                    stt(T["cv"][:, :], T["rx"][:, kk:kk + T1], convwT[:, c, kk:kk + 1], T["cv"][:, :],
                        ALU.mult, ALU.add, [K["rx"], K["cv"], "c_convwT"], [K["cv"]])
                cp("pool", cvb[i_][:, :], T["cv"][:, :], [K["cv"]], [("cvb", i_)])
                pr, prk = next_ps()
                mm(pr[:, :T1], wbd[:, 0, c, :], cvb[i_][:, :], True, True, ["wbd", ("cvb", i_)], [prk])
                pi, pik = next_ps()
                mm(pi[:, :T1], wbd[:, 1, c, :], cvb[i_][:, :], True, True, ["wbd", ("cvb", i_)], [pik])
                act(T["r"][:, :], pr[:, :T1], AF.Sigmoid, [prk, "c_baT"], [K["r"]], bias=baT[:, c:c + 1])
                act(T["i"][:, :], pi[:, :T1], AF.Sigmoid, [pik, "c_bxT"], [K["i"]], bias=bxT[:, c:c + 1])
                act(T["a"][:, :], T["r"][:, :], AF.Exp, [K["r"], "kneg"], [K["a"]], scale=kneg[:, c:c + 1])
                act(T["m"][:, :], T["r"][:, :], AF.Exp, [K["r"], "k2"], [K["m"]], scale=k2[:, c:c + 1])
                act(T["m"][:, :], T["m"][:, :], AF.Sqrt, [K["m"]], [K["m"]], scale=-1.0, bias=1.0)
                tt("pool", T["u"][:, :], T["i"][:, :], T["cv"][:, :], ALU.mult, [K["i"], K["cv"]], [K["u"]])
                tt("pool", T["u"][:, :], T["u"][:, :], T["m"][:, :], ALU.mult, [K["u"], K["m"]], [K["u"]])
                P.op("dve", lambda e, T=T, c=c: e.tensor_tensor_scan(T["hs"][:, :], T["a"][:, :], T["u"][:, :],
                                                                       hstate[:, c:c + 1], ALU.mult, ALU.add),
                     [K["a"], K["u"], "hstate"], [K["hs"]])
                cp("pool", hstate[:, c:c + 1], T["hs"][:, T1 - 1:T1], [K["hs"]], ["hstate"])
                pg, pgk = next_ps()
                for k in range(KC):
                    mm(pg[:, :T1], w1[:, k, c * 128:(c + 1) * 128], hT[:, k, :], k == 0, k == KC - 1,
                       [w1k, "hT"], [pgk])
                gelu("dve", T["g"][:, :], pg[:, :T1], T1, [pgk], [K["g"]], T["g2"][:, :], K["g2"])
                tt("dve", yrT[:, c, :], T["hs"][:, :], T["g"][:, :], ALU.mult, [K["hs"], K["g"]], ["yrT"])
            w3, w3k = wload(3)
            for s in range(S1):
                vi = s % 2
                for cb in range(NCB):
                    pv, pvk = next_ps()
                    for k in range(KC):
                        mm(pv[:, :CB], hT[:, k, s * 128:(s + 1) * 128], w3[:, k, cb * CB:(cb + 1) * CB],
                           k == 0, k == KC - 1, [w3k, "hT"], [pvk])
                    gelu("dve", vtm[vi][:, cb * CB:(cb + 1) * CB], pv[:, :CB], CB, [pvk], [("vtm", vi)],
                         vt2[vi][:, cb * CB:(cb + 1) * CB], ("vt2", vi))
                nchunk = max(1, D // 512)
                cw = D // nchunk
                for q in range(nchunk):
                    P.op("dve", lambda e, vi=vi, q=q: e.bn_stats(bnst[:, q, :], vtm[vi][:, q * cw:(q + 1) * cw]),
                         [("vtm", vi)], ["bnst"])
                P.op("dve", lambda e: e.bn_aggr(mv[:], bnst[:, :nchunk, :].rearrange("p a b -> p (a b)")),
                     ["bnst"], ["mv"])
                act(mv[:, 1:2], mv[:, 1:2], AF.Sqrt, ["mv"], ["mv"], bias=EPS)
                P.op("dve", lambda e: e.reciprocal(mv[:, 1:2], mv[:, 1:2]), ["mv"], ["mv"])
                ts("dve", vtm[vi][:, :], vtm[vi][:, :], mv[:, 0:1], mv[:, 1:2], ALU.subtract, ALU.mult,
                   [("vtm", vi), "mv"], [("vtm", vi)])
                tt("pool", vtm[vi][:, :], vtm[vi][:, :], lng[:], ALU.mult, [("vtm", vi), "c_lng"], [("vtm", vi)])
                tt("pool", xn[:, s, :], vtm[vi][:, :], lnb[:], ALU.add, [("vtm", vi), "c_lnb"], [("xn", s)])
            w2, w2k = wload(2)
            for g in range(KC):
                i_ = g % NB
                T = {n: tmp[n][i_] for n in tmp}
                K = {n: ("t", n, i_) for n in tmp}
                pu, puk = next_ps()
                for k in range(KC):
                    mm(pu[:, :T1], w2[:, k, g * 128:(g + 1) * 128], hT[:, k, :], k == 0, k == KC - 1,
                       [w2k, "hT"], [puk])
                gelu("dve", T["g"][:, :], pu[:, :T1], T1, [puk], [K["g"]], T["g2"][:, :], K["g2"])
                psv, psvk = next_ps()
                for s in range(S1):
                    mm(psv[:, s * 128:(s + 1) * 128], xn[:, s, g * 128:(g + 1) * 128], wsT[:, g, :], True, True,
                       [("xn", s), "wsT"], [psvk])
                for s in range(S1):
                    tt("dve", T["q"][:, s * 128:(s + 1) * 128], psv[:, s * 128:(s + 1) * 128], bsb[:, g, :], ALU.add,
                       [psvk, "c_bsb"], [K["q"]])
                tt("pool", ysT[:, g, :], T["q"][:, :], T["g"][:, :], ALU.mult, [K["q"], K["g"]], ["ysT"])
            for pas, (ga, gb_, yT, yk) in enumerate(((4, 6, yrT, "yrT"), (5, 7, ysT, "ysT"))):
                wg, wgk = wload(ga)
                wb, wbk = wload(gb_)
                for oc in range(KC):
                    i_ = oc % NB
                    T = {n: tmp[n][i_] for n in tmp}
                    K = {n: ("t", n, i_) for n in tmp}
                    pgt, pgtk = next_ps()
                    for k in range(KC):
                        mm(pgt[:, :T1], wg[:, k, oc * 128:(oc + 1) * 128], hT[:, k, :], k == 0, k == KC - 1,
                           [wgk, "hT"], [pgtk])
                    act(T["g"][:, :], pgt[:, :T1], AF.Sigmoid, [pgtk], [K["g"]])
                    pbr, pbrk = next_ps()
                    for k in range(KC):
                        mm(pbr[:, :T1], wb[:, k, oc * 128:(oc + 1) * 128], yT[:, k, :], k == 0, k == KC - 1,
                           [wbk, yk], [pbrk])
                    if pas == 0:
                        tt("dve", tmpR[:, oc, :], T["g"][:, :], pbr[:, :T1], ALU.mult, [K["g"], pbrk], [("tmpR", oc)])
                    else:
                        tt("dve", T["i"][:, :], T["g"][:, :], pbr[:, :T1], ALU.mult, [K["g"], pbrk], [K["i"]])
                        tt("pool", mT[:, oc, :], tmpR[:, oc, :], T["i"][:, :], ALU.add, [("tmpR", oc), K["i"]], ["mT"])
            w8, w8k = wload(8)
            for s in range(S1):
                for cb in range(NCB):
                    po, pok = next_ps()
                    for k in range(KC):
                        mm(po[:, :CB], mT[:, k, s * 128:(s + 1) * 128], w8[:, k, cb * CB:(cb + 1) * CB],
                           k == 0, k == KC - 1, [w8k, "mT"], [pok])
                    tt("dve", junk[:, cb * CB:(cb + 1) * CB], po[:, :CB], gtb[:, 0, b, cb * CB:(cb + 1) * CB], ALU.mult,
                       [pok, "gtb"], ["junk"])
                    tt("pool", X[:, s, cb * CB:(cb + 1) * CB], X[:, s, cb * CB:(cb + 1) * CB],
                       junk[:, cb * CB:(cb + 1) * CB], ALU.add, ["X", "junk"], ["X"])
            dma("sp", x1_s[tok0:tok0 + T1, :].rearrange("(s p) d -> p s d", p=128), X[:], ["X"], ["x1_s"], x1st_tr)
            rmsnorm_to_T(X, "X", S1, h2T_o, "h2T_o",
                         lambda k: s2[:, b, k:k + 1], lambda k: modT[:, b, 3 * KC + k:3 * KC + k + 1], ["s2", "modT"])
            dma("sp", h2T_s[:, :, tok0:tok0 + T1], h2T_o[:], ["h2T_o"], ["h2T_s"], h2st_tr)
            for s in range(S1):
                sub = (tok0 // 128) + s
                pl, plk = next_ps()
                for k in range(KC):
                    mm(pl[:, :NE], h2T_o[:, k, s * 128:(s + 1) * 128], wr[:, k, :], k == 0, k == KC - 1,
                       ["h2T_o", "wr"], [plk])
                tt("dve", lg[:], pl[:, :NE], brb[:], ALU.add, [plk, "c_brb"], ["lg"])
                P.op("dve", lambda e: e.max(top8[:], lg[:]), ["lg"], ["top8"])
                ts("dve", negmx[:], top8[:, 0:1], -1.0, None, ALU.mult, None, ["top8"], ["negmx"])
                ts("dve", msk[:], lg[:], top8[:, TOPK - 1:TOPK], None, ALU.is_ge, None, ["lg", "top8"], ["msk"])
                act(ex[:], lg[:], AF.Exp, ["lg", "negmx"], ["ex"], bias=negmx[:, 0:1])
                tt("dve", ex[:], ex[:], msk[:], ALU.mult, ["ex", "msk"], ["ex"])
                P.op("dve", lambda e: e.reduce_sum(den[:], ex[:], axis=mybir.AxisListType.X), ["ex"], ["den"])
                P.op("dve", lambda e: e.reciprocal(den[:], den[:]), ["den"], ["den"])
                ts("dve", wts[:, sub, :], ex[:], den[:, 0:1], None, ALU.mult, None, ["ex", "den"], ["wts"])

    P.barrier()
    A32.reset()
    A16.reset()
    junk = ar("junk2", [128, D])
    NQ = (2 * DE) // 512 if 2 * DE >= 512 else 1
    QW = (2 * DE) // NQ
    ND = NCB
    wq = [ar("wq%d" % i, [128, KC, QW], BF16) for i in range(NQ)]
    wd = [ar("wd%d" % i, [128, CE, CB], BF16) for i in range(ND)]
    wq_tr = [P.dma_track("wq%d" % i) for i in range(NQ)]
    wd_tr = [P.dma_track("wd%d" % i) for i in range(ND)]
    h2g = ar("h2g", [128, KC, G2], BF16)
    h2g_tr = P.dma_track("h2g")
    yacc = ar("yacc", [128, G2 // 128, D])
    actT = [ar("actT%d" % i, [128, CE, T2], BF16) for i in range(2)]
    mt = {n: [ar("m_%s%d" % (n, i), [128, T2]) for i in range(2)] for n in ("g", "s", "u")}
    wtsT = sb("wtsT", [NE, 128])
    x1t = [ar("x1t%d" % i, [128, D]) for i in range(2)]
    x1_tr = [P.dma_track("x1t%d" % i) for i in range(2)]
    ot_tr = [P.dma_track("ot%d" % i) for i in range(2)]
    NT2 = G2 // T2
    S2 = T2 // 128
    tctr = [0]
    for grp in range(NG):
        gt0 = grp * G2
        b = gt0 // SEQ
        dma("sp", h2g[:], h2T_s[:, :, gt0:gt0 + G2], ["h2T_s"], ["h2g"], h2g_tr)
        for s in range(G2 // 128):
            sub = gt0 // 128 + s
            pw, pwk = next_ps()
            tr(pw[:NE, :128], wts[:, sub, :], ident32[:], ["wts", "ident32"], [pwk])
            cp("dve", wtsT[:, :], pw[:NE, :128], [pwk], ["wtsT"])
            for cb in range(NCB):
                pb, pbk = next_ps()
                mm(pb[:, :CB], wtsT[:, :], bdn[:, cb * CB:(cb + 1) * CB], True, True, ["wtsT", "c_bdn"], [pbk])
                cp("dve", yacc[:, s, cb * CB:(cb + 1) * CB], pb[:, :CB], [pbk], [("yacc", s)])
        for e_ in range(NE):
            for q in range(NQ):
                dma("sp", wq[q][:], wgu_s[e_][:, :, q * QW:(q + 1) * QW], [("wgu", e_)], [("wq", q)], wq_tr[q])
            for d_ in range(ND):
                dma("sp", wd[d_][:], wdn_s[e_][:, :, d_ * CB:(d_ + 1) * CB], [("wdn", e_)], [("wd", d_)], wd_tr[d_])
            for t in range(NT2):
                ai = tctr[0] % 2
                tctr[0] += 1
                aT = actT[ai]
                for c in range(CE):
                    mi = c % 2
                    G_, S_, U_ = mt["g"][mi], mt["s"][mi], mt["u"][mi]
                    gk, sk, uk = ("mg", mi), ("ms", mi), ("mu", mi)
                    gcol = c * 128
                    ucol = DE + c * 128
                    pgm, pgmk = next_ps()
                    for k in range(KC):
                        mm(pgm[:, :T2], wq[gcol // QW][:, k, gcol % QW:gcol % QW + 128], h2g[:, k, t * T2:(t + 1) * T2],
                           k == 0, k == KC - 1, [("wq", gcol // QW), "h2g"], [pgmk])
                    pum, pumk = next_ps()
                    for k in range(KC):
                        mm(pum[:, :T2], wq[ucol // QW][:, k, ucol % QW:ucol % QW + 128], h2g[:, k, t * T2:(t + 1) * T2],
                           k == 0, k == KC - 1, [("wq", ucol // QW), "h2g"], [pumk])
                    ts("dve", G_[:, :], pgm[:, :T2], bguT[:, e_, c:c + 1], LIMIT, ALU.add, ALU.min,
                       [pgmk, "c_bguT"], [gk])
                    act(S_[:, :], G_[:, :], AF.Sigmoid, [gk], [sk], scale=ALPHA)
                    ts("dve", U_[:, :], pum[:, :T2], bguT[:, e_, CE + c:CE + c + 1], LIMIT, ALU.add, ALU.min,
                       [pumk, "c_bguT"], [uk])
                    ts("dve", U_[:, :], U_[:, :], -LIMIT, 1.0, ALU.max, ALU.add, [uk], [uk])
                    tt("pool", S_[:, :], S_[:, :], G_[:, :], ALU.mult, [sk, gk], [sk])
                    tt("dve", aT[:, c, :], U_[:, :], S_[:, :], ALU.mult, [uk, sk], [("actT", ai)])
                for s in range(S2):
                    ys = t * S2 + s
                    sub = gt0 // 128 + ys
                    for cb in range(NCB):
                        pd, pdk = next_ps()
                        for k in range(CE):
                            mm(pd[:, :CB], aT[:, k, s * 128:(s + 1) * 128], wd[cb][:, k, :], k == 0, k == CE - 1,
                               [("actT", ai), ("wd", cb)], [pdk])
                        stt(yacc[:, ys, cb * CB:(cb + 1) * CB], pd[:, :CB], wts[:, sub, e_:e_ + 1],
                            yacc[:, ys, cb * CB:(cb + 1) * CB], ALU.mult, ALU.add,
                            [pdk, "wts", ("yacc", ys)], [("yacc", ys)])
        for s in range(G2 // 128):
            r0 = gt0 + s * 128
            xi = s % 2
            dma("sp", x1t[xi][:], x1_s[r0:r0 + 128, :], ["x1_s"], [("x1t", xi)], x1_tr[xi])
            tt("pool", yacc[:, s, :], yacc[:, s, :], gtb[:, 1, b, :], ALU.mult, [("yacc", s), "gtb"], [("yacc", s)])
            tt("dve", x1t[xi][:], x1t[xi][:], yacc[:, s, :], ALU.add, [("x1t", xi), ("yacc", s)], [("x1t", xi)])
            act(junk[:], x1t[xi][:], AF.Square, [("x1t", xi)], ["junk"], accum_out=ssq[:, 0:1])
            P.last_write[("ssq", 0)] = P.last_write["junk"]
            P.readers[("ssq", 0)] = []
            act(rstd[:, 0:1], ssq[:, 0:1], AF.Sqrt, [("ssq", 0)], [("rstd", 0)], scale=1.0 / D, bias=EPS)
            P.op("dve", lambda e: e.reciprocal(rstd[:, 0:1], rstd[:, 0:1]), [("rstd", 0)], [("rstd", 0)])
            stt(x1t[xi][:], x1t[xi][:], rstd[:, 0:1], fgb[:], ALU.mult, ALU.mult,
                [("x1t", xi), ("rstd", 0), "c_fgb"], [("x1t", xi)])
            dma("sp", out_d[r0:r0 + 128, :], x1t[xi][:], [("x1t", xi)], ["out"], ot_tr[xi])
    P.wait_all("sp", ot_tr)
    print("[build] sbuf bytes remaining/partition:", nc.sbuf_bytes_remaining, "A32 hi", A32.hi * 4, "A16 hi", A16.hi * 2,
          "n_ops", sum(len(v) for v in P.issue.values()), flush=True)
    P.emit(st)
    st.close()
    return nc


def _layout(inputs, cfg):
    D, DE, NE, SEQ, NSEQ, NCORES = cfg["D"], cfg["DE"], cfg["NE"], cfg["SEQ"], cfg["NSEQ"], cfg["NCORES"]
    KC, CE = D // 128, DE // 128
    f = lambda a: np.ascontiguousarray(np.asarray(a, dtype=np.float32))
    g = {k: np.asarray(v) for k, v in inputs.items()}

    def fm(v):
        return f(v.reshape(-1, 128).T)

    def km(w):
        return f(w.reshape(-1, 128, w.shape[-1]).transpose(1, 0, 2))

    def bc(v):
        return f(np.broadcast_to(v[None, :], (128, v.shape[0])))
    shared = {}
    shared["ada_w"] = km(g["ada_w"][0])
    ada_b = g["ada_b"][0]
    shared["ada_bT"] = fm(ada_b)
    shared["ada_bg"] = f(np.stack([np.broadcast_to(ada_b[2 * D:3 * D], (128, D)),
                                   np.broadcast_to(ada_b[5 * D:6 * D], (128, D))], axis=1))
    shared["g1T"] = fm(g["norm1_g"][0])
    shared["g2T"] = fm(g["norm2_g"][0])
    shared["w_in"] = km(g["w_in"][0])
    shared["conv_wT"] = f(g["conv_w"][0].reshape(4, KC, 128).transpose(2, 1, 0))
    shared["conv_bT"] = fm(g["conv_b"][0])
    shared["lru_wa"] = f(g["lru_wa"][0].reshape(KC, 2, 64, 64))
    shared["lru_wx"] = f(g["lru_wx"][0].reshape(KC, 2, 64, 64))
    shared["lru_baT"] = fm(g["lru_ba"][0])
    shared["lru_bxT"] = fm(g["lru_bx"][0])
    shared["lamT"] = fm(g["lru_lam"][0])
    shared["ln_g_b"] = bc(g["sg_ln_g"][0])
    shared["ln_b_b"] = bc(g["sg_ln_b"][0])
    shared["sg_wsT"] = f(g["sg_ws"][0].transpose(2, 0, 1))
    shared["sg_bs_b"] = f(np.broadcast_to(g["sg_bs"][0][None], (128, KC, 128)))
    shared["w_br"] = f(np.stack([km(g["w_br_rnn"][0]), km(g["w_br_sg"][0]), km(g["w_out"][0])]))
    shared["w_router"] = km(g["w_router"][0])
    shared["b_router_b"] = bc(g["b_router"][0])
    shared["w_gu"] = f(g["w_gu"][0].reshape(NE, KC, 128, 2 * DE).transpose(0, 2, 1, 3))
    shared["b_guT"] = f(g["b_gu"][0].reshape(NE, 2 * CE, 128).transpose(2, 0, 1))
    shared["w_down"] = f(g["w_down"][0].reshape(NE, CE, 128, D).transpose(0, 2, 1, 3))
    shared["b_down"] = f(g["b_down"][0])
    shared["final_g_b"] = bc(g["final_g"])
    x = g["x"].reshape(NCORES, NSEQ * SEQ, D)
    c = g["c"].reshape(NCORES, NSEQ, KC, 128)
    maps = []
    for i in range(NCORES):
        m = dict(shared)
        m["x"] = f(x[i])
        m["cT"] = f(c[i].transpose(2, 1, 0))
        maps.append(m)
    return maps


_NC_CACHE = {}


def run(inputs, cfg):
    key = tuple(sorted(cfg.items()))
    if key not in _NC_CACHE:
        _NC_CACHE[key] = build_nc(cfg)
    nc = _NC_CACHE[key]
    maps = _layout(inputs, cfg)
    res = run_bass_kernel_spmd(nc, maps, core_ids=list(range(cfg["NCORES"])))
    out = np.stack([r["out"] for r in res.results], axis=0)
    B = cfg["NCORES"] * cfg["NSEQ"]
    return out.reshape(B, cfg["SEQ"], cfg["D"]).astype(np.float32)


def kernel(**inputs):
    return run(inputs, CFG_FULL)
```

```python
from contextlib import ExitStack
import numpy as np
import concourse.bass as bass
import concourse.mybir as mybir
from concourse.bass_utils import run_bass_kernel_spmd

F32 = mybir.dt.float32
BF16 = mybir.dt.bfloat16
I32 = mybir.dt.int32
AF = mybir.ActivationFunctionType
ALU = mybir.AluOpType
ENGS = ("pe", "act", "dve", "pool", "sp")

CFG_FULL = dict(D=1024, DE=1024, NE=32, SEQ=4096, NSEQ=2, NCORES=8)
EPS = 1e-6
LIMIT = 7.0
ALPHA = 1.702
TOPK = 4
USE_GELU_TANH_LUT = False
BR = 256
USE_COND_SKIP = False


class Prog:
    def __init__(self, nc):
        self.nc = nc
        self.tracks = {}
        self.issue = {e: [] for e in ENGS}
        self.last_write = {}
        self.readers = {}
        self.waited = {e: {} for e in ENGS}
        self.n_dma_tracks = 0

    def dma_track(self, name=""):
        self.n_dma_tracks += 1
        return "dma:%d:%s" % (self.n_dma_tracks, name)

    def op(self, eng, fn, reads=(), writes=(), track=None):
        track = track or eng
        tl = self.tracks.setdefault(track, [])
        idx = len(tl)
        deps = {}

        def add(d):
            t, i = d
            if t == track and t.startswith("dma:"):
                return
            if deps.get(t, -1) < i:
                deps[t] = i
        for k in reads:
            lw = self.last_write.get(k)
            if lw is not None:
                add(lw)
        for k in writes:
            lw = self.last_write.get(k)
            if lw is not None and lw[0] != track:
                add(lw)
            for r in self.readers.get(k, ()):
                if r[0] != track:
                    add(r)
        waits = []
        wd = self.waited[eng]
        for t, i in deps.items():
            if t.startswith("dma:"):
                i = len(self.tracks[t]) - 1
            if wd.get(t, -1) >= i:
                continue
            if t == eng and eng == "pe":
                continue
            wd[t] = i
            self.tracks[t][i]["need"] = True
            waits.append((t, i))
        rec = dict(eng=eng, track=track, fn=fn, waits=waits, need=track.startswith("dma:"))
        tl.append(rec)
        self.issue[eng].append(rec)
        for k in reads:
            self.readers.setdefault(k, []).append((track, idx))
        for k in writes:
            self.last_write[k] = (track, idx)
            self.readers[k] = []
        return rec

    def wait_all(self, eng, tracks):
        waits = []
        for t in tracks:
            tl = self.tracks.get(t)
            if not tl:
                continue
            i = len(tl) - 1
            tl[i]["need"] = True
            waits.append((t, i))
        self.issue[eng].append(dict(eng=eng, track=eng, fn=None, waits=waits, need=False))

    def barrier(self):
        tr = list(self.tracks.keys())
        for e in ENGS:
            self.wait_all(e, tr)
            for t in tr:
                self.waited[e][t] = len(self.tracks[t]) - 1

    def emit(self, stack):
        nc = self.nc
        sems, cum = {}, {}
        for n, (t, tl) in enumerate(self.tracks.items()):
            sems[t] = stack.enter_context(nc.semaphore("s%d" % n))
            c, step, arr = 0, (16 if t.startswith("dma:") else 1), []
            for rec in tl:
                if rec["need"]:
                    c += step
                arr.append(c)
            cum[t] = arr
        block = stack.enter_context(nc.Block())
        engobj = {"pe": "tensor", "act": "scalar", "dve": "vector", "pool": "gpsimd", "sp": "sync"}

        def make(engname):
            recs = self.issue[engname]

            def body(e):
                for rec in recs:
                    for (t, i) in rec["waits"]:
                        e.wait_ge(sems[t], cum[t][i])
                    if rec["fn"] is None:
                        continue
                    ins = rec["fn"](e)
                    if rec["need"]:
                        t = rec["track"]
                        ins.then_inc(sems[t], 16 if t.startswith("dma:") else 1)
            return body
        for engname in ENGS:
            if self.issue[engname]:
                getattr(block, engobj[engname])(make(engname))


class Arena:
    def __init__(self, nc, st, name, dt, nelem):
        self.t = st.enter_context(nc.sbuf_tensor(name, [128, nelem], dt))
        self.n, self.off, self.hi = nelem, 0, 0

    def reset(self):
        self.off = 0

    def alloc(self, shape):
        n = 1
        for d in shape[1:]:
            n *= d
        o = self.off
        self.off += n
        self.hi = max(self.hi, self.off)
        assert self.off <= self.n, ("arena overflow", self.off, self.n)
        ap = self.t[:shape[0], o:o + n]
        if len(shape) == 3:
            ap = ap.rearrange("p (a b) -> p a b", a=shape[1])
        elif len(shape) == 4:
            ap = ap.rearrange("p (a b c) -> p a b c", a=shape[1], b=shape[2])
        return ap


def build_nc(cfg):
    D, DE, NE, SEQ, NSEQ = cfg["D"], cfg["DE"], cfg["NE"], cfg["SEQ"], cfg["NSEQ"]
    KC, CE = D // 128, DE // 128
    NTOK = NSEQ * SEQ
    T1 = 256
    S1 = T1 // 128
    NT1 = SEQ // T1
    T2 = 512
    G2 = min(1024, NTOK)
    NG = NTOK // G2
    CB = min(512, D)
    NCB = D // CB
    D6 = 6 * D
    NSUBS = NTOK // 128
    KCM = max(KC, CE)
    PW = 256

    nc = bass.Bass("TRN2", target_bir_lowering=False)
    st = ExitStack()
    P = Prog(nc)

    def din(name, shape, dt=F32):
        return nc.dram_tensor(name, list(shape), dt, kind="ExternalInput").ap()

    def dscr(name, shape, dt):
        return nc.dram_tensor(name, list(shape), dt, kind="Internal").ap()

    x_d = din("x", [NTOK, D])
    cT_d = din("cT", [128, KC, NSEQ])
    adaw_d = din("ada_w", [128, KC, D6])
    adabT_d = din("ada_bT", [128, 6 * KC])
    adabg_d = din("ada_bg", [128, 2, D])
    g1T_d = din("g1T", [128, KC])
    g2T_d = din("g2T", [128, KC])
    win_d = din("w_in", [128, KC, D6])
    convwT_d = din("conv_wT", [128, KC, 4])
    convbT_d = din("conv_bT", [128, KC])
    lruwa_d = din("lru_wa", [KC, 2, 64, 64])
    lruwx_d = din("lru_wx", [KC, 2, 64, 64])
    baT_d = din("lru_baT", [128, KC])
    bxT_d = din("lru_bxT", [128, KC])
    lamT_d = din("lamT", [128, KC])
    lng_d = din("ln_g_b", [128, D])
    lnb_d = din("ln_b_b", [128, D])
    wsT_d = din("sg_wsT", [128, KC, 128])
    bsb_d = din("sg_bs_b", [128, KC, 128])
    wbr_d = din("w_br", [3, 128, KC, D])
    wr_d = din("w_router", [128, KC, NE])
    brb_d = din("b_router_b", [128, NE])
    wgu_d = din("w_gu", [NE, 128, KC, 2 * DE])
    bguT_d = din("b_guT", [NE, 128, 2 * CE])
    wdn_d = din("w_down", [NE, 128, CE, D])
    bdn_d = din("b_down", [NE, D])
    fgb_d = din("final_g_b", [128, D])
    out_d = nc.dram_tensor("out", [NTOK, D], F32, kind="ExternalOutput").ap()

    wmix_s = dscr("wmix_s", [9, 128, KC, D], BF16)
    QW = min(512, 2 * DE)
    NQ = (2 * DE) // QW
    ND = NCB
    NBLK = (NTOK * TOPK) // BR + NE
    SB = BR // 128
    wq_s = [dscr("wq_s%d" % q, [NE, 128, KC, QW], BF16) for q in range(NQ)]
    wd_s = [dscr("wd_s%d" % d_, [NE, 128, CE, CB], BF16) for d_ in range(ND)]
    x1_s = dscr("x1_s", [NTOK, D], F32)
    h2tm_s = dscr("h2tm_s", [NTOK, D], BF16)
    xs_s = dscr("xs_s", [NBLK * BR, D], BF16)
    ys_s = dscr("ys_s", [NBLK * BR, D], F32)

    def sb(name, shape, dt=F32):
        return st.enter_context(nc.sbuf_tensor(name, list(shape), dt))

    A32 = Arena(nc, st, "arena32", F32, cfg.get("A32", 15104))
    A16 = Arena(nc, st, "arena16", BF16, cfg.get("A16", 40960))

    def ar(name, shape, dt=F32):
        return (A32 if dt == F32 else A16).alloc(list(shape))

    NPS = 5
    ps = [st.enter_context(nc.psum_tensor("ps%d" % i, [128, 512], F32)) for i in range(NPS)]
    pst = [st.enter_context(nc.psum_tensor("pst%d" % i, [128, 512], BF16)) for i in range(2)]
    psm = st.enter_context(nc.psum_tensor("psm", [128, 512], F32))
    psmk = "psm"
    psc = [0]
    pstc = [0]

    def next_ps():
        i = psc[0] % NPS
        psc[0] += 1
        return ps[i], ("ps", i)

    def next_pst():
        i = pstc[0] % 2
        pstc[0] += 1
        return pst[i], ("pst", i)

    def mm(out, lhsT, rhs, start, stop, reads, writes):
        P.op("pe", lambda e: e.matmul(out, lhsT, rhs, start=start, stop=stop), reads, writes)

    def tr(out, in_, ident, reads, writes):
        P.op("pe", lambda e: e.transpose(out, in_, ident), reads, writes)

    def act(out, in_, func, reads, writes, bias=None, scale=None, accum_out=None, eng="act"):
        kw = {}
        if bias is not None:
            kw["bias"] = bias
        if scale is not None:
            kw["scale"] = scale
        if accum_out is not None:
            kw["accum_out"] = accum_out
        P.op("act", lambda e: e.activation(out, in_, func, **kw), reads, writes)

    def ts(eng, out, in0, s1, s2, op0, op1, reads, writes):
        if op1 is None:
            P.op(eng, lambda e: e.tensor_scalar(out, in0, s1, None, op0), reads, writes)
        else:
            P.op(eng, lambda e: e.tensor_scalar(out, in0, s1, s2, op0, op1), reads, writes)

    def tt(eng, out, in0, in1, op, reads, writes):
        P.op(eng, lambda e: e.tensor_tensor(out, in0, in1, op), reads, writes)

    def stt(out, in0, scalar, in1, op0, op1, reads, writes):
        P.op("dve", lambda e: e.scalar_tensor_tensor(out, in0, scalar, in1, op0, op1), reads, writes)

    def cp(eng, out, in_, reads, writes):
        if eng == "act":
            P.op("act", lambda e: e.copy(out, in_), reads, writes)
        else:
            P.op(eng, lambda e: e.tensor_copy(out, in_), reads, writes)

    def dma(q, out, in_, reads, writes, track):
        P.op(q, lambda e: e.dma_start(out=out, in_=in_), reads, writes, track=track)

    ctrack = P.dma_track("const")
    consts = {}

    def cload(name, dram, shape, dt=F32, q="sp", arena=False):
        t = ar("c_" + name, shape, dt) if arena else sb("c_" + name, shape, dt)
        dma(q, t[:], dram, [], ["c_" + name], ctrack)
        consts[name] = t
        return t

    cT = cload("cT", cT_d, [128, KC, NSEQ])
    adabT = cload("adabT", adabT_d, [128, 6 * KC])
    adabg = cload("adabg", adabg_d, [128, 2, D], arena=True)
    g1T = cload("g1T", g1T_d, [128, KC])
    g2T = cload("g2T", g2T_d, [128, KC])
    convwT = cload("convwT", convwT_d, [128, KC, 4])
    convbT = cload("convbT", convbT_d, [128, KC])
    baT = cload("baT", baT_d, [128, KC])
    bxT = cload("bxT", bxT_d, [128, KC])
    lamT = cload("lamT", lamT_d, [128, KC])
    lng = cload("lng", lng_d, [128, D])
    lnb = cload("lnb", lnb_d, [128, D])
    wsT32 = cload("wsT32", wsT_d, [128, KC, 128], arena=True)
    bsb = cload("bsb", bsb_d, [128, KC, 128])
    wr32 = cload("wr32", wr_d, [128, KC, NE], arena=True)
    brb = cload("brb", brb_d, [128, NE])
    bdn = cload("bdn", bdn_d, [NE, D])
    fgb = cload("fgb", fgb_d, [128, D])
    wbd32 = ar("wbd32", [128, 2, KC, 128])
    P.op("pool", lambda e: e.memset(wbd32[:], 0.0), [], ["c_wbd32"])
    for gi, src in enumerate((lruwa_d, lruwx_d)):
        for half in range(2):
            dma("sp", wbd32[half * 64:(half + 1) * 64, gi, :, half * 64:(half + 1) * 64],
                src[:, half].rearrange("k i j -> i k j"), [], ["c_wbd32"], ctrack)

    ident_bf = sb("ident_bf", [128, 128], BF16)
    ident32 = sb("ident32", [128, 128], F32)
    ones32 = ar("ones32", [128, 128], F32)
    P.op("pool", lambda e: e.memset(ones32[:], 1.0), [], ["ones32"])
    P.op("pool", lambda e: e.affine_select(out=ident32[:], in_=ones32[:], pattern=[[-1, 128]],
                                           compare_op=ALU.is_equal, fill=0.0, base=0, channel_multiplier=1),
         ["ones32"], ["ident32"])
    cp("dve", ident_bf[:], ident32[:], ["ident32"], ["ident_bf"])

    wbd = sb("wbd", [128, 2, KC, 128], BF16)
    cp("dve", wbd[:], wbd32[:], ["c_wbd32"], ["wbd"])
    wsT = sb("wsT", [128, KC, 128], BF16)
    wsTm = ar("wsTm", [128, KC, 128], F32)
    P.op("pool", lambda e: e.affine_select(out=wsTm[:], in_=wsT32[:], pattern=[[0, KC], [1, 128]],
                                           compare_op=ALU.is_ge, fill=0.0, base=0, channel_multiplier=-1),
         ["c_wsT32"], ["wsTm"])
    cp("dve", wsT[:], wsTm[:], ["wsTm"], ["wsT"])
    wr = sb("wr", [128, KC, NE], BF16)
    cp("dve", wr[:], wr32[:], ["c_wr32"], ["wr"])

    kneg = sb("kneg", [128, KC])
    k2 = sb("k2", [128, KC])
    ktmp = sb("ktmp", [128, KC])
    act(ktmp[:], lamT[:], AF.Exp, ["c_lamT"], ["ktmp"], scale=-1.0)
    act(ktmp[:], ktmp[:], AF.Ln, ["ktmp"], ["ktmp"], bias=1.0)
    ts("dve", kneg[:], ktmp[:], -8.0, None, ALU.mult, None, ["ktmp"], ["kneg"])
    ts("dve", k2[:], ktmp[:], -16.0, None, ALU.mult, None, ["ktmp"], ["k2"])

    sc = sb("sc", [128, KC, NSEQ])
    sgt = sb("sgt", [128, KC, NSEQ])
    act(sgt[:], cT[:], AF.Sigmoid, ["c_cT"], ["sgt"])
    tt("dve", sc[:], sgt[:], cT[:], ALU.mult, ["sgt", "c_cT"], ["sc"])
    screp = ar("screp", [128, NSEQ, KC, 128])
    for b in range(NSEQ):
        for k in range(KC):
            cp("dve", screp[:, b, k, :], sc[:, k, b:b + 1].to_broadcast([128, 128]), ["sc"], ["screp"])
    modT = sb("modT", [128, NSEQ, 6 * KC])
    gtb = sb("gtb", [128, 2, NSEQ, D])
    stg = [ar("stg%d" % i, [128, KCM, PW]) for i in range(2)]
    stg_tr = [P.dma_track("stg%d" % i) for i in range(2)]
    stgc = [0]

    def stage_load(dram_ap, q="sp"):
        i = stgc[0] % 2
        stgc[0] += 1
        dma(q, stg[i][:, :dram_ap.shape[1], :dram_ap.shape[2]], dram_ap, [], [("stg", i)], stg_tr[i])
        return stg[i], ("stg", i)

    NJB = D6 // PW
    for jb in range(NJB):
        s_t, s_k = stage_load(adaw_d[:, :, jb * PW:(jb + 1) * PW])
        for jj in range(PW // 128):
            j = jb * (PW // 128) + jj
            for b in range(NSEQ):
                for k in range(KC):
                    col = b * 6 * KC + j
                    mm(psm[:, col:col + 1], s_t[:, k, jj * 128:(jj + 1) * 128], sc[:, k, b:b + 1],
                       k == 0, k == KC - 1, [s_k, "sc"], [psmk])
        m = (jb * PW) // D
        if m in (2, 5):
            which = 0 if m == 2 else 1
            c0 = (jb * PW) % D
            for b in range(NSEQ):
                pg, pgk = next_ps()
                for k in range(KC):
                    mm(pg[:, :PW], screp[:, b, k, :], s_t[:, k, :], k == 0, k == KC - 1, [s_k, "screp"], [pgk])
                tt("dve", gtb[:, which, b, c0:c0 + PW], pg[:, :PW], adabg[:, which, c0:c0 + PW], ALU.add,
                   [pgk, "c_adabg"], ["gtb"])
    for b in range(NSEQ):
        tt("dve", modT[:, b, :], psm[:, b * 6 * KC:(b + 1) * 6 * KC], adabT[:], ALU.add,
           [psmk, "c_adabT"], ["modT"])
    s1 = sb("s1", [128, NSEQ, KC])
    s2 = sb("s2", [128, NSEQ, KC])
    for b in range(NSEQ):
        stt(s1[:, b, :], modT[:, b, KC:2 * KC], 1.0, g1T[:], ALU.add, ALU.mult, ["modT", "c_g1T"], ["s1"])
        stt(s2[:, b, :], modT[:, b, 4 * KC:5 * KC], 1.0, g2T[:], ALU.add, ALU.mult, ["modT", "c_g2T"], ["s2"])

    cbf = [ar("cbf%d" % i, [128, KCM, PW], BF16) for i in range(2)]
    cbf_tr = [P.dma_track("cbf%d" % i) for i in range(2)]
    cbc = [0]

    def precast(src_ap, dst_ap, dst_key):
        kc, w = src_ap.shape[1], src_ap.shape[2]
        s_t, s_k = stage_load(src_ap)
        i = cbc[0] % 2
        cbc[0] += 1
        eng = "act" if i == 0 else "pool"
        cp(eng, cbf[i][:, :kc, :w], s_t[:, :kc, :w], [s_k], [("cbf", i)])
        dma("sp", dst_ap, cbf[i][:, :kc, :w], [("cbf", i)], [dst_key], cbf_tr[i])

    for blk in range(9):
        for c0 in range(0, D, PW):
            w = min(PW, D - c0)
            src = win_d[:, :, blk * D + c0: blk * D + c0 + w] if blk < 6 else wbr_d[blk - 6][:, :, c0:c0 + w]
            precast(src, wmix_s[blk][:, :, c0:c0 + w], ("wmix", blk))
    for e_ in range(NE):
        for c0 in range(0, 2 * DE, PW):
            w = min(PW, 2 * DE - c0)
            precast(wgu_d[e_][:, :, c0:c0 + w], wq_s[c0 // QW][e_][:, :, c0 % QW:c0 % QW + w], "wq_s")
        for c0 in range(0, D, PW):
            w = min(PW, D - c0)
            precast(wdn_d[e_][:, :, c0:c0 + w], wd_s[c0 // CB][e_][:, :, c0 % CB:c0 % CB + w], "wd_s")

    P.barrier()
    A32.reset()
    A16.reset()
    wts = sb("wts", [128, NSUBS, NE])
    hist = sb("hist", [128, KC, 3])
    hstate = sb("hstate", [128, KC])
    RING = 3
    wring = [ar("wring%d" % i, [128, KC, D], BF16) for i in range(RING)]
    wring_tr = [P.dma_track("wring%d" % i) for i in range(RING)]
    wrc = [0]

    def wload(blk):
        i = wrc[0] % RING
        wrc[0] += 1
        dma("sp", wring[i][:], wmix_s[blk], [("wmix", blk)], [("wring", i)], wring_tr[i])
        return wring[i], ("wring", i)

    X = ar("X", [128, S1, D])
    x_tr = P.dma_track("x")
    xn = ar("xn", [128, S1, D], BF16)
    hT = ar("hT", [128, KC, T1], BF16)
    yrT = ar("yrT", [128, KC, T1], BF16)
    ysT = ar("ysT", [128, KC, T1], BF16)
    mT = ar("mT", [128, KC, T1], BF16)
    tmpR = ar("tmpR", [128, KC, T1])
    junk = ar("junk", [128, D])
    ssq = sb("ssq", [128, 4])
    rstd = sb("rstd", [128, 4])
    NB = 2
    tmp = {n: [ar("t_%s%d" % (n, i), [128, T1 + (4 if n == "rx" else 0)]) for i in range(NB)]
           for n in ("rx", "cv", "r", "i", "a", "m", "u", "hs", "g", "g2", "q")}
    cvb = [ar("cvb%d" % i, [128, T1], BF16) for i in range(NB)]
    vtm = [ar("vtm%d" % i, [128, D]) for i in range(2)]
    vt2 = [ar("vt2%d" % i, [128, D]) for i in range(2)]
    bnst = sb("bnst", [128, 2 * max(1, D // 512), 6])
    mv = sb("mv", [128, 2])
    x1st_tr = P.dma_track("x1st")
    h2st_tr = P.dma_track("h2st")
    h2tm = ar("h2tm", [128, S1, D], BF16)
    h2T_o = ar("h2T_o", [128, KC, T1], BF16)
    lg = sb("lg", [128, NE])
    top8 = sb("top8", [128, 8])
    negmx = sb("negmx", [128, 1])
    msk = sb("msk", [128, NE])
    ex = sb("ex", [128, NE])
    den = sb("den", [128, 1])

    def gelu(eng_alt, out, in_, n, reads, writes, tg, tgk):
        if USE_GELU_TANH_LUT:
            act(out, in_, AF.Gelu_apprx_tanh, reads, writes)
            return
        act(tg, in_, AF.Square, reads, [tgk])
        ts("dve", tg, tg, 0.044715, 1.0, ALU.mult, ALU.add, [tgk], [tgk])
        tt("dve", tg, tg, in_, ALU.mult, [tgk] + list(reads), [tgk])
        act(tg, tg, AF.Sigmoid, [tgk], [tgk], scale=1.5957691216057308)
        tt("dve", out, tg, in_, ALU.mult, [tgk] + list(reads), writes)

    def rmsnorm_to_T(src, src_key, nsub, dstT, dstT_key, scale_ap_fn, bias_ap_fn, sk_reads):
        for s in range(nsub):
            act(junk[:], src[:, s, :], AF.Square, [src_key], ["junk"], accum_out=ssq[:, s:s + 1])
            P.issue
            P.last_write[("ssq", s)] = P.last_write["junk"]
            P.readers[("ssq", s)] = []
        for s in range(nsub):
            act(rstd[:, s:s + 1], ssq[:, s:s + 1], AF.Sqrt, [("ssq", s)], [("rstd", s)], scale=1.0 / D, bias=EPS)
            P.op("dve", lambda e, s=s: e.reciprocal(rstd[:, s:s + 1], rstd[:, s:s + 1]), [("rstd", s)], [("rstd", s)])
            ts("dve", xn[:, s, :], src[:, s, :], rstd[:, s:s + 1], None, ALU.mult, None,
               [src_key, ("rstd", s)], [("xn", s)])
        for k in range(KC):
            pt, ptk = next_pst()
            for s in range(nsub):
                tr(pt[:, s * 128:(s + 1) * 128], xn[:, s, k * 128:(k + 1) * 128], ident_bf[:],
                   [("xn", s), "ident_bf"], [ptk])
            act(dstT[:, k, :nsub * 128], pt[:, :nsub * 128], AF.Identity, [ptk] + sk_reads, [dstT_key],
                scale=scale_ap_fn(k), bias=bias_ap_fn(k))

    for b in range(NSEQ):
        P.op("pool", lambda e: e.memset(hist[:], 0.0), [], ["hist"])
        P.op("pool", lambda e: e.memset(hstate[:], 0.0), [], ["hstate"])
        for j in range(NT1):
            tok0 = b * SEQ + j * T1
            dma("sp", X[:], x_d[tok0:tok0 + T1, :].rearrange("(s p) d -> p s d", p=128), [], ["X"], x_tr)
            rmsnorm_to_T(X, "X", S1, hT, "hT",
                         lambda k: s1[:, b, k:k + 1], lambda k: modT[:, b, k:k + 1], ["s1", "modT"])
            w0, w0k = wload(0)
            w1, w1k = wload(1)
            for c in range(KC):
                i_ = c % NB
                T = {n: tmp[n][i_] for n in tmp}
                K = {n: ("t", n, i_) for n in tmp}
                pz, pzk = next_ps()
                for k in range(KC):
                    mm(pz[:, :T1], w0[:, k, c * 128:(c + 1) * 128], hT[:, k, :], k == 0, k == KC - 1,
                       [w0k, "hT"], [pzk])
                cp("act", T["rx"][:, 0:3], hist[:, c, :], ["hist"], [K["rx"]])
                cp("act", T["rx"][:, 3:3 + T1], pz[:, :T1], [pzk], [K["rx"]])
                cp("act", hist[:, c, :], T["rx"][:, T1:T1 + 3], [K["rx"]], ["hist"])
                ts("dve", T["cv"][:, :], T["rx"][:, 0:T1], convwT[:, c, 0:1], convbT[:, c:c + 1], ALU.mult, ALU.add,
                   [K["rx"], "c_convwT", "c_convbT"], [K["cv"]])
                for kk in range(1, 4):
                    stt(T["cv"][:, :], T["rx"][:, kk:kk + T1], convwT[:, c, kk:kk + 1], T["cv"][:, :],
                        ALU.mult, ALU.add, [K["rx"], K["cv"], "c_convwT"], [K["cv"]])
                cp("pool", cvb[i_][:, :], T["cv"][:, :], [K["cv"]], [("cvb", i_)])
                pr, prk = next_ps()
                mm(pr[:, :T1], wbd[:, 0, c, :], cvb[i_][:, :], True, True, ["wbd", ("cvb", i_)], [prk])
                pi, pik = next_ps()
                mm(pi[:, :T1], wbd[:, 1, c, :], cvb[i_][:, :], True, True, ["wbd", ("cvb", i_)], [pik])
                act(T["r"][:, :], pr[:, :T1], AF.Sigmoid, [prk, "c_baT"], [K["r"]], bias=baT[:, c:c + 1])
                act(T["i"][:, :], pi[:, :T1], AF.Sigmoid, [pik, "c_bxT"], [K["i"]], bias=bxT[:, c:c + 1])
                act(T["a"][:, :], T["r"][:, :], AF.Exp, [K["r"], "kneg"], [K["a"]], scale=kneg[:, c:c + 1])
                act(T["m"][:, :], T["r"][:, :], AF.Exp, [K["r"], "k2"], [K["m"]], scale=k2[:, c:c + 1])
                act(T["m"][:, :], T["m"][:, :], AF.Sqrt, [K["m"]], [K["m"]], scale=-1.0, bias=1.0)
                tt("pool", T["u"][:, :], T["i"][:, :], T["cv"][:, :], ALU.mult, [K["i"], K["cv"]], [K["u"]])
                tt("pool", T["u"][:, :], T["u"][:, :], T["m"][:, :], ALU.mult, [K["u"], K["m"]], [K["u"]])
                P.op("dve", lambda e, T=T, c=c: e.tensor_tensor_scan(T["hs"][:, :], T["a"][:, :], T["u"][:, :],
                                                                       hstate[:, c:c + 1], ALU.mult, ALU.add),
                     [K["a"], K["u"], "hstate"], [K["hs"]])
                cp("pool", hstate[:, c:c + 1], T["hs"][:, T1 - 1:T1], [K["hs"]], ["hstate"])
                pg, pgk = next_ps()
                for k in range(KC):
                    mm(pg[:, :T1], w1[:, k, c * 128:(c + 1) * 128], hT[:, k, :], k == 0, k == KC - 1,
                       [w1k, "hT"], [pgk])
                gelu("dve", T["g"][:, :], pg[:, :T1], T1, [pgk], [K["g"]], T["g2"][:, :], K["g2"])
                tt("dve", yrT[:, c, :], T["hs"][:, :], T["g"][:, :], ALU.mult, [K["hs"], K["g"]], ["yrT"])
            w3, w3k = wload(3)
            for s in range(S1):
                vi = s % 2
                for cb in range(NCB):
                    pv, pvk = next_ps()
                    for k in range(KC):
                        mm(pv[:, :CB], hT[:, k, s * 128:(s + 1) * 128], w3[:, k, cb * CB:(cb + 1) * CB],
                           k == 0, k == KC - 1, [w3k, "hT"], [pvk])
                    gelu("dve", vtm[vi][:, cb * CB:(cb + 1) * CB], pv[:, :CB], CB, [pvk], [("vtm", vi)],
                         vt2[vi][:, cb * CB:(cb + 1) * CB], ("vt2", vi))
                nchunk = max(1, D // 512)
                cw = D // nchunk
                for q in range(nchunk):
                    P.op("dve", lambda e, vi=vi, q=q: e.bn_stats(bnst[:, q, :], vtm[vi][:, q * cw:(q + 1) * cw]),
                         [("vtm", vi)], ["bnst"])
                P.op("dve", lambda e: e.bn_aggr(mv[:], bnst[:, :nchunk, :].rearrange("p a b -> p (a b)")),
                     ["bnst"], ["mv"])
                act(mv[:, 1:2], mv[:, 1:2], AF.Sqrt, ["mv"], ["mv"], bias=EPS)
                P.op("dve", lambda e: e.reciprocal(mv[:, 1:2], mv[:, 1:2]), ["mv"], ["mv"])
                ts("dve", vtm[vi][:, :], vtm[vi][:, :], mv[:, 0:1], mv[:, 1:2], ALU.subtract, ALU.mult,
                   [("vtm", vi), "mv"], [("vtm", vi)])
                tt("pool", vtm[vi][:, :], vtm[vi][:, :], lng[:], ALU.mult, [("vtm", vi), "c_lng"], [("vtm", vi)])
                tt("pool", xn[:, s, :], vtm[vi][:, :], lnb[:], ALU.add, [("vtm", vi), "c_lnb"], [("xn", s)])
            w2, w2k = wload(2)
            for g in range(KC):
                i_ = g % NB
                T = {n: tmp[n][i_] for n in tmp}
                K = {n: ("t", n, i_) for n in tmp}
                pu, puk = next_ps()
                for k in range(KC):
                    mm(pu[:, :T1], w2[:, k, g * 128:(g + 1) * 128], hT[:, k, :], k == 0, k == KC - 1,
                       [w2k, "hT"], [puk])
                gelu("dve", T["g"][:, :], pu[:, :T1], T1, [puk], [K["g"]], T["g2"][:, :], K["g2"])
                psv, psvk = next_ps()
                for s in range(S1):
                    mm(psv[:, s * 128:(s + 1) * 128], xn[:, s, g * 128:(g + 1) * 128], wsT[:, g, :], True, True,
                       [("xn", s), "wsT"], [psvk])
                for s in range(S1):
                    tt("dve", T["q"][:, s * 128:(s + 1) * 128], psv[:, s * 128:(s + 1) * 128], bsb[:, g, :], ALU.add,
                       [psvk, "c_bsb"], [K["q"]])
                tt("pool", ysT[:, g, :], T["q"][:, :], T["g"][:, :], ALU.mult, [K["q"], K["g"]], ["ysT"])
            for pas, (ga, gb_, yT, yk) in enumerate(((4, 6, yrT, "yrT"), (5, 7, ysT, "ysT"))):
                wg, wgk = wload(ga)
                wb, wbk = wload(gb_)
                for oc in range(KC):
                    i_ = oc % NB
                    T = {n: tmp[n][i_] for n in tmp}
                    K = {n: ("t", n, i_) for n in tmp}
                    pgt, pgtk = next_ps()
                    for k in range(KC):
                        mm(pgt[:, :T1], wg[:, k, oc * 128:(oc + 1) * 128], hT[:, k, :], k == 0, k == KC - 1,
                           [wgk, "hT"], [pgtk])
                    act(T["g"][:, :], pgt[:, :T1], AF.Sigmoid, [pgtk], [K["g"]])
                    pbr, pbrk = next_ps()
                    for k in range(KC):
                        mm(pbr[:, :T1], wb[:, k, oc * 128:(oc + 1) * 128], yT[:, k, :], k == 0, k == KC - 1,
                           [wbk, yk], [pbrk])
                    if pas == 0:
                        tt("dve", tmpR[:, oc, :], T["g"][:, :], pbr[:, :T1], ALU.mult, [K["g"], pbrk], [("tmpR", oc)])
                    else:
                        tt("dve", T["i"][:, :], T["g"][:, :], pbr[:, :T1], ALU.mult, [K["g"], pbrk], [K["i"]])
                        tt("pool", mT[:, oc, :], tmpR[:, oc, :], T["i"][:, :], ALU.add, [("tmpR", oc), K["i"]], ["mT"])
            w8, w8k = wload(8)
            for s in range(S1):
                for cb in range(NCB):
                    po, pok = next_ps()
                    for k in range(KC):
                        mm(po[:, :CB], mT[:, k, s * 128:(s + 1) * 128], w8[:, k, cb * CB:(cb + 1) * CB],
                           k == 0, k == KC - 1, [w8k, "mT"], [pok])
                    tt("dve", junk[:, cb * CB:(cb + 1) * CB], po[:, :CB], gtb[:, 0, b, cb * CB:(cb + 1) * CB], ALU.mult,
                       [pok, "gtb"], ["junk"])
                    tt("pool", X[:, s, cb * CB:(cb + 1) * CB], X[:, s, cb * CB:(cb + 1) * CB],
                       junk[:, cb * CB:(cb + 1) * CB], ALU.add, ["X", "junk"], ["X"])
            dma("sp", x1_s[tok0:tok0 + T1, :].rearrange("(s p) d -> p s d", p=128), X[:], ["X"], ["x1_s"], x1st_tr)
            rmsnorm_to_T(X, "X", S1, h2T_o, "h2T_o",
                         lambda k: s2[:, b, k:k + 1], lambda k: modT[:, b, 3 * KC + k:3 * KC + k + 1], ["s2", "modT"])
            for s in range(S1):
                for k0 in range(0, KC, 4):
                    pt, ptk = next_pst()
                    nk = min(4, KC - k0)
                    for kk in range(nk):
                        tr(pt[:, kk * 128:(kk + 1) * 128], h2T_o[:, k0 + kk, s * 128:(s + 1) * 128], ident_bf[:],
                           ["h2T_o", "ident_bf"], [ptk])
                    cp("act", h2tm[:, s, k0 * 128:(k0 + nk) * 128], pt[:, :nk * 128], [ptk], ["h2tm"])
            dma("sp", h2tm_s[tok0:tok0 + T1, :].rearrange("(s p) d -> p s d", p=128), h2tm[:], ["h2tm"], ["h2tm_s"], h2st_tr)
            for s in range(S1):
                sub = (tok0 // 128) + s
                pl, plk = next_ps()
                for k in range(KC):
                    mm(pl[:, :NE], h2T_o[:, k, s * 128:(s + 1) * 128], wr[:, k, :], k == 0, k == KC - 1,
                       ["h2T_o", "wr"], [plk])
                tt("dve", lg[:], pl[:, :NE], brb[:], ALU.add, [plk, "c_brb"], ["lg"])
                P.op("dve", lambda e: e.max(top8[:], lg[:]), ["lg"], ["top8"])
                ts("dve", negmx[:], top8[:, 0:1], -1.0, None, ALU.mult, None, ["top8"], ["negmx"])
                ts("dve", msk[:], lg[:], top8[:, TOPK - 1:TOPK], None, ALU.is_ge, None, ["lg", "top8"], ["msk"])
                act(ex[:], lg[:], AF.Exp, ["lg", "negmx"], ["ex"], bias=negmx[:, 0:1])
                tt("dve", ex[:], ex[:], msk[:], ALU.mult, ["ex", "msk"], ["ex"])
                P.op("dve", lambda e: e.reduce_sum(den[:], ex[:], axis=mybir.AxisListType.X), ["ex"], ["den"])
                P.op("dve", lambda e: e.reciprocal(den[:], den[:]), ["den"], ["den"])
                ts("dve", wts[:, sub, :], ex[:], den[:, 0:1], None, ALU.mult, None, ["ex", "den"], ["wts"])

    P.barrier()
    A32.reset()
    A16.reset()
    LOGB = BR.bit_length() - 1
    d4f = sb("d4f", [128, NSUBS, 8])
    d4i = sb("d4i", [128, NSUBS, 4], I32)
    w4 = sb("w4", [128, NSUBS, 4])
    be_i = sb("be_i", [128, NBLK], I32)
    ld_i = sb("ld_i", [128, NBLK], I32)
    widx = sb("widx", [128, NBLK], I32)
    mask = ar("mask", [128, NSUBS, NE])
    dest = ar("dest", [128, NSUBS, NE])
    dkey = ar("dkey", [128, NSUBS, NE])
    ustr = ar("ustr", [128, 128])
    onesq = ar("onesq", [128, 128])
    macc = ar("macc", [128, NE])
    cnt = ar("cnt", [128, NE])
    padded = ar("padded", [128, NE])
    pend = ar("pend", [128, NE])
    pstart = ar("pstart", [128, NE])
    onesne = ar("onesne", [128, NE])
    eqt = ar("eqt", [128, NE])
    bst_i = A32.alloc([128, NBLK]).bitcast(I32)
    bst = ar("bst", [128, NBLK])
    bef = ar("bef", [128, NBLK])
    bet = ar("bet", [128, NBLK])
    P.op("pool", lambda e: e.memset(onesq[:], 1.0), [], ["onesq"])
    P.op("pool", lambda e: e.memset(onesne[:], 1.0), [], ["onesne"])
    P.op("pool", lambda e: e.memset(macc[:], 0.0), [], ["macc"])
    P.op("pool", lambda e: e.affine_select(out=ustr[:], in_=onesq[:], pattern=[[1, 128]], compare_op=ALU.is_ge,
                                           fill=0.0, base=-1, channel_multiplier=-1), ["onesq"], ["ustr"])
    ts("dve", mask[:], wts[:], 0.0, None, ALU.is_gt, None, ["wts"], ["mask"])
    for i in range(NSUBS):
        prk_t, prk = next_ps()
        mm(prk_t[:, :NE], ustr[:], mask[:, i, :], True, False, ["ustr", "mask"], [prk])
        mm(prk_t[:, :NE], onesq[:], macc[:], False, True, ["onesq", "macc"], [prk])
        cp("act", dest[:, i, :], prk_t[:, :NE], [prk], [("dest", i)])
        tt("dve", macc[:], macc[:], mask[:, i, :], ALU.add, ["macc", "mask"], ["macc"])
    pc_t, pck = next_ps()
    mm(pc_t[:, :NE], onesq[:], macc[:], True, True, ["onesq", "macc"], [pck])
    cp("dve", cnt[:], pc_t[:, :NE], [pck], ["cnt"])
    P.op("pool", lambda e: e.memset(padded[:], 0.0), [], ["padded"])
    for j in range(NTOK // BR):
        stt(padded[:], cnt[:], float(j * BR), padded[:], ALU.is_gt, ALU.add, ["cnt", "padded"], ["padded"])
    ts("dve", padded[:], padded[:], float(BR), None, ALU.mult, None, ["padded"], ["padded"])
    P.op("dve", lambda e: e.tensor_tensor_scan(pend[:], onesne[:], padded[:], 0.0, ALU.mult, ALU.add),
         ["onesne", "padded"], ["pend"])
    tt("dve", pstart[:], pend[:], padded[:], ALU.subtract, ["pend", "padded"], ["pstart"])
    for i in range(NSUBS):
        tt("dve", dest[:, i, :], dest[:, i, :], pstart[:], ALU.add, [("dest", i), "pstart"], [("dest", i)])
    dkeys = [("dest", i) for i in range(NSUBS)]
    stt(dkey[:], dest[:], 1.0, mask[:], ALU.add, ALU.mult, dkeys + ["mask"], ["dkey"])
    ts("dve", dkey[:], dkey[:], -1.0, None, ALU.add, None, ["dkey"], ["dkey"])
    for i in range(NSUBS):
        P.op("dve", lambda e, i=i: e.max(d4f[:, i, :], dkey[:, i, :]), ["dkey"], [("d4f", i)])
        for k4 in range(TOPK):
            ts("dve", eqt[:], dkey[:, i, :], d4f[:, i, k4:k4 + 1], None, ALU.is_equal, None, ["dkey", ("d4f", i)], ["eqt"])
            tt("dve", eqt[:], eqt[:], wts[:, i, :], ALU.mult, ["eqt", "wts"], ["eqt"])
            P.op("dve", lambda e, i=i, k4=k4: e.reduce_sum(w4[:, i, k4:k4 + 1], eqt[:], axis=mybir.AxisListType.X),
                 ["eqt"], ["w4"])
        cp("dve", d4i[:, i, :], d4f[:, i, 0:TOPK], [("d4f", i)], ["d4i"])
    P.op("pool", lambda e: e.iota(bst_i, pattern=[[BR, NBLK]], base=0, channel_multiplier=0), [], ["bst_i"])
    cp("dve", bst[:], bst_i, ["bst_i"], ["bst"])
    P.op("pool", lambda e: e.memset(bef[:], 0.0), [], ["bef"])
    for e_ in range(NE):
        ts("dve", bet[:], bst[:], pend[:, e_:e_ + 1], None, ALU.is_ge, None, ["bst", "pend"], ["bet"])
        tt("dve", bef[:], bef[:], bet[:], ALU.add, ["bef", "bet"], ["bef"])
    ts("dve", bef[:], bef[:], float(NE - 1), None, ALU.min, None, ["bef"], ["bef"])
    cp("dve", be_i[:], bef[:], ["bef"], ["be_i"])
    P.op("pool", lambda e: e.memset(bet[:], 1.0), ["bet"], ["bet"])
    if NBLK > 1:
        tt("dve", bet[:, 1:NBLK], bef[:, 1:NBLK], bef[:, 0:NBLK - 1], ALU.not_equal, ["bef", "bet"], ["bet"])
    cp("dve", ld_i[:], bet[:], ["bet"], ["ld_i"])
    pidx_i = A32.alloc([128, 1]).bitcast(I32)
    pidx = ar("pidx", [128, 1])
    widf = ar("widf", [128, NBLK])
    P.op("pool", lambda e: e.iota(pidx_i, pattern=[[0, 1]], base=0, channel_multiplier=1), [], ["pidx_i"])
    cp("dve", pidx[:], pidx_i, ["pidx_i"], ["pidx"])
    ts("dve", widf[:], bef[:], 128.0, pidx[:, 0:1], ALU.mult, ALU.add, ["bef", "pidx"], ["widf"])
    if USE_COND_SKIP:
        ts("dve", bet[:], bet[:], -1.0, -float(2 ** 30), ALU.add, ALU.mult, ["bet"], ["bet"])
        tt("dve", widf[:], widf[:], bet[:], ALU.add, ["widf", "bet"], ["widf"])
    cp("dve", widx[:], widf[:], ["widf"], ["widx"])

    if cfg.get("DEBUG"):
        dbg_tr = P.dma_track("dbg")
        MX = max(NE, NBLK)
        dd = {"dbg_d4f": (d4f, [128, NSUBS, 8], ["d4i"]), "dbg_w4": (w4, [128, NSUBS, 4], ["w4"]),
              "dbg_wts": (wts, [128, NSUBS, NE], ["wts"]), "dbg_cnt": (cnt, [128, NE], ["cnt"]),
              "dbg_pend": (pend, [128, NE], ["pend"]), "dbg_bef": (bef, [128, NBLK], ["bef"]),
              "dbg_widf": (widf, [128, NBLK], ["widx"]), "dbg_dest": (dest, [128, NSUBS, NE], ["dkey"]),
              "dbg_dkey": (dkey, [128, NSUBS, NE], ["dkey", "d4i"])}
        for nm, (t_, shp, rd) in dd.items():
            o_ = nc.dram_tensor(nm, shp, F32, kind="ExternalOutput").ap()
            dma("sp", o_, t_[:], rd, [nm], dbg_tr)
    P.barrier()
    A32.reset()
    A16.reset()
    zt = ar("zt", [128, 4, D], BF16)
    z_tr = P.dma_track("zfill")
    P.op("pool", lambda e: e.memset(zt[:], 0.0), [], ["zt"])
    for r0 in range(0, NBLK * BR, 512):
        dma("sp", xs_s[r0:r0 + 512, :].rearrange("(s p) d -> p s d", p=128), zt[:], ["zt"], ["xs_s"], z_tr)
    hsc = [ar("hsc%d" % i, [128, D], BF16) for i in range(2)]
    hsc_tr = [P.dma_track("hsc%d" % i) for i in range(2)]
    sc_tr = [P.dma_track("scat%d" % i) for i in range(2)]
    for i in range(NSUBS):
        hi = i % 2
        dma("sp", hsc[hi][:], h2tm_s[i * 128:(i + 1) * 128, :], ["h2tm_s"], [("hsc", hi)], hsc_tr[hi])
        for k4 in range(TOPK):
            P.op("pool", lambda e, hi=hi, i=i, k4=k4: e.indirect_dma_start(
                out=xs_s[:, :], out_offset=bass.IndirectOffsetOnAxis(ap=d4i[:, i, k4:k4 + 1], axis=0),
                in_=hsc[hi][:, :], in_offset=None), [("hsc", hi), "d4i", "xs_s"], ["xs_sc"], track=sc_tr[hi])
    P.barrier()
    A32.reset()
    A16.reset()

    wq = [ar("wq%d" % i, [128, KC, QW], BF16) for i in range(NQ)]
    wd = [ar("wd%d" % i, [128, CE, CB], BF16) for i in range(ND)]
    wq_tr = [P.dma_track("wq%d" % i) for i in range(NQ)]
    wd_tr = [P.dma_track("wd%d" % i) for i in range(ND)]
    bg = [sb("bg%d" % i, [128, 2 * CE]) for i in range(2)]
    bg_tr = [P.dma_track("bg%d" % i) for i in range(2)]
    xb = [ar("xb%d" % i, [128, SB, D], BF16) for i in range(2)]
    xb_tr = [P.dma_track("xb%d" % i) for i in range(2)]
    XT = [ar("XT%d" % i, [128, KC, BR], BF16) for i in range(2)]
    actT = [ar("actT%d" % i, [128, CE, BR], BF16) for i in range(2)]
    mt = {n: [ar("m_%s%d" % (n, i), [128, BR]) for i in range(2)] for n in ("g", "s", "u")}
    Yt = [ar("Yt%d" % i, [128, SB, D]) for i in range(2)]
    y_tr = [P.dma_track("yst%d" % i) for i in range(2)]
    POOL_ET = mybir.EngineType.Pool

    def dyn_load(dst_ap, src_rows, blk, reads, writes, track):
        def fn(e):
            kw = {}
            if USE_COND_SKIP:
                if "r" not in breg:
                    breg["r"] = e.to_reg(NE * 128 - 1)
                kw = dict(bounds_check=breg["r"], oob_is_err=False)
            return e.indirect_dma_start(out=dst_ap, out_offset=None, in_=src_rows,
                                        in_offset=bass.IndirectOffsetOnAxis(ap=widx[:, blk:blk + 1], axis=0), **kw)
        P.op("pool", fn, reads, writes, track=track)

    breg = {}
    wq_rows = [wq_s[q].rearrange("e p a b -> (e p) (a b)") for q in range(NQ)]
    wd_rows = [wd_s[d_].rearrange("e p a b -> (e p) (a b)") for d_ in range(ND)]
    bg_rows = bguT_d.rearrange("e p a -> (e p) a")
    for blk in range(NBLK):
        bi = blk % 2
        dma("sp", xb[bi][:], xs_s[blk * BR:(blk + 1) * BR, :].rearrange("(s p) d -> p s d", p=128),
            ["xs_sc", "xs_s"], [("xb", bi)], xb_tr[bi])
        for q in range(NQ):
            dyn_load(wq[q].rearrange("p a b -> p (a b)"), wq_rows[q], blk, ["widx", "wq_s"], [("wq", q)], wq_tr[q])
        for d_ in range(ND):
            dyn_load(wd[d_].rearrange("p a b -> p (a b)"), wd_rows[d_], blk, ["widx", "wd_s"], [("wd", d_)], wd_tr[d_])
        dyn_load(bg[0][:], bg_rows, blk, ["widx"], [("bg", 0)], bg_tr[0])
        for k in range(KC):
            pt, ptk = next_pst()
            for s_ in range(SB):
                tr(pt[:, s_ * 128:(s_ + 1) * 128], xb[bi][:, s_, k * 128:(k + 1) * 128], ident_bf[:],
                   [("xb", bi), "ident_bf"], [ptk])
            cp("act" if k % 2 == 0 else "dve", XT[bi][:, k, :], pt[:, :BR], [ptk], [("XT", bi)])
        aT = actT[bi]
        for c in range(CE):
            mi = c % 2
            G_, S_, U_ = mt["g"][mi], mt["s"][mi], mt["u"][mi]
            gk, sk, uk = ("mg", mi), ("ms", mi), ("mu", mi)
            gcol = c * 128
            ucol = DE + c * 128
            pgm, pgmk = next_ps()
            for k in range(KC):
                mm(pgm[:, :BR], wq[gcol // QW][:, k, gcol % QW:gcol % QW + 128], XT[bi][:, k, :],
                   k == 0, k == KC - 1, [("wq", gcol // QW), ("XT", bi)], [pgmk])
            pum, pumk = next_ps()
            for k in range(KC):
                mm(pum[:, :BR], wq[ucol // QW][:, k, ucol % QW:ucol % QW + 128], XT[bi][:, k, :],
                   k == 0, k == KC - 1, [("wq", ucol // QW), ("XT", bi)], [pumk])
            ts("dve", G_[:, :], pgm[:, :BR], bg[0][:, c:c + 1], LIMIT, ALU.add, ALU.min, [pgmk, ("bg", 0)], [gk])
            act(S_[:, :], G_[:, :], AF.Sigmoid, [gk], [sk], scale=ALPHA)
            ts("dve", U_[:, :], pum[:, :BR], bg[0][:, CE + c:CE + c + 1], LIMIT, ALU.add, ALU.min,
               [pumk, ("bg", 0)], [uk])
            ts("dve", U_[:, :], U_[:, :], -LIMIT, 1.0, ALU.max, ALU.add, [uk], [uk])
            tt("dve", S_[:, :], S_[:, :], G_[:, :], ALU.mult, [sk, gk], [sk])
            tt("dve", aT[:, c, :], U_[:, :], S_[:, :], ALU.mult, [uk, sk], [("actT", bi)])
        for s_ in range(SB):
            for cb in range(NCB):
                pd, pdk = next_ps()
                for k in range(CE):
                    mm(pd[:, :CB], aT[:, k, s_ * 128:(s_ + 1) * 128], wd[cb][:, k, :], k == 0, k == CE - 1,
                       [("actT", bi), ("wd", cb)], [pdk])
                cp("act", Yt[bi][:, s_, cb * CB:(cb + 1) * CB], pd[:, :CB], [pdk], [("Yt", bi)])
        dma("sp", ys_s[blk * BR:(blk + 1) * BR, :].rearrange("(s p) d -> p s d", p=128), Yt[bi][:],
            [("Yt", bi)], ["ys_s"], y_tr[bi])

    P.barrier()
    A32.reset()
    A16.reset()
    junk = ar("junk2", [128, D])
    wtsT = sb("wtsT", [NE, 128])
    Gt = [[ar("G%d_%d" % (i, k4), [128, D]) for k4 in range(TOPK)] for i in range(2)]
    g_tr = [[P.dma_track("g%d_%d" % (i, k4)) for k4 in range(TOPK)] for i in range(2)]
    acc = [ar("acc%d" % i, [128, D]) for i in range(2)]
    x1t = [ar("x1t%d" % i, [128, D]) for i in range(2)]
    x1_tr = [P.dma_track("x1t%d" % i) for i in range(2)]
    ot_tr = [P.dma_track("ot%d" % i) for i in range(2)]
    for i in range(NSUBS):
        xi = i % 2
        r0 = i * 128
        b = r0 // SEQ
        dma("sp", x1t[xi][:], x1_s[r0:r0 + 128, :], ["x1_s"], [("x1t", xi)], x1_tr[xi])
        for k4 in range(TOPK):
            P.op("pool", lambda e, xi=xi, i=i, k4=k4: e.indirect_dma_start(
                out=Gt[xi][k4][:, :], out_offset=None, in_=ys_s[:, :],
                in_offset=bass.IndirectOffsetOnAxis(ap=d4i[:, i, k4:k4 + 1], axis=0)),
                ["ys_s", "d4i"], [("G", xi, k4)], track=g_tr[xi][k4])
        pw, pwk = next_ps()
        tr(pw[:NE, :128], wts[:, i, :], ident32[:], ["wts", "ident32"], [pwk])
        cp("dve", wtsT[:, :], pw[:NE, :128], [pwk], ["wtsT"])
        for cb in range(NCB):
            pb, pbk = next_ps()
            mm(pb[:, :CB], wtsT[:, :], bdn[:, cb * CB:(cb + 1) * CB], True, True, ["wtsT", "c_bdn"], [pbk])
            cp("act", acc[xi][:, cb * CB:(cb + 1) * CB], pb[:, :CB], [pbk], [("acc", xi)])
        for k4 in range(TOPK):
            stt(acc[xi][:], Gt[xi][k4][:], w4[:, i, k4:k4 + 1], acc[xi][:], ALU.mult, ALU.add,
                [("G", xi, k4), "w4", ("acc", xi)], [("acc", xi)])
        tt("pool", acc[xi][:], acc[xi][:], gtb[:, 1, b, :], ALU.mult, [("acc", xi), "gtb"], [("acc", xi)])
        tt("dve", x1t[xi][:], x1t[xi][:], acc[xi][:], ALU.add, [("x1t", xi), ("acc", xi)], [("x1t", xi)])
        act(junk[:], x1t[xi][:], AF.Square, [("x1t", xi)], ["junk"], accum_out=ssq[:, 0:1])
        P.last_write[("ssq", 0)] = P.last_write["junk"]
        P.readers[("ssq", 0)] = []
        act(rstd[:, 0:1], ssq[:, 0:1], AF.Sqrt, [("ssq", 0)], [("rstd", 0)], scale=1.0 / D, bias=EPS)
        P.op("dve", lambda e: e.reciprocal(rstd[:, 0:1], rstd[:, 0:1]), [("rstd", 0)], [("rstd", 0)])
        stt(x1t[xi][:], x1t[xi][:], rstd[:, 0:1], fgb[:], ALU.mult, ALU.mult,
            [("x1t", xi), ("rstd", 0), "c_fgb"], [("x1t", xi)])
        dma("sp", out_d[r0:r0 + 128, :], x1t[xi][:], [("x1t", xi)], ["out"], ot_tr[xi])
    P.wait_all("sp", ot_tr)
    print("[build] sbuf bytes remaining/partition:", nc.sbuf_bytes_remaining, "A32 hi", A32.hi * 4, "A16 hi", A16.hi * 2,
          "n_ops", sum(len(v) for v in P.issue.values()), flush=True)
    P.emit(st)
    st.close()
    return nc


def _layout(inputs, cfg):
    D, DE, NE, SEQ, NSEQ, NCORES = cfg["D"], cfg["DE"], cfg["NE"], cfg["SEQ"], cfg["NSEQ"], cfg["NCORES"]
    KC, CE = D // 128, DE // 128
    f = lambda a: np.ascontiguousarray(np.asarray(a, dtype=np.float32))
    g = {k: np.asarray(v) for k, v in inputs.items()}

    def fm(v):
        return f(v.reshape(-1, 128).T)

    def km(w):
        return f(w.reshape(-1, 128, w.shape[-1]).transpose(1, 0, 2))

    def bc(v):
        return f(np.broadcast_to(v[None, :], (128, v.shape[0])))
    shared = {}
    shared["ada_w"] = km(g["ada_w"][0])
    ada_b = g["ada_b"][0]
    shared["ada_bT"] = fm(ada_b)
    shared["ada_bg"] = f(np.stack([np.broadcast_to(ada_b[2 * D:3 * D], (128, D)),
                                   np.broadcast_to(ada_b[5 * D:6 * D], (128, D))], axis=1))
    shared["g1T"] = fm(g["norm1_g"][0])
    shared["g2T"] = fm(g["norm2_g"][0])
    shared["w_in"] = km(g["w_in"][0])
    shared["conv_wT"] = f(g["conv_w"][0].reshape(4, KC, 128).transpose(2, 1, 0))
    shared["conv_bT"] = fm(g["conv_b"][0])
    shared["lru_wa"] = f(g["lru_wa"][0].reshape(KC, 2, 64, 64))
    shared["lru_wx"] = f(g["lru_wx"][0].reshape(KC, 2, 64, 64))
    shared["lru_baT"] = fm(g["lru_ba"][0])
    shared["lru_bxT"] = fm(g["lru_bx"][0])
    shared["lamT"] = fm(g["lru_lam"][0])
    shared["ln_g_b"] = bc(g["sg_ln_g"][0])
    shared["ln_b_b"] = bc(g["sg_ln_b"][0])
    shared["sg_wsT"] = f(g["sg_ws"][0].transpose(2, 0, 1))
    shared["sg_bs_b"] = f(np.broadcast_to(g["sg_bs"][0][None], (128, KC, 128)))
    shared["w_br"] = f(np.stack([km(g["w_br_rnn"][0]), km(g["w_br_sg"][0]), km(g["w_out"][0])]))
    shared["w_router"] = km(g["w_router"][0])
    shared["b_router_b"] = bc(g["b_router"][0])
    shared["w_gu"] = f(g["w_gu"][0].reshape(NE, KC, 128, 2 * DE).transpose(0, 2, 1, 3))
    shared["b_guT"] = f(g["b_gu"][0].reshape(NE, 2 * CE, 128).transpose(0, 2, 1))
    shared["w_down"] = f(g["w_down"][0].reshape(NE, CE, 128, D).transpose(0, 2, 1, 3))
    shared["b_down"] = f(g["b_down"][0])
    shared["final_g_b"] = bc(g["final_g"])
    x = g["x"].reshape(NCORES, NSEQ * SEQ, D)
    c = g["c"].reshape(NCORES, NSEQ, KC, 128)
    maps = []
    for i in range(NCORES):
        m = dict(shared)
        m["x"] = f(x[i])
        m["cT"] = f(c[i].transpose(2, 1, 0))
        maps.append(m)
    return maps


_NC_CACHE = {}


def run(inputs, cfg):
    key = tuple(sorted(cfg.items()))
    if key not in _NC_CACHE:
        _NC_CACHE[key] = build_nc(cfg)
    nc = _NC_CACHE[key]
    maps = _layout(inputs, cfg)
    res = run_bass_kernel_spmd(nc, maps, core_ids=list(range(cfg["NCORES"])))
    if cfg.get("DEBUG"):
        global DBG
        DBG = res.results
    out = np.stack([r["out"] for r in res.results], axis=0)
    B = cfg["NCORES"] * cfg["NSEQ"]
    return out.reshape(B, cfg["SEQ"], cfg["D"]).astype(np.float32)


def kernel(**inputs):
    return run(inputs, CFG_FULL)
```

```python
from contextlib import ExitStack
import numpy as np
import concourse.bass as bass
import concourse.mybir as mybir
from concourse.bass_utils import run_bass_kernel_spmd

F32 = mybir.dt.float32
BF16 = mybir.dt.bfloat16
I32 = mybir.dt.int32
AF = mybir.ActivationFunctionType
ALU = mybir.AluOpType
ENGS = ("pe", "act", "dve", "pool", "sp")

CFG_FULL = dict(D=1024, DE=1024, NE=32, SEQ=4096, NSEQ=2, NCORES=8)
EPS = 1e-6
LIMIT = 7.0
ALPHA = 1.702
TOPK = 4
USE_GELU_TANH_LUT = False
BR = 256
USE_COND_SKIP = True


class Prog:
    def __init__(self, nc):
        self.nc = nc
        self.tracks = {}
        self.issue = {e: [] for e in ENGS}
        self.last_write = {}
        self.readers = {}
        self.waited = {e: {} for e in ENGS}
        self.n_dma_tracks = 0

    def dma_track(self, name=""):
        self.n_dma_tracks += 1
        return "dma:%d:%s" % (self.n_dma_tracks, name)

    def op(self, eng, fn, reads=(), writes=(), track=None):
        track = track or eng
        tl = self.tracks.setdefault(track, [])
        idx = len(tl)
        deps = {}

        def add(d):
            t, i = d
            if t == track and t.startswith("dma:"):
                return
            if deps.get(t, -1) < i:
                deps[t] = i
        for k in reads:
            lw = self.last_write.get(k)
            if lw is not None:
                add(lw)
        for k in writes:
            lw = self.last_write.get(k)
            if lw is not None and lw[0] != track:
                add(lw)
            for r in self.readers.get(k, ()):
                if r[0] != track:
                    add(r)
        waits = []
        wd = self.waited[eng]
        for t, i in deps.items():
            if t.startswith("dma:"):
                i = len(self.tracks[t]) - 1
            if wd.get(t, -1) >= i:
                continue
            if t == eng and eng == "pe":
                continue
            wd[t] = i
            self.tracks[t][i]["need"] = True
            waits.append((t, i))
        rec = dict(eng=eng, track=track, fn=fn, waits=waits, need=track.startswith("dma:"))
        tl.append(rec)
        self.issue[eng].append(rec)
        for k in reads:
            self.readers.setdefault(k, []).append((track, idx))
        for k in writes:
            self.last_write[k] = (track, idx)
            self.readers[k] = []
        return rec

    def wait_all(self, eng, tracks):
        waits = []
        for t in tracks:
            tl = self.tracks.get(t)
            if not tl:
                continue
            i = len(tl) - 1
            tl[i]["need"] = True
            waits.append((t, i))
        self.issue[eng].append(dict(eng=eng, track=eng, fn=None, waits=waits, need=False))

    def barrier(self):
        tr = list(self.tracks.keys())
        for e in ENGS:
            self.wait_all(e, tr)
            for t in tr:
                self.waited[e][t] = len(self.tracks[t]) - 1

    def emit(self, stack):
        nc = self.nc
        sems, cum = {}, {}
        for n, (t, tl) in enumerate(self.tracks.items()):
            sems[t] = stack.enter_context(nc.semaphore("s%d" % n))
            c, step, arr = 0, (16 if t.startswith("dma:") else 1), []
            for rec in tl:
                if rec["need"]:
                    c += step
                arr.append(c)
            cum[t] = arr
        block = stack.enter_context(nc.Block())
        engobj = {"pe": "tensor", "act": "scalar", "dve": "vector", "pool": "gpsimd", "sp": "sync"}

        def make(engname):
            recs = self.issue[engname]

            def body(e):
                for rec in recs:
                    for (t, i) in rec["waits"]:
                        e.wait_ge(sems[t], cum[t][i])
                    if rec["fn"] is None:
                        continue
                    ins = rec["fn"](e)
                    if rec["need"]:
                        t = rec["track"]
                        ins.then_inc(sems[t], 16 if t.startswith("dma:") else 1)
            return body
        for engname in ENGS:
            if self.issue[engname]:
                getattr(block, engobj[engname])(make(engname))


class Arena:
    def __init__(self, nc, st, name, dt, nelem):
        self.t = st.enter_context(nc.sbuf_tensor(name, [128, nelem], dt))
        self.n, self.off, self.hi = nelem, 0, 0

    def reset(self):
        self.off = 0

    def alloc(self, shape):
        n = 1
        for d in shape[1:]:
            n *= d
        o = self.off
        self.off += n
        self.hi = max(self.hi, self.off)
        assert self.off <= self.n, ("arena overflow", self.off, self.n)
        ap = self.t[:shape[0], o:o + n]
        if len(shape) == 3:
            ap = ap.rearrange("p (a b) -> p a b", a=shape[1])
        elif len(shape) == 4:
            ap = ap.rearrange("p (a b c) -> p a b c", a=shape[1], b=shape[2])
        return ap


def build_nc(cfg):
    D, DE, NE, SEQ, NSEQ = cfg["D"], cfg["DE"], cfg["NE"], cfg["SEQ"], cfg["NSEQ"]
    KC, CE = D // 128, DE // 128
    NTOK = NSEQ * SEQ
    T1 = 256
    S1 = T1 // 128
    NT1 = SEQ // T1
    T2 = 512
    G2 = min(1024, NTOK)
    NG = NTOK // G2
    CB = min(512, D)
    NCB = D // CB
    D6 = 6 * D
    NSUBS = NTOK // 128
    KCM = max(KC, CE)
    PW = 256

    nc = bass.Bass("TRN2", target_bir_lowering=False)
    st = ExitStack()
    P = Prog(nc)

    def din(name, shape, dt=F32):
        return nc.dram_tensor(name, list(shape), dt, kind="ExternalInput").ap()

    def dscr(name, shape, dt):
        return nc.dram_tensor(name, list(shape), dt, kind="Internal").ap()

    x_d = din("x", [NTOK, D])
    cT_d = din("cT", [128, KC, NSEQ])
    adaw_d = din("ada_w", [128, KC, D6])
    adabT_d = din("ada_bT", [128, 6 * KC])
    adabg_d = din("ada_bg", [128, 2, D])
    g1T_d = din("g1T", [128, KC])
    g2T_d = din("g2T", [128, KC])
    win_d = din("w_in", [128, KC, D6])
    convwT_d = din("conv_wT", [128, KC, 4])
    convbT_d = din("conv_bT", [128, KC])
    lruwa_d = din("lru_wa", [KC, 2, 64, 64])
    lruwx_d = din("lru_wx", [KC, 2, 64, 64])
    baT_d = din("lru_baT", [128, KC])
    bxT_d = din("lru_bxT", [128, KC])
    lamT_d = din("lamT", [128, KC])
    lng_d = din("ln_g_b", [128, D])
    lnb_d = din("ln_b_b", [128, D])
    wsT_d = din("sg_wsT", [128, KC, 128])
    bsb_d = din("sg_bs_b", [128, KC, 128])
    wbr_d = din("w_br", [3, 128, KC, D])
    wr_d = din("w_router", [128, KC, NE])
    brb_d = din("b_router_b", [128, NE])
    wgu_d = din("w_gu", [NE, 128, KC, 2 * DE])
    bguT_d = din("b_guT", [NE, 128, 2 * CE])
    wdn_d = din("w_down", [NE, 128, CE, D])
    bdn_d = din("b_down", [NE, D])
    fgb_d = din("final_g_b", [128, D])
    out_d = nc.dram_tensor("out", [NTOK, D], F32, kind="ExternalOutput").ap()

    wmix_s = dscr("wmix_s", [9, 128, KC, D], BF16)
    QW = min(512, 2 * DE)
    NQ = (2 * DE) // QW
    ND = NCB
    NBLK = (NTOK * TOPK) // BR + NE
    SB = BR // 128
    wq_s = [dscr("wq_s%d" % q, [NE, 128, KC, QW], BF16) for q in range(NQ)]
    wd_s = [dscr("wd_s%d" % d_, [NE, 128, CE, CB], BF16) for d_ in range(ND)]
    x1_s = dscr("x1_s", [NTOK, D], F32)
    h2tm_s = dscr("h2tm_s", [NTOK, D], BF16)
    xs_s = dscr("xs_s", [NBLK * BR, D], BF16)
    ys_s = dscr("ys_s", [NBLK * BR, D], F32)

    def sb(name, shape, dt=F32):
        return st.enter_context(nc.sbuf_tensor(name, list(shape), dt))

    A32 = Arena(nc, st, "arena32", F32, cfg.get("A32", 15104))
    A16 = Arena(nc, st, "arena16", BF16, cfg.get("A16", 40960))

    def ar(name, shape, dt=F32):
        return (A32 if dt == F32 else A16).alloc(list(shape))

    NPS = 5
    ps = [st.enter_context(nc.psum_tensor("ps%d" % i, [128, 512], F32)) for i in range(NPS)]
    pst = [st.enter_context(nc.psum_tensor("pst%d" % i, [128, 512], BF16)) for i in range(2)]
    psm = st.enter_context(nc.psum_tensor("psm", [128, 512], F32))
    psmk = "psm"
    psc = [0]
    pstc = [0]

    def next_ps():
        i = psc[0] % NPS
        psc[0] += 1
        return ps[i], ("ps", i)

    def next_pst():
        i = pstc[0] % 2
        pstc[0] += 1
        return pst[i], ("pst", i)

    def mm(out, lhsT, rhs, start, stop, reads, writes):
        P.op("pe", lambda e: e.matmul(out, lhsT, rhs, start=start, stop=stop), reads, writes)

    def tr(out, in_, ident, reads, writes):
        P.op("pe", lambda e: e.transpose(out, in_, ident), reads, writes)

    def act(out, in_, func, reads, writes, bias=None, scale=None, accum_out=None, eng="act"):
        kw = {}
        if bias is not None:
            kw["bias"] = bias
        if scale is not None:
            kw["scale"] = scale
        if accum_out is not None:
            kw["accum_out"] = accum_out
        P.op("act", lambda e: e.activation(out, in_, func, **kw), reads, writes)

    def ts(eng, out, in0, s1, s2, op0, op1, reads, writes):
        if op1 is None:
            P.op(eng, lambda e: e.tensor_scalar(out, in0, s1, None, op0), reads, writes)
        else:
            P.op(eng, lambda e: e.tensor_scalar(out, in0, s1, s2, op0, op1), reads, writes)

    def tt(eng, out, in0, in1, op, reads, writes):
        P.op(eng, lambda e: e.tensor_tensor(out, in0, in1, op), reads, writes)

    def stt(out, in0, scalar, in1, op0, op1, reads, writes):
        P.op("dve", lambda e: e.scalar_tensor_tensor(out, in0, scalar, in1, op0, op1), reads, writes)

    def cp(eng, out, in_, reads, writes):
        if eng == "act":
            P.op("act", lambda e: e.copy(out, in_), reads, writes)
        else:
            P.op(eng, lambda e: e.tensor_copy(out, in_), reads, writes)

    def dma(q, out, in_, reads, writes, track):
        P.op(q, lambda e: e.dma_start(out=out, in_=in_), reads, writes, track=track)

    ctrack = P.dma_track("const")
    consts = {}

    def cload(name, dram, shape, dt=F32, q="sp", arena=False):
        t = ar("c_" + name, shape, dt) if arena else sb("c_" + name, shape, dt)
        dma(q, t[:], dram, [], ["c_" + name], ctrack)
        consts[name] = t
        return t

    cT = cload("cT", cT_d, [128, KC, NSEQ])
    adabT = cload("adabT", adabT_d, [128, 6 * KC])
    adabg = cload("adabg", adabg_d, [128, 2, D], arena=True)
    g1T = cload("g1T", g1T_d, [128, KC])
    g2T = cload("g2T", g2T_d, [128, KC])
    convwT = cload("convwT", convwT_d, [128, KC, 4])
    convbT = cload("convbT", convbT_d, [128, KC])
    baT = cload("baT", baT_d, [128, KC])
    bxT = cload("bxT", bxT_d, [128, KC])
    lamT = cload("lamT", lamT_d, [128, KC])
    lng = cload("lng", lng_d, [128, D])
    lnb = cload("lnb", lnb_d, [128, D])
    wsT32 = cload("wsT32", wsT_d, [128, KC, 128], arena=True)
    bsb = cload("bsb", bsb_d, [128, KC, 128])
    wr32 = cload("wr32", wr_d, [128, KC, NE], arena=True)
    brb = cload("brb", brb_d, [128, NE])
    bdn = cload("bdn", bdn_d, [NE, D])
    fgb = cload("fgb", fgb_d, [128, D])
    wbd32 = ar("wbd32", [128, 2, KC, 128])
    P.op("pool", lambda e: e.memset(wbd32[:], 0.0), [], ["c_wbd32"])
    for gi, src in enumerate((lruwa_d, lruwx_d)):
        for half in range(2):
            dma("sp", wbd32[half * 64:(half + 1) * 64, gi, :, half * 64:(half + 1) * 64],
                src[:, half].rearrange("k i j -> i k j"), [], ["c_wbd32"], ctrack)

    ident_bf = sb("ident_bf", [128, 128], BF16)
    ident32 = sb("ident32", [128, 128], F32)
    ones32 = ar("ones32", [128, 128], F32)
    P.op("pool", lambda e: e.memset(ones32[:], 1.0), [], ["ones32"])
    P.op("pool", lambda e: e.affine_select(out=ident32[:], in_=ones32[:], pattern=[[-1, 128]],
                                           compare_op=ALU.is_equal, fill=0.0, base=0, channel_multiplier=1),
         ["ones32"], ["ident32"])
    cp("dve", ident_bf[:], ident32[:], ["ident32"], ["ident_bf"])

    wbd = sb("wbd", [128, 2, KC, 128], BF16)
    cp("dve", wbd[:], wbd32[:], ["c_wbd32"], ["wbd"])
    wsT = sb("wsT", [128, KC, 128], BF16)
    wsTm = ar("wsTm", [128, KC, 128], F32)
    P.op("pool", lambda e: e.affine_select(out=wsTm[:], in_=wsT32[:], pattern=[[0, KC], [1, 128]],
                                           compare_op=ALU.is_ge, fill=0.0, base=0, channel_multiplier=-1),
         ["c_wsT32"], ["wsTm"])
    cp("dve", wsT[:], wsTm[:], ["wsTm"], ["wsT"])
    wr = sb("wr", [128, KC, NE], BF16)
    cp("dve", wr[:], wr32[:], ["c_wr32"], ["wr"])

    kneg = sb("kneg", [128, KC])
    k2 = sb("k2", [128, KC])
    ktmp = sb("ktmp", [128, KC])
    act(ktmp[:], lamT[:], AF.Exp, ["c_lamT"], ["ktmp"], scale=-1.0)
    act(ktmp[:], ktmp[:], AF.Ln, ["ktmp"], ["ktmp"], bias=1.0)
    ts("dve", kneg[:], ktmp[:], -8.0, None, ALU.mult, None, ["ktmp"], ["kneg"])
    ts("dve", k2[:], ktmp[:], -16.0, None, ALU.mult, None, ["ktmp"], ["k2"])

    sc = sb("sc", [128, KC, NSEQ])
    sgt = sb("sgt", [128, KC, NSEQ])
    act(sgt[:], cT[:], AF.Sigmoid, ["c_cT"], ["sgt"])
    tt("dve", sc[:], sgt[:], cT[:], ALU.mult, ["sgt", "c_cT"], ["sc"])
    screp = ar("screp", [128, NSEQ, KC, 128])
    for b in range(NSEQ):
        for k in range(KC):
            cp("dve", screp[:, b, k, :], sc[:, k, b:b + 1].to_broadcast([128, 128]), ["sc"], ["screp"])
    modT = sb("modT", [128, NSEQ, 6 * KC])
    gtb = sb("gtb", [128, 2, NSEQ, D])
    stg = [ar("stg%d" % i, [128, KCM, PW]) for i in range(2)]
    stg_tr = [P.dma_track("stg%d" % i) for i in range(2)]
    stgc = [0]

    def stage_load(dram_ap, q="sp"):
        i = stgc[0] % 2
        stgc[0] += 1
        dma(q, stg[i][:, :dram_ap.shape[1], :dram_ap.shape[2]], dram_ap, [], [("stg", i)], stg_tr[i])
        return stg[i], ("stg", i)

    NJB = D6 // PW
    for jb in range(NJB):
        s_t, s_k = stage_load(adaw_d[:, :, jb * PW:(jb + 1) * PW])
        for jj in range(PW // 128):
            j = jb * (PW // 128) + jj
            for b in range(NSEQ):
                for k in range(KC):
                    col = b * 6 * KC + j
                    mm(psm[:, col:col + 1], s_t[:, k, jj * 128:(jj + 1) * 128], sc[:, k, b:b + 1],
                       k == 0, k == KC - 1, [s_k, "sc"], [psmk])
        m = (jb * PW) // D
        if m in (2, 5):
            which = 0 if m == 2 else 1
            c0 = (jb * PW) % D
            for b in range(NSEQ):
                pg, pgk = next_ps()
                for k in range(KC):
                    mm(pg[:, :PW], screp[:, b, k, :], s_t[:, k, :], k == 0, k == KC - 1, [s_k, "screp"], [pgk])
                tt("dve", gtb[:, which, b, c0:c0 + PW], pg[:, :PW], adabg[:, which, c0:c0 + PW], ALU.add,
                   [pgk, "c_adabg"], ["gtb"])
    for b in range(NSEQ):
        tt("dve", modT[:, b, :], psm[:, b * 6 * KC:(b + 1) * 6 * KC], adabT[:], ALU.add,
           [psmk, "c_adabT"], ["modT"])
    s1 = sb("s1", [128, NSEQ, KC])
    s2 = sb("s2", [128, NSEQ, KC])
    for b in range(NSEQ):
        stt(s1[:, b, :], modT[:, b, KC:2 * KC], 1.0, g1T[:], ALU.add, ALU.mult, ["modT", "c_g1T"], ["s1"])
        stt(s2[:, b, :], modT[:, b, 4 * KC:5 * KC], 1.0, g2T[:], ALU.add, ALU.mult, ["modT", "c_g2T"], ["s2"])

    cbf = [ar("cbf%d" % i, [128, KCM, PW], BF16) for i in range(2)]
    cbf_tr = [P.dma_track("cbf%d" % i) for i in range(2)]
    cbc = [0]

    def precast(src_ap, dst_ap, dst_key):
        kc, w = src_ap.shape[1], src_ap.shape[2]
        s_t, s_k = stage_load(src_ap)
        i = cbc[0] % 2
        cbc[0] += 1
        eng = "act" if i == 0 else "pool"
        cp(eng, cbf[i][:, :kc, :w], s_t[:, :kc, :w], [s_k], [("cbf", i)])
        dma("sp", dst_ap, cbf[i][:, :kc, :w], [("cbf", i)], [dst_key], cbf_tr[i])

    for blk in range(9):
        for c0 in range(0, D, PW):
            w = min(PW, D - c0)
            src = win_d[:, :, blk * D + c0: blk * D + c0 + w] if blk < 6 else wbr_d[blk - 6][:, :, c0:c0 + w]
            precast(src, wmix_s[blk][:, :, c0:c0 + w], ("wmix", blk))
    for e_ in range(NE):
        for c0 in range(0, 2 * DE, PW):
            w = min(PW, 2 * DE - c0)
            precast(wgu_d[e_][:, :, c0:c0 + w], wq_s[c0 // QW][e_][:, :, c0 % QW:c0 % QW + w], "wq_s")
        for c0 in range(0, D, PW):
            w = min(PW, D - c0)
            precast(wdn_d[e_][:, :, c0:c0 + w], wd_s[c0 // CB][e_][:, :, c0 % CB:c0 % CB + w], "wd_s")

    P.barrier()
    A32.reset()
    A16.reset()
    wts = sb("wts", [128, NSUBS, NE])
    hist = sb("hist", [128, KC, 3])
    hstate = sb("hstate", [128, KC])
    RING = 3
    wring = [ar("wring%d" % i, [128, KC, D], BF16) for i in range(RING)]
    wring_tr = [P.dma_track("wring%d" % i) for i in range(RING)]
    wrc = [0]

    def wload(blk):
        i = wrc[0] % RING
        wrc[0] += 1
        dma("sp", wring[i][:], wmix_s[blk], [("wmix", blk)], [("wring", i)], wring_tr[i])
        return wring[i], ("wring", i)

    X = ar("X", [128, S1, D])
    x_tr = P.dma_track("x")
    xn = ar("xn", [128, S1, D], BF16)
    hT = ar("hT", [128, KC, T1], BF16)
    yrT = ar("yrT", [128, KC, T1], BF16)
    ysT = ar("ysT", [128, KC, T1], BF16)
    mT = ar("mT", [128, KC, T1], BF16)
    tmpR = ar("tmpR", [128, KC, T1])
    junk = ar("junk", [128, D])
    ssq = sb("ssq", [128, 4])
    rstd = sb("rstd", [128, 4])
    NB = 2
    tmp = {n: [ar("t_%s%d" % (n, i), [128, T1 + (4 if n == "rx" else 0)]) for i in range(NB)]
           for n in ("rx", "cv", "r", "i", "a", "m", "u", "hs", "g", "g2", "q")}
    cvb = [ar("cvb%d" % i, [128, T1], BF16) for i in range(NB)]
    vtm = [ar("vtm%d" % i, [128, D]) for i in range(2)]
    vt2 = [ar("vt2%d" % i, [128, D]) for i in range(2)]
    bnst = sb("bnst", [128, 2 * max(1, D // 512), 6])
    mv = sb("mv", [128, 2])
    x1st_tr = P.dma_track("x1st")
    h2st_tr = P.dma_track("h2st")
    h2tm = ar("h2tm", [128, S1, D], BF16)
    h2T_o = ar("h2T_o", [128, KC, T1], BF16)
    lg = sb("lg", [128, NE])
    top8 = sb("top8", [128, 8])
    negmx = sb("negmx", [128, 1])
    msk = sb("msk", [128, NE])
    ex = sb("ex", [128, NE])
    den = sb("den", [128, 1])

    def gelu(eng_alt, out, in_, n, reads, writes, tg, tgk):
        if USE_GELU_TANH_LUT:
            act(out, in_, AF.Gelu_apprx_tanh, reads, writes)
            return
        act(tg, in_, AF.Square, reads, [tgk])
        ts("dve", tg, tg, 0.044715, 1.0, ALU.mult, ALU.add, [tgk], [tgk])
        tt("dve", tg, tg, in_, ALU.mult, [tgk] + list(reads), [tgk])
        act(tg, tg, AF.Sigmoid, [tgk], [tgk], scale=1.5957691216057308)
        tt("dve", out, tg, in_, ALU.mult, [tgk] + list(reads), writes)

    def rmsnorm_to_T(src, src_key, nsub, dstT, dstT_key, scale_ap_fn, bias_ap_fn, sk_reads):
        for s in range(nsub):
            act(junk[:], src[:, s, :], AF.Square, [src_key], ["junk"], accum_out=ssq[:, s:s + 1])
            P.issue
            P.last_write[("ssq", s)] = P.last_write["junk"]
            P.readers[("ssq", s)] = []
        for s in range(nsub):
            act(rstd[:, s:s + 1], ssq[:, s:s + 1], AF.Sqrt, [("ssq", s)], [("rstd", s)], scale=1.0 / D, bias=EPS)
            P.op("dve", lambda e, s=s: e.reciprocal(rstd[:, s:s + 1], rstd[:, s:s + 1]), [("rstd", s)], [("rstd", s)])
            ts("dve", xn[:, s, :], src[:, s, :], rstd[:, s:s + 1], None, ALU.mult, None,
               [src_key, ("rstd", s)], [("xn", s)])
        for k in range(KC):
            pt, ptk = next_pst()
            for s in range(nsub):
                tr(pt[:, s * 128:(s + 1) * 128], xn[:, s, k * 128:(k + 1) * 128], ident_bf[:],
                   [("xn", s), "ident_bf"], [ptk])
            act(dstT[:, k, :nsub * 128], pt[:, :nsub * 128], AF.Identity, [ptk] + sk_reads, [dstT_key],
                scale=scale_ap_fn(k), bias=bias_ap_fn(k))

    for b in range(NSEQ):
        P.op("pool", lambda e: e.memset(hist[:], 0.0), [], ["hist"])
        P.op("pool", lambda e: e.memset(hstate[:], 0.0), [], ["hstate"])
        for j in range(NT1):
            tok0 = b * SEQ + j * T1
            dma("sp", X[:], x_d[tok0:tok0 + T1, :].rearrange("(s p) d -> p s d", p=128), [], ["X"], x_tr)
            rmsnorm_to_T(X, "X", S1, hT, "hT",
                         lambda k: s1[:, b, k:k + 1], lambda k: modT[:, b, k:k + 1], ["s1", "modT"])
            w0, w0k = wload(0)
            w1, w1k = wload(1)
            for c in range(KC):
                i_ = c % NB
                T = {n: tmp[n][i_] for n in tmp}
                K = {n: ("t", n, i_) for n in tmp}
                pz, pzk = next_ps()
                for k in range(KC):
                    mm(pz[:, :T1], w0[:, k, c * 128:(c + 1) * 128], hT[:, k, :], k == 0, k == KC - 1,
                       [w0k, "hT"], [pzk])
                cp("act", T["rx"][:, 0:3], hist[:, c, :], ["hist"], [K["rx"]])
                cp("act", T["rx"][:, 3:3 + T1], pz[:, :T1], [pzk], [K["rx"]])
                cp("act", hist[:, c, :], T["rx"][:, T1:T1 + 3], [K["rx"]], ["hist"])
                ts("dve", T["cv"][:, :], T["rx"][:, 0:T1], convwT[:, c, 0:1], convbT[:, c:c + 1], ALU.mult, ALU.add,
                   [K["rx"], "c_convwT", "c_convbT"], [K["cv"]])
                for kk in range(1, 4):
                    stt(T["cv"][:, :], T["rx"][:, kk:kk + T1], convwT[:, c, kk:kk + 1], T["cv"][:, :],
                        ALU.mult, ALU.add, [K["rx"], K["cv"], "c_convwT"], [K["cv"]])
                cp("pool", cvb[i_][:, :], T["cv"][:, :], [K["cv"]], [("cvb", i_)])
                pr, prk = next_ps()
                mm(pr[:, :T1], wbd[:, 0, c, :], cvb[i_][:, :], True, True, ["wbd", ("cvb", i_)], [prk])
                pi, pik = next_ps()
                mm(pi[:, :T1], wbd[:, 1, c, :], cvb[i_][:, :], True, True, ["wbd", ("cvb", i_)], [pik])
                act(T["r"][:, :], pr[:, :T1], AF.Sigmoid, [prk, "c_baT"], [K["r"]], bias=baT[:, c:c + 1])
                act(T["i"][:, :], pi[:, :T1], AF.Sigmoid, [pik, "c_bxT"], [K["i"]], bias=bxT[:, c:c + 1])
                act(T["a"][:, :], T["r"][:, :], AF.Exp, [K["r"], "kneg"], [K["a"]], scale=kneg[:, c:c + 1])
                act(T["m"][:, :], T["r"][:, :], AF.Exp, [K["r"], "k2"], [K["m"]], scale=k2[:, c:c + 1])
                act(T["m"][:, :], T["m"][:, :], AF.Sqrt, [K["m"]], [K["m"]], scale=-1.0, bias=1.0)
                tt("pool", T["u"][:, :], T["i"][:, :], T["cv"][:, :], ALU.mult, [K["i"], K["cv"]], [K["u"]])
                tt("pool", T["u"][:, :], T["u"][:, :], T["m"][:, :], ALU.mult, [K["u"], K["m"]], [K["u"]])
                P.op("dve", lambda e, T=T, c=c: e.tensor_tensor_scan(T["hs"][:, :], T["a"][:, :], T["u"][:, :],
                                                                       hstate[:, c:c + 1], ALU.mult, ALU.add),
                     [K["a"], K["u"], "hstate"], [K["hs"]])
                cp("pool", hstate[:, c:c + 1], T["hs"][:, T1 - 1:T1], [K["hs"]], ["hstate"])
                pg, pgk = next_ps()
                for k in range(KC):
                    mm(pg[:, :T1], w1[:, k, c * 128:(c + 1) * 128], hT[:, k, :], k == 0, k == KC - 1,
                       [w1k, "hT"], [pgk])
                gelu("dve", T["g"][:, :], pg[:, :T1], T1, [pgk], [K["g"]], T["g2"][:, :], K["g2"])
                tt("dve", yrT[:, c, :], T["hs"][:, :], T["g"][:, :], ALU.mult, [K["hs"], K["g"]], ["yrT"])
            w3, w3k = wload(3)
            for s in range(S1):
                vi = s % 2
                for cb in range(NCB):
                    pv, pvk = next_ps()
                    for k in range(KC):
                        mm(pv[:, :CB], hT[:, k, s * 128:(s + 1) * 128], w3[:, k, cb * CB:(cb + 1) * CB],
                           k == 0, k == KC - 1, [w3k, "hT"], [pvk])
                    gelu("dve", vtm[vi][:, cb * CB:(cb + 1) * CB], pv[:, :CB], CB, [pvk], [("vtm", vi)],
                         vt2[vi][:, cb * CB:(cb + 1) * CB], ("vt2", vi))
                nchunk = max(1, D // 512)
                cw = D // nchunk
                for q in range(nchunk):
                    P.op("dve", lambda e, vi=vi, q=q: e.bn_stats(bnst[:, q, :], vtm[vi][:, q * cw:(q + 1) * cw]),
                         [("vtm", vi)], ["bnst"])
                P.op("dve", lambda e: e.bn_aggr(mv[:], bnst[:, :nchunk, :].rearrange("p a b -> p (a b)")),
                     ["bnst"], ["mv"])
                act(mv[:, 1:2], mv[:, 1:2], AF.Sqrt, ["mv"], ["mv"], bias=EPS)
                P.op("dve", lambda e: e.reciprocal(mv[:, 1:2], mv[:, 1:2]), ["mv"], ["mv"])
                ts("dve", vtm[vi][:, :], vtm[vi][:, :], mv[:, 0:1], mv[:, 1:2], ALU.subtract, ALU.mult,
                   [("vtm", vi), "mv"], [("vtm", vi)])
                tt("pool", vtm[vi][:, :], vtm[vi][:, :], lng[:], ALU.mult, [("vtm", vi), "c_lng"], [("vtm", vi)])
                tt("pool", xn[:, s, :], vtm[vi][:, :], lnb[:], ALU.add, [("vtm", vi), "c_lnb"], [("xn", s)])
            w2, w2k = wload(2)
            for g in range(KC):
                i_ = g % NB
                T = {n: tmp[n][i_] for n in tmp}
                K = {n: ("t", n, i_) for n in tmp}
                pu, puk = next_ps()
                for k in range(KC):
                    mm(pu[:, :T1], w2[:, k, g * 128:(g + 1) * 128], hT[:, k, :], k == 0, k == KC - 1,
                       [w2k, "hT"], [puk])
                gelu("dve", T["g"][:, :], pu[:, :T1], T1, [puk], [K["g"]], T["g2"][:, :], K["g2"])
                psv, psvk = next_ps()
                for s in range(S1):
                    mm(psv[:, s * 128:(s + 1) * 128], xn[:, s, g * 128:(g + 1) * 128], wsT[:, g, :], True, True,
                       [("xn", s), "wsT"], [psvk])
                for s in range(S1):
                    tt("dve", T["q"][:, s * 128:(s + 1) * 128], psv[:, s * 128:(s + 1) * 128], bsb[:, g, :], ALU.add,
                       [psvk, "c_bsb"], [K["q"]])
                tt("pool", ysT[:, g, :], T["q"][:, :], T["g"][:, :], ALU.mult, [K["q"], K["g"]], ["ysT"])
            for pas, (ga, gb_, yT, yk) in enumerate(((4, 6, yrT, "yrT"), (5, 7, ysT, "ysT"))):
                wg, wgk = wload(ga)
                wb, wbk = wload(gb_)
                for oc in range(KC):
                    i_ = oc % NB
                    T = {n: tmp[n][i_] for n in tmp}
                    K = {n: ("t", n, i_) for n in tmp}
                    pgt, pgtk = next_ps()
                    for k in range(KC):
                        mm(pgt[:, :T1], wg[:, k, oc * 128:(oc + 1) * 128], hT[:, k, :], k == 0, k == KC - 1,
                           [wgk, "hT"], [pgtk])
                    act(T["g"][:, :], pgt[:, :T1], AF.Sigmoid, [pgtk], [K["g"]])
                    pbr, pbrk = next_ps()
                    for k in range(KC):
                        mm(pbr[:, :T1], wb[:, k, oc * 128:(oc + 1) * 128], yT[:, k, :], k == 0, k == KC - 1,
                           [wbk, yk], [pbrk])
                    if pas == 0:
                        tt("dve", tmpR[:, oc, :], T["g"][:, :], pbr[:, :T1], ALU.mult, [K["g"], pbrk], [("tmpR", oc)])
                    else:
                        tt("dve", T["i"][:, :], T["g"][:, :], pbr[:, :T1], ALU.mult, [K["g"], pbrk], [K["i"]])
                        tt("pool", mT[:, oc, :], tmpR[:, oc, :], T["i"][:, :], ALU.add, [("tmpR", oc), K["i"]], ["mT"])
            w8, w8k = wload(8)
            for s in range(S1):
                for cb in range(NCB):
                    po, pok = next_ps()
                    for k in range(KC):
                        mm(po[:, :CB], mT[:, k, s * 128:(s + 1) * 128], w8[:, k, cb * CB:(cb + 1) * CB],
                           k == 0, k == KC - 1, [w8k, "mT"], [pok])
                    tt("dve", junk[:, cb * CB:(cb + 1) * CB], po[:, :CB], gtb[:, 0, b, cb * CB:(cb + 1) * CB], ALU.mult,
                       [pok, "gtb"], ["junk"])
                    tt("pool", X[:, s, cb * CB:(cb + 1) * CB], X[:, s, cb * CB:(cb + 1) * CB],
                       junk[:, cb * CB:(cb + 1) * CB], ALU.add, ["X", "junk"], ["X"])
            dma("sp", x1_s[tok0:tok0 + T1, :].rearrange("(s p) d -> p s d", p=128), X[:], ["X"], ["x1_s"], x1st_tr)
            rmsnorm_to_T(X, "X", S1, h2T_o, "h2T_o",
                         lambda k: s2[:, b, k:k + 1], lambda k: modT[:, b, 3 * KC + k:3 * KC + k + 1], ["s2", "modT"])
            for s in range(S1):
                for k0 in range(0, KC, 4):
                    pt, ptk = next_pst()
                    nk = min(4, KC - k0)
                    for kk in range(nk):
                        tr(pt[:, kk * 128:(kk + 1) * 128], h2T_o[:, k0 + kk, s * 128:(s + 1) * 128], ident_bf[:],
                           ["h2T_o", "ident_bf"], [ptk])
                    cp("act", h2tm[:, s, k0 * 128:(k0 + nk) * 128], pt[:, :nk * 128], [ptk], ["h2tm"])
            dma("sp", h2tm_s[tok0:tok0 + T1, :].rearrange("(s p) d -> p s d", p=128), h2tm[:], ["h2tm"], ["h2tm_s"], h2st_tr)
            for s in range(S1):
                sub = (tok0 // 128) + s
                pl, plk = next_ps()
                for k in range(KC):
                    mm(pl[:, :NE], h2T_o[:, k, s * 128:(s + 1) * 128], wr[:, k, :], k == 0, k == KC - 1,
                       ["h2T_o", "wr"], [plk])
                tt("dve", lg[:], pl[:, :NE], brb[:], ALU.add, [plk, "c_brb"], ["lg"])
                P.op("dve", lambda e: e.max(top8[:], lg[:]), ["lg"], ["top8"])
                ts("dve", negmx[:], top8[:, 0:1], -1.0, None, ALU.mult, None, ["top8"], ["negmx"])
                ts("dve", msk[:], lg[:], top8[:, TOPK - 1:TOPK], None, ALU.is_ge, None, ["lg", "top8"], ["msk"])
                act(ex[:], lg[:], AF.Exp, ["lg", "negmx"], ["ex"], bias=negmx[:, 0:1])
                tt("dve", ex[:], ex[:], msk[:], ALU.mult, ["ex", "msk"], ["ex"])
                P.op("dve", lambda e: e.reduce_sum(den[:], ex[:], axis=mybir.AxisListType.X), ["ex"], ["den"])
                P.op("dve", lambda e: e.reciprocal(den[:], den[:]), ["den"], ["den"])
                ts("dve", wts[:, sub, :], ex[:], den[:, 0:1], None, ALU.mult, None, ["ex", "den"], ["wts"])

    P.barrier()
    A32.reset()
    A16.reset()
    LOGB = BR.bit_length() - 1
    d4f = sb("d4f", [128, NSUBS, 8])
    d4i = sb("d4i", [128, NSUBS, 4], I32)
    w4 = sb("w4", [128, NSUBS, 4])
    be_i = sb("be_i", [128, NBLK], I32)
    ld_i = sb("ld_i", [128, NBLK], I32)
    widx = sb("widx", [128, NBLK], I32)
    mask = ar("mask", [128, NSUBS, NE])
    dest = ar("dest", [128, NSUBS, NE])
    dkey = ar("dkey", [128, NSUBS, NE])
    ustr = ar("ustr", [128, 128])
    onesq = ar("onesq", [128, 128])
    macc = ar("macc", [128, NE])
    cnt = ar("cnt", [128, NE])
    padded = ar("padded", [128, NE])
    pend = ar("pend", [128, NE])
    pstart = ar("pstart", [128, NE])
    onesne = ar("onesne", [128, NE])
    eqt = ar("eqt", [128, NE])
    bst_i = A32.alloc([128, NBLK]).bitcast(I32)
    bst = ar("bst", [128, NBLK])
    bef = ar("bef", [128, NBLK])
    bet = ar("bet", [128, NBLK])
    P.op("pool", lambda e: e.memset(onesq[:], 1.0), [], ["onesq"])
    P.op("pool", lambda e: e.memset(onesne[:], 1.0), [], ["onesne"])
    P.op("pool", lambda e: e.memset(macc[:], 0.0), [], ["macc"])
    P.op("pool", lambda e: e.affine_select(out=ustr[:], in_=onesq[:], pattern=[[1, 128]], compare_op=ALU.is_ge,
                                           fill=0.0, base=-1, channel_multiplier=-1), ["onesq"], ["ustr"])
    ts("dve", mask[:], wts[:], 0.0, None, ALU.is_gt, None, ["wts"], ["mask"])
    for i in range(NSUBS):
        prk_t, prk = next_ps()
        mm(prk_t[:, :NE], ustr[:], mask[:, i, :], True, False, ["ustr", "mask"], [prk])
        mm(prk_t[:, :NE], onesq[:], macc[:], False, True, ["onesq", "macc"], [prk])
        cp("act", dest[:, i, :], prk_t[:, :NE], [prk], [("dest", i)])
        tt("dve", macc[:], macc[:], mask[:, i, :], ALU.add, ["macc", "mask"], ["macc"])
    pc_t, pck = next_ps()
    mm(pc_t[:, :NE], onesq[:], macc[:], True, True, ["onesq", "macc"], [pck])
    cp("dve", cnt[:], pc_t[:, :NE], [pck], ["cnt"])
    P.op("pool", lambda e: e.memset(padded[:], 0.0), [], ["padded"])
    for j in range(NTOK // BR):
        stt(padded[:], cnt[:], float(j * BR), padded[:], ALU.is_gt, ALU.add, ["cnt", "padded"], ["padded"])
    ts("dve", padded[:], padded[:], float(BR), None, ALU.mult, None, ["padded"], ["padded"])
    P.op("dve", lambda e: e.tensor_tensor_scan(pend[:], onesne[:], padded[:], 0.0, ALU.mult, ALU.add),
         ["onesne", "padded"], ["pend"])
    tt("dve", pstart[:], pend[:], padded[:], ALU.subtract, ["pend", "padded"], ["pstart"])
    for i in range(NSUBS):
        tt("dve", dest[:, i, :], dest[:, i, :], pstart[:], ALU.add, [("dest", i), "pstart"], [("dest", i)])
    dkeys = [("dest", i) for i in range(NSUBS)]
    stt(dkey[:], dest[:], 1.0, mask[:], ALU.add, ALU.mult, dkeys + ["mask"], ["dkey"])
    ts("dve", dkey[:], dkey[:], -1.0, None, ALU.add, None, ["dkey"], ["dkey"])
    for i in range(NSUBS):
        P.op("dve", lambda e, i=i: e.max(d4f[:, i, :], dkey[:, i, :]), ["dkey"], [("d4f", i)])
        for k4 in range(TOPK):
            ts("dve", eqt[:], dkey[:, i, :], d4f[:, i, k4:k4 + 1], None, ALU.is_equal, None, ["dkey", ("d4f", i)], ["eqt"])
            tt("dve", eqt[:], eqt[:], wts[:, i, :], ALU.mult, ["eqt", "wts"], ["eqt"])
            P.op("dve", lambda e, i=i, k4=k4: e.reduce_sum(w4[:, i, k4:k4 + 1], eqt[:], axis=mybir.AxisListType.X),
                 ["eqt"], ["w4"])
        cp("dve", d4i[:, i, :], d4f[:, i, 0:TOPK], [("d4f", i)], ["d4i"])
    P.op("pool", lambda e: e.iota(bst_i, pattern=[[BR, NBLK]], base=0, channel_multiplier=0), [], ["bst_i"])
    cp("dve", bst[:], bst_i, ["bst_i"], ["bst"])
    P.op("pool", lambda e: e.memset(bef[:], 0.0), [], ["bef"])
    for e_ in range(NE):
        ts("dve", bet[:], bst[:], pend[:, e_:e_ + 1], None, ALU.is_ge, None, ["bst", "pend"], ["bet"])
        tt("dve", bef[:], bef[:], bet[:], ALU.add, ["bef", "bet"], ["bef"])
    ts("dve", bef[:], bef[:], float(NE - 1), None, ALU.min, None, ["bef"], ["bef"])
    cp("dve", be_i[:], bef[:], ["bef"], ["be_i"])
    P.op("pool", lambda e: e.memset(bet[:], 1.0), ["bet"], ["bet"])
    if NBLK > 1:
        tt("dve", bet[:, 1:NBLK], bef[:, 1:NBLK], bef[:, 0:NBLK - 1], ALU.not_equal, ["bef", "bet"], ["bet"])
    cp("dve", ld_i[:], bet[:], ["bet"], ["ld_i"])
    pidx_i = A32.alloc([128, 1]).bitcast(I32)
    pidx = ar("pidx", [128, 1])
    widf = ar("widf", [128, NBLK])
    P.op("pool", lambda e: e.iota(pidx_i, pattern=[[0, 1]], base=0, channel_multiplier=1), [], ["pidx_i"])
    cp("dve", pidx[:], pidx_i, ["pidx_i"], ["pidx"])
    ts("dve", widf[:], bef[:], 128.0, pidx[:, 0:1], ALU.mult, ALU.add, ["bef", "pidx"], ["widf"])
    if USE_COND_SKIP:
        ts("dve", bet[:], bet[:], -1.0, -float(2 ** 30), ALU.add, ALU.mult, ["bet"], ["bet"])
        tt("dve", widf[:], widf[:], bet[:], ALU.add, ["widf", "bet"], ["widf"])
    cp("dve", widx[:], widf[:], ["widf"], ["widx"])

    if cfg.get("DEBUG"):
        dbg_tr = P.dma_track("dbg")
        MX = max(NE, NBLK)
        dd = {"dbg_d4f": (d4f, [128, NSUBS, 8], ["d4i"]), "dbg_w4": (w4, [128, NSUBS, 4], ["w4"]),
              "dbg_wts": (wts, [128, NSUBS, NE], ["wts"]), "dbg_cnt": (cnt, [128, NE], ["cnt"]),
              "dbg_pend": (pend, [128, NE], ["pend"]), "dbg_bef": (bef, [128, NBLK], ["bef"]),
              "dbg_widf": (widf, [128, NBLK], ["widx"]), "dbg_dest": (dest, [128, NSUBS, NE], ["dkey"]),
              "dbg_dkey": (dkey, [128, NSUBS, NE], ["dkey", "d4i"])}
        for nm, (t_, shp, rd) in dd.items():
            o_ = nc.dram_tensor(nm, shp, F32, kind="ExternalOutput").ap()
            dma("sp", o_, t_[:], rd, [nm], dbg_tr)
    P.barrier()
    A32.reset()
    A16.reset()
    zt = ar("zt", [128, 4, D], BF16)
    z_tr = P.dma_track("zfill")
    P.op("pool", lambda e: e.memset(zt[:], 0.0), [], ["zt"])
    for r0 in range(0, NBLK * BR, 512):
        dma("sp", xs_s[r0:r0 + 512, :].rearrange("(s p) d -> p s d", p=128), zt[:], ["zt"], ["xs_s"], z_tr)
    hsc = [ar("hsc%d" % i, [128, D], BF16) for i in range(2)]
    hsc_tr = [P.dma_track("hsc%d" % i) for i in range(2)]
    sc_tr = [P.dma_track("scat%d" % i) for i in range(2)]
    for i in range(NSUBS):
        hi = i % 2
        dma("sp", hsc[hi][:], h2tm_s[i * 128:(i + 1) * 128, :], ["h2tm_s"], [("hsc", hi)], hsc_tr[hi])
        for k4 in range(TOPK):
            P.op("pool", lambda e, hi=hi, i=i, k4=k4: e.indirect_dma_start(
                out=xs_s[:, :], out_offset=bass.IndirectOffsetOnAxis(ap=d4i[:, i, k4:k4 + 1], axis=0),
                in_=hsc[hi][:, :], in_offset=None), [("hsc", hi), "d4i", "xs_s"], ["xs_sc"], track=sc_tr[hi])
    P.barrier()
    A32.reset()
    A16.reset()

    wq = [ar("wq%d" % i, [128, KC, QW], BF16) for i in range(NQ)]
    wd = [ar("wd%d" % i, [128, CE, CB], BF16) for i in range(ND)]
    wq_tr = [P.dma_track("wq%d" % i) for i in range(NQ)]
    wd_tr = [P.dma_track("wd%d" % i) for i in range(ND)]
    bg = [sb("bg%d" % i, [128, 2 * CE]) for i in range(2)]
    bg_tr = [P.dma_track("bg%d" % i) for i in range(2)]
    xb = [ar("xb%d" % i, [128, SB, D], BF16) for i in range(2)]
    xb_tr = [P.dma_track("xb%d" % i) for i in range(2)]
    XT = [ar("XT%d" % i, [128, KC, BR], BF16) for i in range(2)]
    actT = [ar("actT%d" % i, [128, CE, BR], BF16) for i in range(2)]
    mt = {n: [ar("m_%s%d" % (n, i), [128, BR]) for i in range(2)] for n in ("g", "s", "u")}
    Yt = [ar("Yt%d" % i, [128, SB, D]) for i in range(2)]
    y_tr = [P.dma_track("yst%d" % i) for i in range(2)]
    POOL_ET = mybir.EngineType.Pool

    def dyn_load(dst_ap, src_rows, blk, reads, writes, track):
        def fn(e):
            kw = {}
            if USE_COND_SKIP:
                if "r" not in breg:
                    breg["r"] = e.to_reg(NE * 128 - 1)
                kw = dict(bounds_check=breg["r"], oob_is_err=False)
            return e.indirect_dma_start(out=dst_ap, out_offset=None, in_=src_rows,
                                        in_offset=bass.IndirectOffsetOnAxis(ap=widx[:, blk:blk + 1], axis=0), **kw)
        P.op("pool", fn, reads, writes, track=track)

    breg = {}
    wq_rows = [wq_s[q].rearrange("e p a b -> (e p) (a b)") for q in range(NQ)]
    wd_rows = [wd_s[d_].rearrange("e p a b -> (e p) (a b)") for d_ in range(ND)]
    bg_rows = bguT_d.rearrange("e p a -> (e p) a")
    for blk in range(NBLK):
        bi = blk % 2
        dma("sp", xb[bi][:], xs_s[blk * BR:(blk + 1) * BR, :].rearrange("(s p) d -> p s d", p=128),
            ["xs_sc", "xs_s"], [("xb", bi)], xb_tr[bi])
        for q in range(NQ):
            dyn_load(wq[q].rearrange("p a b -> p (a b)"), wq_rows[q], blk, ["widx", "wq_s"], [("wq", q)], wq_tr[q])
        for d_ in range(ND):
            dyn_load(wd[d_].rearrange("p a b -> p (a b)"), wd_rows[d_], blk, ["widx", "wd_s"], [("wd", d_)], wd_tr[d_])
        dyn_load(bg[0][:], bg_rows, blk, ["widx"], [("bg", 0)], bg_tr[0])
        for k in range(KC):
            pt, ptk = next_pst()
            for s_ in range(SB):
                tr(pt[:, s_ * 128:(s_ + 1) * 128], xb[bi][:, s_, k * 128:(k + 1) * 128], ident_bf[:],
                   [("xb", bi), "ident_bf"], [ptk])
            cp("act" if k % 2 == 0 else "dve", XT[bi][:, k, :], pt[:, :BR], [ptk], [("XT", bi)])
        aT = actT[bi]
        for c in range(CE):
            mi = c % 2
            G_, S_, U_ = mt["g"][mi], mt["s"][mi], mt["u"][mi]
            gk, sk, uk = ("mg", mi), ("ms", mi), ("mu", mi)
            gcol = c * 128
            ucol = DE + c * 128
            pgm, pgmk = next_ps()
            for k in range(KC):
                mm(pgm[:, :BR], wq[gcol // QW][:, k, gcol % QW:gcol % QW + 128], XT[bi][:, k, :],
                   k == 0, k == KC - 1, [("wq", gcol // QW), ("XT", bi)], [pgmk])
            pum, pumk = next_ps()
            for k in range(KC):
                mm(pum[:, :BR], wq[ucol // QW][:, k, ucol % QW:ucol % QW + 128], XT[bi][:, k, :],
                   k == 0, k == KC - 1, [("wq", ucol // QW), ("XT", bi)], [pumk])
            ts("dve", G_[:, :], pgm[:, :BR], bg[0][:, c:c + 1], LIMIT, ALU.add, ALU.min, [pgmk, ("bg", 0)], [gk])
            act(S_[:, :], G_[:, :], AF.Sigmoid, [gk], [sk], scale=ALPHA)
            ts("dve", U_[:, :], pum[:, :BR], bg[0][:, CE + c:CE + c + 1], LIMIT, ALU.add, ALU.min,
               [pumk, ("bg", 0)], [uk])
            ts("dve", U_[:, :], U_[:, :], -LIMIT, 1.0, ALU.max, ALU.add, [uk], [uk])
            tt("dve", S_[:, :], S_[:, :], G_[:, :], ALU.mult, [sk, gk], [sk])
            tt("dve", aT[:, c, :], U_[:, :], S_[:, :], ALU.mult, [uk, sk], [("actT", bi)])
        for s_ in range(SB):
            for cb in range(NCB):
                pd, pdk = next_ps()
                for k in range(CE):
                    mm(pd[:, :CB], aT[:, k, s_ * 128:(s_ + 1) * 128], wd[cb][:, k, :], k == 0, k == CE - 1,
                       [("actT", bi), ("wd", cb)], [pdk])
                cp("act", Yt[bi][:, s_, cb * CB:(cb + 1) * CB], pd[:, :CB], [pdk], [("Yt", bi)])
        dma("sp", ys_s[blk * BR:(blk + 1) * BR, :].rearrange("(s p) d -> p s d", p=128), Yt[bi][:],
            [("Yt", bi)], ["ys_s"], y_tr[bi])

    P.barrier()
    A32.reset()
    A16.reset()
    junk = ar("junk2", [128, D])
    wtsT = sb("wtsT", [NE, 128])
    Gt = [[ar("G%d_%d" % (i, k4), [128, D]) for k4 in range(TOPK)] for i in range(2)]
    g_tr = [[P.dma_track("g%d_%d" % (i, k4)) for k4 in range(TOPK)] for i in range(2)]
    acc = [ar("acc%d" % i, [128, D]) for i in range(2)]
    x1t = [ar("x1t%d" % i, [128, D]) for i in range(2)]
    x1_tr = [P.dma_track("x1t%d" % i) for i in range(2)]
    ot_tr = [P.dma_track("ot%d" % i) for i in range(2)]
    for i in range(NSUBS):
        xi = i % 2
        r0 = i * 128
        b = r0 // SEQ
        dma("sp", x1t[xi][:], x1_s[r0:r0 + 128, :], ["x1_s"], [("x1t", xi)], x1_tr[xi])
        for k4 in range(TOPK):
            P.op("pool", lambda e, xi=xi, i=i, k4=k4: e.indirect_dma_start(
                out=Gt[xi][k4][:, :], out_offset=None, in_=ys_s[:, :],
                in_offset=bass.IndirectOffsetOnAxis(ap=d4i[:, i, k4:k4 + 1], axis=0)),
                ["ys_s", "d4i"], [("G", xi, k4)], track=g_tr[xi][k4])
        pw, pwk = next_ps()
        tr(pw[:NE, :128], wts[:, i, :], ident32[:], ["wts", "ident32"], [pwk])
        cp("dve", wtsT[:, :], pw[:NE, :128], [pwk], ["wtsT"])
        for cb in range(NCB):
            pb, pbk = next_ps()
            mm(pb[:, :CB], wtsT[:, :], bdn[:, cb * CB:(cb + 1) * CB], True, True, ["wtsT", "c_bdn"], [pbk])
            cp("act", acc[xi][:, cb * CB:(cb + 1) * CB], pb[:, :CB], [pbk], [("acc", xi)])
        for k4 in range(TOPK):
            stt(acc[xi][:], Gt[xi][k4][:], w4[:, i, k4:k4 + 1], acc[xi][:], ALU.mult, ALU.add,
                [("G", xi, k4), "w4", ("acc", xi)], [("acc", xi)])
        tt("pool", acc[xi][:], acc[xi][:], gtb[:, 1, b, :], ALU.mult, [("acc", xi), "gtb"], [("acc", xi)])
        tt("dve", x1t[xi][:], x1t[xi][:], acc[xi][:], ALU.add, [("x1t", xi), ("acc", xi)], [("x1t", xi)])
        act(junk[:], x1t[xi][:], AF.Square, [("x1t", xi)], ["junk"], accum_out=ssq[:, 0:1])
        P.last_write[("ssq", 0)] = P.last_write["junk"]
        P.readers[("ssq", 0)] = []
        act(rstd[:, 0:1], ssq[:, 0:1], AF.Sqrt, [("ssq", 0)], [("rstd", 0)], scale=1.0 / D, bias=EPS)
        P.op("dve", lambda e: e.reciprocal(rstd[:, 0:1], rstd[:, 0:1]), [("rstd", 0)], [("rstd", 0)])
        stt(x1t[xi][:], x1t[xi][:], rstd[:, 0:1], fgb[:], ALU.mult, ALU.mult,
            [("x1t", xi), ("rstd", 0), "c_fgb"], [("x1t", xi)])
        dma("sp", out_d[r0:r0 + 128, :], x1t[xi][:], [("x1t", xi)], ["out"], ot_tr[xi])
    P.wait_all("sp", ot_tr)
    print("[build] sbuf bytes remaining/partition:", nc.sbuf_bytes_remaining, "A32 hi", A32.hi * 4, "A16 hi", A16.hi * 2,
          "n_ops", sum(len(v) for v in P.issue.values()), flush=True)
    P.emit(st)
    st.close()
    return nc


def _layout(inputs, cfg):
    D, DE, NE, SEQ, NSEQ, NCORES = cfg["D"], cfg["DE"], cfg["NE"], cfg["SEQ"], cfg["NSEQ"], cfg["NCORES"]
    KC, CE = D // 128, DE // 128
    f = lambda a: np.ascontiguousarray(np.asarray(a, dtype=np.float32))
    g = {k: np.asarray(v) for k, v in inputs.items()}

    def fm(v):
        return f(v.reshape(-1, 128).T)

    def km(w):
        return f(w.reshape(-1, 128, w.shape[-1]).transpose(1, 0, 2))

    def bc(v):
        return f(np.broadcast_to(v[None, :], (128, v.shape[0])))
    shared = {}
    shared["ada_w"] = km(g["ada_w"][0])
    ada_b = g["ada_b"][0]
    shared["ada_bT"] = fm(ada_b)
    shared["ada_bg"] = f(np.stack([np.broadcast_to(ada_b[2 * D:3 * D], (128, D)),
                                   np.broadcast_to(ada_b[5 * D:6 * D], (128, D))], axis=1))
    shared["g1T"] = fm(g["norm1_g"][0])
    shared["g2T"] = fm(g["norm2_g"][0])
    shared["w_in"] = km(g["w_in"][0])
    shared["conv_wT"] = f(g["conv_w"][0].reshape(4, KC, 128).transpose(2, 1, 0))
    shared["conv_bT"] = fm(g["conv_b"][0])
    shared["lru_wa"] = f(g["lru_wa"][0].reshape(KC, 2, 64, 64))
    shared["lru_wx"] = f(g["lru_wx"][0].reshape(KC, 2, 64, 64))
    shared["lru_baT"] = fm(g["lru_ba"][0])
    shared["lru_bxT"] = fm(g["lru_bx"][0])
    shared["lamT"] = fm(g["lru_lam"][0])
    shared["ln_g_b"] = bc(g["sg_ln_g"][0])
    shared["ln_b_b"] = bc(g["sg_ln_b"][0])
    shared["sg_wsT"] = f(g["sg_ws"][0].transpose(2, 0, 1))
    shared["sg_bs_b"] = f(np.broadcast_to(g["sg_bs"][0][None], (128, KC, 128)))
    shared["w_br"] = f(np.stack([km(g["w_br_rnn"][0]), km(g["w_br_sg"][0]), km(g["w_out"][0])]))
    shared["w_router"] = km(g["w_router"][0])
    shared["b_router_b"] = bc(g["b_router"][0])
    shared["w_gu"] = f(g["w_gu"][0].reshape(NE, KC, 128, 2 * DE).transpose(0, 2, 1, 3))
    shared["b_guT"] = f(g["b_gu"][0].reshape(NE, 2 * CE, 128).transpose(0, 2, 1))
    shared["w_down"] = f(g["w_down"][0].reshape(NE, CE, 128, D).transpose(0, 2, 1, 3))
    shared["b_down"] = f(g["b_down"][0])
    shared["final_g_b"] = bc(g["final_g"])
    x = g["x"].reshape(NCORES, NSEQ * SEQ, D)
    c = g["c"].reshape(NCORES, NSEQ, KC, 128)
    maps = []
    for i in range(NCORES):
        m = dict(shared)
        m["x"] = f(x[i])
        m["cT"] = f(c[i].transpose(2, 1, 0))
        maps.append(m)
    return maps


_NC_CACHE = {}


def run(inputs, cfg):
    key = tuple(sorted(cfg.items()))
    if key not in _NC_CACHE:
        _NC_CACHE[key] = build_nc(cfg)
    nc = _NC_CACHE[key]
    maps = _layout(inputs, cfg)
    res = run_bass_kernel_spmd(nc, maps, core_ids=list(range(cfg["NCORES"])))
    if cfg.get("DEBUG"):
        global DBG
        DBG = res.results
    out = np.stack([r["out"] for r in res.results], axis=0)
    B = cfg["NCORES"] * cfg["NSEQ"]
    return out.reshape(B, cfg["SEQ"], cfg["D"]).astype(np.float32)


def kernel(**inputs):
    return run(inputs, CFG_FULL)
```

```python
from contextlib import ExitStack
import numpy as np
import concourse.bass as bass
import concourse.mybir as mybir
from concourse.bass_utils import run_bass_kernel_spmd

F32 = mybir.dt.float32
BF16 = mybir.dt.bfloat16
I32 = mybir.dt.int32
AF = mybir.ActivationFunctionType
ALU = mybir.AluOpType
ENGS = ("pe", "act", "dve", "pool", "sp")

CFG_FULL = dict(D=1024, DE=1024, NE=32, SEQ=4096, NSEQ=2, NCORES=8)
EPS = 1e-6
LIMIT = 7.0
ALPHA = 1.702
TOPK = 4
USE_GELU_TANH_LUT = False
BR = 256
USE_COND_SKIP = True


class Prog:
    def __init__(self, nc):
        self.nc = nc
        self.tracks = {}
        self.issue = {e: [] for e in ENGS}
        self.last_write = {}
        self.readers = {}
        self.waited = {e: {} for e in ENGS}
        self.n_dma_tracks = 0

    def dma_track(self, name=""):
        self.n_dma_tracks += 1
        return "dma:%d:%s" % (self.n_dma_tracks, name)

    def op(self, eng, fn, reads=(), writes=(), track=None):
        track = track or eng
        tl = self.tracks.setdefault(track, [])
        idx = len(tl)
        deps = {}

        def add(d):
            t, i = d
            if t == track and t.startswith("dma:"):
                return
            if deps.get(t, -1) < i:
                deps[t] = i
        for k in reads:
            lw = self.last_write.get(k)
            if lw is not None:
                add(lw)
        for k in writes:
            lw = self.last_write.get(k)
            if lw is not None and lw[0] != track:
                add(lw)
            for r in self.readers.get(k, ()):
                if r[0] != track:
                    add(r)
        waits = []
        wd = self.waited[eng]
        for t, i in deps.items():
            if t.startswith("dma:"):
                i = len(self.tracks[t]) - 1
            if wd.get(t, -1) >= i:
                continue
            if t == eng and eng == "pe":
                continue
            wd[t] = i
            self.tracks[t][i]["need"] = True
            waits.append((t, i))
        rec = dict(eng=eng, track=track, fn=fn, waits=waits, need=track.startswith("dma:"))
        tl.append(rec)
        self.issue[eng].append(rec)
        for k in reads:
            self.readers.setdefault(k, []).append((track, idx))
        for k in writes:
            self.last_write[k] = (track, idx)
            self.readers[k] = []
        return rec

    def wait_all(self, eng, tracks):
        waits = []
        for t in tracks:
            tl = self.tracks.get(t)
            if not tl:
                continue
            i = len(tl) - 1
            tl[i]["need"] = True
            waits.append((t, i))
        self.issue[eng].append(dict(eng=eng, track=eng, fn=None, waits=waits, need=False))

    def barrier(self):
        tr = list(self.tracks.keys())
        for e in ENGS:
            self.wait_all(e, tr)
            for t in tr:
                self.waited[e][t] = len(self.tracks[t]) - 1

    def emit(self, stack):
        nc = self.nc
        sems, cum = {}, {}
        for n, (t, tl) in enumerate(self.tracks.items()):
            sems[t] = stack.enter_context(nc.semaphore("s%d" % n))
            c, step, arr = 0, (16 if t.startswith("dma:") else 1), []
            for rec in tl:
                if rec["need"]:
                    c += step
                arr.append(c)
            cum[t] = arr
        block = stack.enter_context(nc.Block())
        engobj = {"pe": "tensor", "act": "scalar", "dve": "vector", "pool": "gpsimd", "sp": "sync"}

        def make(engname):
            recs = self.issue[engname]

            def body(e):
                for rec in recs:
                    for (t, i) in rec["waits"]:
                        e.wait_ge(sems[t], cum[t][i])
                    if rec["fn"] is None:
                        continue
                    ins = rec["fn"](e)
                    if rec["need"]:
                        t = rec["track"]
                        ins.then_inc(sems[t], 16 if t.startswith("dma:") else 1)
            return body
        for engname in ENGS:
            if self.issue[engname]:
                getattr(block, engobj[engname])(make(engname))


class Arena:
    def __init__(self, nc, st, name, dt, nelem):
        self.t = st.enter_context(nc.sbuf_tensor(name, [128, nelem], dt))
        self.n, self.off, self.hi = nelem, 0, 0

    def reset(self):
        self.off = 0

    def alloc(self, shape):
        n = 1
        for d in shape[1:]:
            n *= d
        o = self.off
        self.off += n
        self.hi = max(self.hi, self.off)
        assert self.off <= self.n, ("arena overflow", self.off, self.n)
        ap = self.t[:shape[0], o:o + n]
        if len(shape) == 3:
            ap = ap.rearrange("p (a b) -> p a b", a=shape[1])
        elif len(shape) == 4:
            ap = ap.rearrange("p (a b c) -> p a b c", a=shape[1], b=shape[2])
        return ap


def build_nc(cfg):
    D, DE, NE, SEQ, NSEQ = cfg["D"], cfg["DE"], cfg["NE"], cfg["SEQ"], cfg["NSEQ"]
    KC, CE = D // 128, DE // 128
    NTOK = NSEQ * SEQ
    T1 = 256
    S1 = T1 // 128
    NT1 = SEQ // T1
    T2 = 512
    G2 = min(1024, NTOK)
    NG = NTOK // G2
    CB = min(512, D)
    NCB = D // CB
    D6 = 6 * D
    NSUBS = NTOK // 128
    KCM = max(KC, CE)
    PW = 256

    nc = bass.Bass("TRN2", target_bir_lowering=False)
    st = ExitStack()
    P = Prog(nc)

    def din(name, shape, dt=F32):
        return nc.dram_tensor(name, list(shape), dt, kind="ExternalInput").ap()

    def dscr(name, shape, dt):
        return nc.dram_tensor(name, list(shape), dt, kind="Internal").ap()

    x_d = din("x", [NTOK, D])
    cT_d = din("cT", [128, KC, NSEQ])
    adaw_d = din("ada_w", [128, KC, D6])
    adabT_d = din("ada_bT", [128, 6 * KC])
    adabg_d = din("ada_bg", [128, 2, D])
    g1T_d = din("g1T", [128, KC])
    g2T_d = din("g2T", [128, KC])
    win_d = din("w_in", [128, KC, D6])
    convwT_d = din("conv_wT", [128, KC, 4])
    convbT_d = din("conv_bT", [128, KC])
    lruwa_d = din("lru_wa", [KC, 2, 64, 64])
    lruwx_d = din("lru_wx", [KC, 2, 64, 64])
    baT_d = din("lru_baT", [128, KC])
    bxT_d = din("lru_bxT", [128, KC])
    lamT_d = din("lamT", [128, KC])
    lng_d = din("ln_g_b", [128, D])
    lnb_d = din("ln_b_b", [128, D])
    wsT_d = din("sg_wsT", [128, KC, 128])
    bsb_d = din("sg_bs_b", [128, KC, 128])
    wbr_d = din("w_br", [3, 128, KC, D])
    wr_d = din("w_router", [128, KC, NE])
    brb_d = din("b_router_b", [128, NE])
    wgu_d = din("w_gu", [NE, 128, KC, 2 * DE])
    bguT_d = din("b_guT", [NE, 128, 2 * CE])
    wdn_d = din("w_down", [NE, 128, CE, D])
    bdn_d = din("b_down", [NE, D])
    fgb_d = din("final_g_b", [128, D])
    out_d = nc.dram_tensor("out", [NTOK, D], F32, kind="ExternalOutput").ap()

    wmix_s = dscr("wmix_s", [9, 128, KC, D], BF16)
    QW = min(512, 2 * DE)
    NQ = (2 * DE) // QW
    ND = NCB
    NBLK = (NTOK * TOPK) // BR + NE
    SB = BR // 128
    wq_s = [dscr("wq_s%d" % q, [NE, 128, KC, QW], BF16) for q in range(NQ)]
    wd_s = [dscr("wd_s%d" % d_, [NE, 128, CE, CB], BF16) for d_ in range(ND)]
    x1_s = dscr("x1_s", [NTOK, D], F32)
    h2tm_s = dscr("h2tm_s", [NTOK, D], BF16)
    xs_s = dscr("xs_s", [NBLK * BR, D], BF16)
    ys_s = dscr("ys_s", [NBLK * BR, D], F32)

    def sb(name, shape, dt=F32):
        return st.enter_context(nc.sbuf_tensor(name, list(shape), dt))

    A32 = Arena(nc, st, "arena32", F32, cfg.get("A32", 15104))
    A16 = Arena(nc, st, "arena16", BF16, cfg.get("A16", 40960))

    def ar(name, shape, dt=F32):
        return (A32 if dt == F32 else A16).alloc(list(shape))

    NPS = 5
    ps = [st.enter_context(nc.psum_tensor("ps%d" % i, [128, 512], F32)) for i in range(NPS)]
    pst = [st.enter_context(nc.psum_tensor("pst%d" % i, [128, 512], BF16)) for i in range(2)]
    psm = st.enter_context(nc.psum_tensor("psm", [128, 512], F32))
    psmk = "psm"
    psc = [0]
    pstc = [0]

    def next_ps():
        i = psc[0] % NPS
        psc[0] += 1
        return ps[i], ("ps", i)

    def next_pst():
        i = pstc[0] % 2
        pstc[0] += 1
        return pst[i], ("pst", i)

    def mm(out, lhsT, rhs, start, stop, reads, writes):
        P.op("pe", lambda e: e.matmul(out, lhsT, rhs, start=start, stop=stop), reads, writes)

    def tr(out, in_, ident, reads, writes):
        P.op("pe", lambda e: e.transpose(out, in_, ident), reads, writes)

    def act(out, in_, func, reads, writes, bias=None, scale=None, accum_out=None, eng="act"):
        kw = {}
        if bias is not None:
            kw["bias"] = bias
        if scale is not None:
            kw["scale"] = scale
        if accum_out is not None:
            kw["accum_out"] = accum_out
        P.op("act", lambda e: e.activation(out, in_, func, **kw), reads, writes)

    def ts(eng, out, in0, s1, s2, op0, op1, reads, writes):
        if op1 is None:
            P.op(eng, lambda e: e.tensor_scalar(out, in0, s1, None, op0), reads, writes)
        else:
            P.op(eng, lambda e: e.tensor_scalar(out, in0, s1, s2, op0, op1), reads, writes)

    def tt(eng, out, in0, in1, op, reads, writes):
        P.op(eng, lambda e: e.tensor_tensor(out, in0, in1, op), reads, writes)

    def stt(out, in0, scalar, in1, op0, op1, reads, writes):
        P.op("dve", lambda e: e.scalar_tensor_tensor(out, in0, scalar, in1, op0, op1), reads, writes)

    def cp(eng, out, in_, reads, writes):
        if eng == "act":
            P.op("act", lambda e: e.copy(out, in_), reads, writes)
        else:
            P.op(eng, lambda e: e.tensor_copy(out, in_), reads, writes)

    def dma(q, out, in_, reads, writes, track):
        P.op(q, lambda e: e.dma_start(out=out, in_=in_), reads, writes, track=track)

    ctrack = P.dma_track("const")
    consts = {}

    def cload(name, dram, shape, dt=F32, q="sp", arena=False):
        t = ar("c_" + name, shape, dt) if arena else sb("c_" + name, shape, dt)
        dma(q, t[:], dram, [], ["c_" + name], ctrack)
        consts[name] = t
        return t

    cT = cload("cT", cT_d, [128, KC, NSEQ])
    adabT = cload("adabT", adabT_d, [128, 6 * KC])
    adabg = cload("adabg", adabg_d, [128, 2, D], arena=True)
    g1T = cload("g1T", g1T_d, [128, KC])
    g2T = cload("g2T", g2T_d, [128, KC])
    convwT = cload("convwT", convwT_d, [128, KC, 4])
    convbT = cload("convbT", convbT_d, [128, KC])
    baT = cload("baT", baT_d, [128, KC])
    bxT = cload("bxT", bxT_d, [128, KC])
    lamT = cload("lamT", lamT_d, [128, KC])
    lng = cload("lng", lng_d, [128, D])
    lnb = cload("lnb", lnb_d, [128, D])
    wsT32 = cload("wsT32", wsT_d, [128, KC, 128], arena=True)
    bsb = cload("bsb", bsb_d, [128, KC, 128])
    wr32 = cload("wr32", wr_d, [128, KC, NE], arena=True)
    brb = cload("brb", brb_d, [128, NE])
    bdn = cload("bdn", bdn_d, [NE, D])
    fgb = cload("fgb", fgb_d, [128, D])
    wbd32 = ar("wbd32", [128, 2, KC, 128])
    P.op("pool", lambda e: e.memset(wbd32[:], 0.0), [], ["c_wbd32"])
    for gi, src in enumerate((lruwa_d, lruwx_d)):
        for half in range(2):
            dma("sp", wbd32[half * 64:(half + 1) * 64, gi, :, half * 64:(half + 1) * 64],
                src[:, half].rearrange("k i j -> i k j"), [], ["c_wbd32"], ctrack)

    ident_bf = sb("ident_bf", [128, 128], BF16)
    ident32 = sb("ident32", [128, 128], F32)
    ones32 = ar("ones32", [128, 128], F32)
    P.op("pool", lambda e: e.memset(ones32[:], 1.0), [], ["ones32"])
    P.op("pool", lambda e: e.affine_select(out=ident32[:], in_=ones32[:], pattern=[[-1, 128]],
                                           compare_op=ALU.is_equal, fill=0.0, base=0, channel_multiplier=1),
         ["ones32"], ["ident32"])
    cp("dve", ident_bf[:], ident32[:], ["ident32"], ["ident_bf"])

    wbd = sb("wbd", [128, 2, KC, 128], BF16)
    cp("dve", wbd[:], wbd32[:], ["c_wbd32"], ["wbd"])
    wsT = sb("wsT", [128, KC, 128], BF16)
    wsTm = ar("wsTm", [128, KC, 128], F32)
    P.op("pool", lambda e: e.affine_select(out=wsTm[:], in_=wsT32[:], pattern=[[0, KC], [1, 128]],
                                           compare_op=ALU.is_ge, fill=0.0, base=0, channel_multiplier=-1),
         ["c_wsT32"], ["wsTm"])
    cp("dve", wsT[:], wsTm[:], ["wsTm"], ["wsT"])
    wr = sb("wr", [128, KC, NE], BF16)
    cp("dve", wr[:], wr32[:], ["c_wr32"], ["wr"])

    kneg = sb("kneg", [128, KC])
    k2 = sb("k2", [128, KC])
    ktmp = sb("ktmp", [128, KC])
    act(ktmp[:], lamT[:], AF.Exp, ["c_lamT"], ["ktmp"], scale=-1.0)
    act(ktmp[:], ktmp[:], AF.Ln, ["ktmp"], ["ktmp"], bias=1.0)
    ts("dve", kneg[:], ktmp[:], -8.0, None, ALU.mult, None, ["ktmp"], ["kneg"])
    ts("dve", k2[:], ktmp[:], -16.0, None, ALU.mult, None, ["ktmp"], ["k2"])

    sc = sb("sc", [128, KC, NSEQ])
    sgt = sb("sgt", [128, KC, NSEQ])
    act(sgt[:], cT[:], AF.Sigmoid, ["c_cT"], ["sgt"])
    tt("dve", sc[:], sgt[:], cT[:], ALU.mult, ["sgt", "c_cT"], ["sc"])
    screp = ar("screp", [128, NSEQ, KC, 128])
    for b in range(NSEQ):
        for k in range(KC):
            cp("dve", screp[:, b, k, :], sc[:, k, b:b + 1].to_broadcast([128, 128]), ["sc"], ["screp"])
    modT = sb("modT", [128, NSEQ, 6 * KC])
    gtb = sb("gtb", [128, 2, NSEQ, D])
    stg = [ar("stg%d" % i, [128, KCM, PW]) for i in range(2)]
    stg_tr = [P.dma_track("stg%d" % i) for i in range(2)]
    stgc = [0]

    def stage_load(dram_ap, q="sp"):
        i = stgc[0] % 2
        stgc[0] += 1
        dma(q, stg[i][:, :dram_ap.shape[1], :dram_ap.shape[2]], dram_ap, [], [("stg", i)], stg_tr[i])
        return stg[i], ("stg", i)

    NJB = D6 // PW
    for jb in range(NJB):
        s_t, s_k = stage_load(adaw_d[:, :, jb * PW:(jb + 1) * PW])
        for jj in range(PW // 128):
            j = jb * (PW // 128) + jj
            for b in range(NSEQ):
                for k in range(KC):
                    col = b * 6 * KC + j
                    mm(psm[:, col:col + 1], s_t[:, k, jj * 128:(jj + 1) * 128], sc[:, k, b:b + 1],
                       k == 0, k == KC - 1, [s_k, "sc"], [psmk])
        m = (jb * PW) // D
        if m in (2, 5):
            which = 0 if m == 2 else 1
            c0 = (jb * PW) % D
            for b in range(NSEQ):
                pg, pgk = next_ps()
                for k in range(KC):
                    mm(pg[:, :PW], screp[:, b, k, :], s_t[:, k, :], k == 0, k == KC - 1, [s_k, "screp"], [pgk])
                tt("dve", gtb[:, which, b, c0:c0 + PW], pg[:, :PW], adabg[:, which, c0:c0 + PW], ALU.add,
                   [pgk, "c_adabg"], ["gtb"])
    for b in range(NSEQ):
        tt("dve", modT[:, b, :], psm[:, b * 6 * KC:(b + 1) * 6 * KC], adabT[:], ALU.add,
           [psmk, "c_adabT"], ["modT"])
    s1 = sb("s1", [128, NSEQ, KC])
    s2 = sb("s2", [128, NSEQ, KC])
    for b in range(NSEQ):
        stt(s1[:, b, :], modT[:, b, KC:2 * KC], 1.0, g1T[:], ALU.add, ALU.mult, ["modT", "c_g1T"], ["s1"])
        stt(s2[:, b, :], modT[:, b, 4 * KC:5 * KC], 1.0, g2T[:], ALU.add, ALU.mult, ["modT", "c_g2T"], ["s2"])

    cbf = [ar("cbf%d" % i, [128, KCM, PW], BF16) for i in range(2)]
    cbf_tr = [P.dma_track("cbf%d" % i) for i in range(2)]
    cbc = [0]

    def precast(src_ap, dst_ap, dst_key):
        kc, w = src_ap.shape[1], src_ap.shape[2]
        s_t, s_k = stage_load(src_ap)
        i = cbc[0] % 2
        cbc[0] += 1
        eng = "act" if i == 0 else "pool"
        cp(eng, cbf[i][:, :kc, :w], s_t[:, :kc, :w], [s_k], [("cbf", i)])
        dma("sp", dst_ap, cbf[i][:, :kc, :w], [("cbf", i)], [dst_key], cbf_tr[i])

    for blk in range(9):
        for c0 in range(0, D, PW):
            w = min(PW, D - c0)
            src = win_d[:, :, blk * D + c0: blk * D + c0 + w] if blk < 6 else wbr_d[blk - 6][:, :, c0:c0 + w]
            precast(src, wmix_s[blk][:, :, c0:c0 + w], ("wmix", blk, c0))
    for e_ in range(NE):
        for c0 in range(0, 2 * DE, PW):
            w = min(PW, 2 * DE - c0)
            precast(wgu_d[e_][:, :, c0:c0 + w], wq_s[c0 // QW][e_][:, :, c0 % QW:c0 % QW + w], ("wq_s", e_, c0))
        for c0 in range(0, D, PW):
            w = min(PW, D - c0)
            precast(wdn_d[e_][:, :, c0:c0 + w], wd_s[c0 // CB][e_][:, :, c0 % CB:c0 % CB + w], ("wd_s", e_, c0))

    P.barrier()
    A32.reset()
    A16.reset()
    wts = sb("wts", [128, NSUBS, NE])
    hist = sb("hist", [128, KC, 3])
    hstate = sb("hstate", [128, KC])
    RING = 3
    wring = [ar("wring%d" % i, [128, KC, D], BF16) for i in range(RING)]
    wring_tr = [P.dma_track("wring%d" % i) for i in range(RING)]
    wrc = [0]

    def wload(blk):
        i = wrc[0] % RING
        wrc[0] += 1
        dma("sp", wring[i][:], wmix_s[blk], [("wmix", blk, c0) for c0 in range(0, D, PW)], [("wring", i)], wring_tr[i])
        return wring[i], ("wring", i)

    X = ar("X", [128, S1, D])
    x_tr = P.dma_track("x")
    xn = ar("xn", [128, S1, D], BF16)
    hT = ar("hT", [128, KC, T1], BF16)
    yrT = ar("yrT", [128, KC, T1], BF16)
    ysT = ar("ysT", [128, KC, T1], BF16)
    mT = ar("mT", [128, KC, T1], BF16)
    tmpR = ar("tmpR", [128, KC, T1])
    junk = ar("junk", [128, D])
    ssq = sb("ssq", [128, 4])
    rstd = sb("rstd", [128, 4])
    NB = 2
    tmp = {n: [ar("t_%s%d" % (n, i), [128, T1 + (4 if n == "rx" else 0)]) for i in range(NB)]
           for n in ("rx", "cv", "r", "i", "a", "m", "u", "hs", "g", "g2", "q")}
    cvb = [ar("cvb%d" % i, [128, T1], BF16) for i in range(NB)]
    vtm = [ar("vtm%d" % i, [128, D]) for i in range(2)]
    vt2 = [ar("vt2%d" % i, [128, D]) for i in range(2)]
    bnst = sb("bnst", [128, 2 * max(1, D // 512), 6])
    mv = sb("mv", [128, 2])
    x1st_tr = P.dma_track("x1st")
    h2st_tr = P.dma_track("h2st")
    h2tm = ar("h2tm", [128, S1, D], BF16)
    h2T_o = ar("h2T_o", [128, KC, T1], BF16)
    lg = sb("lg", [128, NE])
    top8 = sb("top8", [128, 8])
    negmx = sb("negmx", [128, 1])
    msk = sb("msk", [128, NE])
    ex = sb("ex", [128, NE])
    den = sb("den", [128, 1])

    def gelu(eng_alt, out, in_, n, reads, writes, tg, tgk):
        if USE_GELU_TANH_LUT:
            act(out, in_, AF.Gelu_apprx_tanh, reads, writes)
            return
        act(tg, in_, AF.Square, reads, [tgk])
        ts("dve", tg, tg, 0.044715, 1.0, ALU.mult, ALU.add, [tgk], [tgk])
        tt("dve", tg, tg, in_, ALU.mult, [tgk] + list(reads), [tgk])
        act(tg, tg, AF.Sigmoid, [tgk], [tgk], scale=1.5957691216057308)
        tt("dve", out, tg, in_, ALU.mult, [tgk] + list(reads), writes)

    def rmsnorm_to_T(src, src_key, nsub, dstT, dstT_key, scale_ap_fn, bias_ap_fn, sk_reads):
        for s in range(nsub):
            act(junk[:], src[:, s, :], AF.Square, [src_key], ["junk"], accum_out=ssq[:, s:s + 1])
            P.issue
            P.last_write[("ssq", s)] = P.last_write["junk"]
            P.readers[("ssq", s)] = []
        for s in range(nsub):
            act(rstd[:, s:s + 1], ssq[:, s:s + 1], AF.Sqrt, [("ssq", s)], [("rstd", s)], scale=1.0 / D, bias=EPS)
            P.op("dve", lambda e, s=s: e.reciprocal(rstd[:, s:s + 1], rstd[:, s:s + 1]), [("rstd", s)], [("rstd", s)])
            ts("dve", xn[:, s, :], src[:, s, :], rstd[:, s:s + 1], None, ALU.mult, None,
               [src_key, ("rstd", s)], [("xn", s)])
        for k in range(KC):
            pt, ptk = next_pst()
            for s in range(nsub):
                tr(pt[:, s * 128:(s + 1) * 128], xn[:, s, k * 128:(k + 1) * 128], ident_bf[:],
                   [("xn", s), "ident_bf"], [ptk])
            act(dstT[:, k, :nsub * 128], pt[:, :nsub * 128], AF.Identity, [ptk] + sk_reads, [dstT_key],
                scale=scale_ap_fn(k), bias=bias_ap_fn(k))

    for b in range(NSEQ):
        P.op("pool", lambda e: e.memset(hist[:], 0.0), [], ["hist"])
        P.op("pool", lambda e: e.memset(hstate[:], 0.0), [], ["hstate"])
        for j in range(NT1):
            tok0 = b * SEQ + j * T1
            dma("sp", X[:], x_d[tok0:tok0 + T1, :].rearrange("(s p) d -> p s d", p=128), [], ["X"], x_tr)
            rmsnorm_to_T(X, "X", S1, hT, "hT",
                         lambda k: s1[:, b, k:k + 1], lambda k: modT[:, b, k:k + 1], ["s1", "modT"])
            w0, w0k = wload(0)
            w1, w1k = wload(1)
            for c in range(KC):
                i_ = c % NB
                T = {n: tmp[n][i_] for n in tmp}
                K = {n: ("t", n, i_) for n in tmp}
                pz, pzk = next_ps()
                for k in range(KC):
                    mm(pz[:, :T1], w0[:, k, c * 128:(c + 1) * 128], hT[:, k, :], k == 0, k == KC - 1,
                       [w0k, "hT"], [pzk])
                cp("act", T["rx"][:, 0:3], hist[:, c, :], ["hist"], [K["rx"]])
                cp("act", T["rx"][:, 3:3 + T1], pz[:, :T1], [pzk], [K["rx"]])
                cp("act", hist[:, c, :], T["rx"][:, T1:T1 + 3], [K["rx"]], ["hist"])
                ts("dve", T["cv"][:, :], T["rx"][:, 0:T1], convwT[:, c, 0:1], convbT[:, c:c + 1], ALU.mult, ALU.add,
                   [K["rx"], "c_convwT", "c_convbT"], [K["cv"]])
                for kk in range(1, 4):
                    stt(T["cv"][:, :], T["rx"][:, kk:kk + T1], convwT[:, c, kk:kk + 1], T["cv"][:, :],
                        ALU.mult, ALU.add, [K["rx"], K["cv"], "c_convwT"], [K["cv"]])
                cp("pool", cvb[i_][:, :], T["cv"][:, :], [K["cv"]], [("cvb", i_)])
                pr, prk = next_ps()
                mm(pr[:, :T1], wbd[:, 0, c, :], cvb[i_][:, :], True, True, ["wbd", ("cvb", i_)], [prk])
                pi, pik = next_ps()
                mm(pi[:, :T1], wbd[:, 1, c, :], cvb[i_][:, :], True, True, ["wbd", ("cvb", i_)], [pik])
                act(T["r"][:, :], pr[:, :T1], AF.Sigmoid, [prk, "c_baT"], [K["r"]], bias=baT[:, c:c + 1])
                act(T["i"][:, :], pi[:, :T1], AF.Sigmoid, [pik, "c_bxT"], [K["i"]], bias=bxT[:, c:c + 1])
                act(T["a"][:, :], T["r"][:, :], AF.Exp, [K["r"], "kneg"], [K["a"]], scale=kneg[:, c:c + 1])
                act(T["m"][:, :], T["r"][:, :], AF.Exp, [K["r"], "k2"], [K["m"]], scale=k2[:, c:c + 1])
                act(T["m"][:, :], T["m"][:, :], AF.Sqrt, [K["m"]], [K["m"]], scale=-1.0, bias=1.0)
                tt("pool", T["u"][:, :], T["i"][:, :], T["cv"][:, :], ALU.mult, [K["i"], K["cv"]], [K["u"]])
                tt("pool", T["u"][:, :], T["u"][:, :], T["m"][:, :], ALU.mult, [K["u"], K["m"]], [K["u"]])
                P.op("dve", lambda e, T=T, c=c: e.tensor_tensor_scan(T["hs"][:, :], T["a"][:, :], T["u"][:, :],
                                                                       hstate[:, c:c + 1], ALU.mult, ALU.add),
                     [K["a"], K["u"], "hstate"], [K["hs"]])
                cp("pool", hstate[:, c:c + 1], T["hs"][:, T1 - 1:T1], [K["hs"]], ["hstate"])
                pg, pgk = next_ps()
                for k in range(KC):
                    mm(pg[:, :T1], w1[:, k, c * 128:(c + 1) * 128], hT[:, k, :], k == 0, k == KC - 1,
                       [w1k, "hT"], [pgk])
                gelu("dve", T["g"][:, :], pg[:, :T1], T1, [pgk], [K["g"]], T["g2"][:, :], K["g2"])
                tt("dve", yrT[:, c, :], T["hs"][:, :], T["g"][:, :], ALU.mult, [K["hs"], K["g"]], ["yrT"])
            w3, w3k = wload(3)
            for s in range(S1):
                vi = s % 2
                for cb in range(NCB):
                    pv, pvk = next_ps()
                    for k in range(KC):
                        mm(pv[:, :CB], hT[:, k, s * 128:(s + 1) * 128], w3[:, k, cb * CB:(cb + 1) * CB],
                           k == 0, k == KC - 1, [w3k, "hT"], [pvk])
                    gelu("dve", vtm[vi][:, cb * CB:(cb + 1) * CB], pv[:, :CB], CB, [pvk], [("vtm", vi)],
                         vt2[vi][:, cb * CB:(cb + 1) * CB], ("vt2", vi))
                nchunk = max(1, D // 512)
                cw = D // nchunk
                for q in range(nchunk):
                    P.op("dve", lambda e, vi=vi, q=q: e.bn_stats(bnst[:, q, :], vtm[vi][:, q * cw:(q + 1) * cw]),
                         [("vtm", vi)], ["bnst"])
                P.op("dve", lambda e: e.bn_aggr(mv[:], bnst[:, :nchunk, :].rearrange("p a b -> p (a b)")),
                     ["bnst"], ["mv"])
                act(mv[:, 1:2], mv[:, 1:2], AF.Sqrt, ["mv"], ["mv"], bias=EPS)
                P.op("dve", lambda e: e.reciprocal(mv[:, 1:2], mv[:, 1:2]), ["mv"], ["mv"])
                ts("dve", vtm[vi][:, :], vtm[vi][:, :], mv[:, 0:1], mv[:, 1:2], ALU.subtract, ALU.mult,
                   [("vtm", vi), "mv"], [("vtm", vi)])
                tt("pool", vtm[vi][:, :], vtm[vi][:, :], lng[:], ALU.mult, [("vtm", vi), "c_lng"], [("vtm", vi)])
                tt("pool", xn[:, s, :], vtm[vi][:, :], lnb[:], ALU.add, [("vtm", vi), "c_lnb"], [("xn", s)])
            w2, w2k = wload(2)
            for g in range(KC):
                i_ = g % NB
                T = {n: tmp[n][i_] for n in tmp}
                K = {n: ("t", n, i_) for n in tmp}
                pu, puk = next_ps()
                for k in range(KC):
                    mm(pu[:, :T1], w2[:, k, g * 128:(g + 1) * 128], hT[:, k, :], k == 0, k == KC - 1,
                       [w2k, "hT"], [puk])
                gelu("dve", T["g"][:, :], pu[:, :T1], T1, [puk], [K["g"]], T["g2"][:, :], K["g2"])
                psv, psvk = next_ps()
                for s in range(S1):
                    mm(psv[:, s * 128:(s + 1) * 128], xn[:, s, g * 128:(g + 1) * 128], wsT[:, g, :], True, True,
                       [("xn", s), "wsT"], [psvk])
                for s in range(S1):
                    tt("dve", T["q"][:, s * 128:(s + 1) * 128], psv[:, s * 128:(s + 1) * 128], bsb[:, g, :], ALU.add,
                       [psvk, "c_bsb"], [K["q"]])
                tt("pool", ysT[:, g, :], T["q"][:, :], T["g"][:, :], ALU.mult, [K["q"], K["g"]], ["ysT"])
            for pas, (ga, gb_, yT, yk) in enumerate(((4, 6, yrT, "yrT"), (5, 7, ysT, "ysT"))):
                wg, wgk = wload(ga)
                wb, wbk = wload(gb_)
                for oc in range(KC):
                    i_ = oc % NB
                    T = {n: tmp[n][i_] for n in tmp}
                    K = {n: ("t", n, i_) for n in tmp}
                    pgt, pgtk = next_ps()
                    for k in range(KC):
                        mm(pgt[:, :T1], wg[:, k, oc * 128:(oc + 1) * 128], hT[:, k, :], k == 0, k == KC - 1,
                           [wgk, "hT"], [pgtk])
                    act(T["g"][:, :], pgt[:, :T1], AF.Sigmoid, [pgtk], [K["g"]])
                    pbr, pbrk = next_ps()
                    for k in range(KC):
                        mm(pbr[:, :T1], wb[:, k, oc * 128:(oc + 1) * 128], yT[:, k, :], k == 0, k == KC - 1,
                           [wbk, yk], [pbrk])
                    if pas == 0:
                        tt("dve", tmpR[:, oc, :], T["g"][:, :], pbr[:, :T1], ALU.mult, [K["g"], pbrk], [("tmpR", oc)])
                    else:
                        tt("dve", T["i"][:, :], T["g"][:, :], pbr[:, :T1], ALU.mult, [K["g"], pbrk], [K["i"]])
                        tt("pool", mT[:, oc, :], tmpR[:, oc, :], T["i"][:, :], ALU.add, [("tmpR", oc), K["i"]], ["mT"])
            w8, w8k = wload(8)
            for s in range(S1):
                for cb in range(NCB):
                    po, pok = next_ps()
                    for k in range(KC):
                        mm(po[:, :CB], mT[:, k, s * 128:(s + 1) * 128], w8[:, k, cb * CB:(cb + 1) * CB],
                           k == 0, k == KC - 1, [w8k, "mT"], [pok])
                    tt("dve", junk[:, cb * CB:(cb + 1) * CB], po[:, :CB], gtb[:, 0, b, cb * CB:(cb + 1) * CB], ALU.mult,
                       [pok, "gtb"], ["junk"])
                    tt("pool", X[:, s, cb * CB:(cb + 1) * CB], X[:, s, cb * CB:(cb + 1) * CB],
                       junk[:, cb * CB:(cb + 1) * CB], ALU.add, ["X", "junk"], ["X"])
            dma("sp", x1_s[tok0:tok0 + T1, :].rearrange("(s p) d -> p s d", p=128), X[:], ["X"], ["x1_s"], x1st_tr)
            rmsnorm_to_T(X, "X", S1, h2T_o, "h2T_o",
                         lambda k: s2[:, b, k:k + 1], lambda k: modT[:, b, 3 * KC + k:3 * KC + k + 1], ["s2", "modT"])
            for s in range(S1):
                for k0 in range(0, KC, 4):
                    pt, ptk = next_pst()
                    nk = min(4, KC - k0)
                    for kk in range(nk):
                        tr(pt[:, kk * 128:(kk + 1) * 128], h2T_o[:, k0 + kk, s * 128:(s + 1) * 128], ident_bf[:],
                           ["h2T_o", "ident_bf"], [ptk])
                    cp("act", h2tm[:, s, k0 * 128:(k0 + nk) * 128], pt[:, :nk * 128], [ptk], ["h2tm"])
            dma("sp", h2tm_s[tok0:tok0 + T1, :].rearrange("(s p) d -> p s d", p=128), h2tm[:], ["h2tm"], ["h2tm_s"], h2st_tr)
            for s in range(S1):
                sub = (tok0 // 128) + s
                pl, plk = next_ps()
                for k in range(KC):
                    mm(pl[:, :NE], h2T_o[:, k, s * 128:(s + 1) * 128], wr[:, k, :], k == 0, k == KC - 1,
                       ["h2T_o", "wr"], [plk])
                tt("dve", lg[:], pl[:, :NE], brb[:], ALU.add, [plk, "c_brb"], ["lg"])
                P.op("dve", lambda e: e.max(top8[:], lg[:]), ["lg"], ["top8"])
                ts("dve", negmx[:], top8[:, 0:1], -1.0, None, ALU.mult, None, ["top8"], ["negmx"])
                ts("dve", msk[:], lg[:], top8[:, TOPK - 1:TOPK], None, ALU.is_ge, None, ["lg", "top8"], ["msk"])
                act(ex[:], lg[:], AF.Exp, ["lg", "negmx"], ["ex"], bias=negmx[:, 0:1])
                tt("dve", ex[:], ex[:], msk[:], ALU.mult, ["ex", "msk"], ["ex"])
                P.op("dve", lambda e: e.reduce_sum(den[:], ex[:], axis=mybir.AxisListType.X), ["ex"], ["den"])
                P.op("dve", lambda e: e.reciprocal(den[:], den[:]), ["den"], ["den"])
                ts("dve", wts[:, sub, :], ex[:], den[:, 0:1], None, ALU.mult, None, ["ex", "den"], ["wts"])

    P.barrier()
    A32.reset()
    A16.reset()
    LOGB = BR.bit_length() - 1
    d4f = sb("d4f", [128, NSUBS, 8])
    d4i = sb("d4i", [128, NSUBS, 4], I32)
    w4 = sb("w4", [128, NSUBS, 4])
    be_i = sb("be_i", [128, NBLK], I32)
    ld_i = sb("ld_i", [128, NBLK], I32)
    widx = sb("widx", [128, NBLK], I32)
    mask = ar("mask", [128, NSUBS, NE])
    dest = ar("dest", [128, NSUBS, NE])
    dkey = ar("dkey", [128, NSUBS, NE])
    ustr = ar("ustr", [128, 128])
    onesq = ar("onesq", [128, 128])
    macc = ar("macc", [128, NE])
    cnt = ar("cnt", [128, NE])
    padded = ar("padded", [128, NE])
    pend = ar("pend", [128, NE])
    pstart = ar("pstart", [128, NE])
    onesne = ar("onesne", [128, NE])
    eqt = ar("eqt", [128, NE])
    bst_i = A32.alloc([128, NBLK]).bitcast(I32)
    bst = ar("bst", [128, NBLK])
    bef = ar("bef", [128, NBLK])
    bet = ar("bet", [128, NBLK])
    P.op("pool", lambda e: e.memset(onesq[:], 1.0), [], ["onesq"])
    P.op("pool", lambda e: e.memset(onesne[:], 1.0), [], ["onesne"])
    P.op("pool", lambda e: e.memset(macc[:], 0.0), [], ["macc"])
    P.op("pool", lambda e: e.affine_select(out=ustr[:], in_=onesq[:], pattern=[[1, 128]], compare_op=ALU.is_ge,
                                           fill=0.0, base=-1, channel_multiplier=-1), ["onesq"], ["ustr"])
    ts("dve", mask[:], wts[:], 0.0, None, ALU.is_gt, None, ["wts"], ["mask"])
    for i in range(NSUBS):
        prk_t, prk = next_ps()
        mm(prk_t[:, :NE], ustr[:], mask[:, i, :], True, False, ["ustr", "mask"], [prk])
        mm(prk_t[:, :NE], onesq[:], macc[:], False, True, ["onesq", "macc"], [prk])
        cp("act", dest[:, i, :], prk_t[:, :NE], [prk], [("dest", i)])
        tt("dve", macc[:], macc[:], mask[:, i, :], ALU.add, ["macc", "mask"], ["macc"])
    pc_t, pck = next_ps()
    mm(pc_t[:, :NE], onesq[:], macc[:], True, True, ["onesq", "macc"], [pck])
    cp("dve", cnt[:], pc_t[:, :NE], [pck], ["cnt"])
    P.op("pool", lambda e: e.memset(padded[:], 0.0), [], ["padded"])
    for j in range(NTOK // BR):
        stt(padded[:], cnt[:], float(j * BR), padded[:], ALU.is_gt, ALU.add, ["cnt", "padded"], ["padded"])
    ts("dve", padded[:], padded[:], float(BR), None, ALU.mult, None, ["padded"], ["padded"])
    P.op("dve", lambda e: e.tensor_tensor_scan(pend[:], onesne[:], padded[:], 0.0, ALU.mult, ALU.add),
         ["onesne", "padded"], ["pend"])
    tt("dve", pstart[:], pend[:], padded[:], ALU.subtract, ["pend", "padded"], ["pstart"])
    for i in range(NSUBS):
        tt("dve", dest[:, i, :], dest[:, i, :], pstart[:], ALU.add, [("dest", i), "pstart"], [("dest", i)])
    dkeys = [("dest", i) for i in range(NSUBS)]
    stt(dkey[:], dest[:], 1.0, mask[:], ALU.add, ALU.mult, dkeys + ["mask"], ["dkey"])
    ts("dve", dkey[:], dkey[:], -1.0, None, ALU.add, None, ["dkey"], ["dkey"])
    for i in range(NSUBS):
        P.op("dve", lambda e, i=i: e.max(d4f[:, i, :], dkey[:, i, :]), ["dkey"], [("d4f", i)])
        for k4 in range(TOPK):
            ts("dve", eqt[:], dkey[:, i, :], d4f[:, i, k4:k4 + 1], None, ALU.is_equal, None, ["dkey", ("d4f", i)], ["eqt"])
            tt("dve", eqt[:], eqt[:], wts[:, i, :], ALU.mult, ["eqt", "wts"], ["eqt"])
            P.op("dve", lambda e, i=i, k4=k4: e.reduce_sum(w4[:, i, k4:k4 + 1], eqt[:], axis=mybir.AxisListType.X),
                 ["eqt"], ["w4"])
        cp("dve", d4i[:, i, :], d4f[:, i, 0:TOPK], [("d4f", i)], ["d4i"])
    P.op("pool", lambda e: e.iota(bst_i, pattern=[[BR, NBLK]], base=0, channel_multiplier=0), [], ["bst_i"])
    cp("dve", bst[:], bst_i, ["bst_i"], ["bst"])
    P.op("pool", lambda e: e.memset(bef[:], 0.0), [], ["bef"])
    for e_ in range(NE):
        ts("dve", bet[:], bst[:], pend[:, e_:e_ + 1], None, ALU.is_ge, None, ["bst", "pend"], ["bet"])
        tt("dve", bef[:], bef[:], bet[:], ALU.add, ["bef", "bet"], ["bef"])
    ts("dve", bef[:], bef[:], float(NE - 1), None, ALU.min, None, ["bef"], ["bef"])
    cp("dve", be_i[:], bef[:], ["bef"], ["be_i"])
    P.op("pool", lambda e: e.memset(bet[:], 1.0), ["bet"], ["bet"])
    if NBLK > 1:
        tt("dve", bet[:, 1:NBLK], bef[:, 1:NBLK], bef[:, 0:NBLK - 1], ALU.not_equal, ["bef", "bet"], ["bet"])
    cp("dve", ld_i[:], bet[:], ["bet"], ["ld_i"])
    pidx_i = A32.alloc([128, 1]).bitcast(I32)
    pidx = ar("pidx", [128, 1])
    widf = ar("widf", [128, NBLK])
    P.op("pool", lambda e: e.iota(pidx_i, pattern=[[0, 1]], base=0, channel_multiplier=1), [], ["pidx_i"])
    cp("dve", pidx[:], pidx_i, ["pidx_i"], ["pidx"])
    ts("dve", widf[:], bef[:], 128.0, pidx[:, 0:1], ALU.mult, ALU.add, ["bef", "pidx"], ["widf"])
    if USE_COND_SKIP:
        ts("dve", bet[:], bet[:], -1.0, -float(2 ** 30), ALU.add, ALU.mult, ["bet"], ["bet"])
        tt("dve", widf[:], widf[:], bet[:], ALU.add, ["widf", "bet"], ["widf"])
    cp("dve", widx[:], widf[:], ["widf"], ["widx"])

    if cfg.get("DEBUG"):
        dbg_tr = P.dma_track("dbg")
        MX = max(NE, NBLK)
        dd = {"dbg_d4f": (d4f, [128, NSUBS, 8], ["d4i"]), "dbg_w4": (w4, [128, NSUBS, 4], ["w4"]),
              "dbg_wts": (wts, [128, NSUBS, NE], ["wts"]), "dbg_cnt": (cnt, [128, NE], ["cnt"]),
              "dbg_pend": (pend, [128, NE], ["pend"]), "dbg_bef": (bef, [128, NBLK], ["bef"]),
              "dbg_widf": (widf, [128, NBLK], ["widx"]), "dbg_dest": (dest, [128, NSUBS, NE], ["dkey"]),
              "dbg_dkey": (dkey, [128, NSUBS, NE], ["dkey", "d4i"])}
        for nm, (t_, shp, rd) in dd.items():
            o_ = nc.dram_tensor(nm, shp, F32, kind="ExternalOutput").ap()
            dma("sp", o_, t_[:], rd, [nm], dbg_tr)
    P.barrier()
    A32.reset()
    A16.reset()
    zt = ar("zt", [128, 4, D], BF16)
    z_tr = P.dma_track("zfill")
    P.op("pool", lambda e: e.memset(zt[:], 0.0), [], ["zt"])
    for r0 in range(0, NBLK * BR, 512):
        dma("sp", xs_s[r0:r0 + 512, :].rearrange("(s p) d -> p s d", p=128), zt[:], ["zt"], ["xs_s"], z_tr)
    hsc = [ar("hsc%d" % i, [128, D], BF16) for i in range(2)]
    hsc_tr = [P.dma_track("hsc%d" % i) for i in range(2)]
    sc_tr = [P.dma_track("scat%d" % i) for i in range(2)]
    for i in range(NSUBS):
        hi = i % 2
        dma("sp", hsc[hi][:], h2tm_s[i * 128:(i + 1) * 128, :], ["h2tm_s"], [("hsc", hi)], hsc_tr[hi])
        for k4 in range(TOPK):
            P.op("pool", lambda e, hi=hi, i=i, k4=k4: e.indirect_dma_start(
                out=xs_s[:, :], out_offset=bass.IndirectOffsetOnAxis(ap=d4i[:, i, k4:k4 + 1], axis=0),
                in_=hsc[hi][:, :], in_offset=None), [("hsc", hi), "d4i", "xs_s"], [("xs_sc", i, k4)], track=sc_tr[hi])
    P.barrier()
    A32.reset()
    A16.reset()

    wq = [ar("wq%d" % i, [128, KC, QW], BF16) for i in range(NQ)]
    wd = [ar("wd%d" % i, [128, CE, CB], BF16) for i in range(ND)]
    wq_tr = [P.dma_track("wq%d" % i) for i in range(NQ)]
    wd_tr = [P.dma_track("wd%d" % i) for i in range(ND)]
    bg = [sb("bg%d" % i, [128, 2 * CE]) for i in range(2)]
    bg_tr = [P.dma_track("bg%d" % i) for i in range(2)]
    xb = [ar("xb%d" % i, [128, SB, D], BF16) for i in range(2)]
    xb_tr = [P.dma_track("xb%d" % i) for i in range(2)]
    XT = [ar("XT%d" % i, [128, KC, BR], BF16) for i in range(2)]
    actT = [ar("actT%d" % i, [128, CE, BR], BF16) for i in range(2)]
    mt = {n: [ar("m_%s%d" % (n, i), [128, BR]) for i in range(2)] for n in ("g", "s", "u")}
    Yt = [ar("Yt%d" % i, [128, SB, D]) for i in range(2)]
    y_tr = [P.dma_track("yst%d" % i) for i in range(2)]
    POOL_ET = mybir.EngineType.Pool

    def dyn_load(dst_ap, src_rows, blk, reads, writes, track):
        def fn(e):
            kw = {}
            if USE_COND_SKIP:
                if "r" not in breg:
                    breg["r"] = e.to_reg(NE * 128 - 1)
                kw = dict(bounds_check=breg["r"], oob_is_err=False)
            return e.indirect_dma_start(out=dst_ap, out_offset=None, in_=src_rows,
                                        in_offset=bass.IndirectOffsetOnAxis(ap=widx[:, blk:blk + 1], axis=0), **kw)
        P.op("pool", fn, reads, writes, track=track)

    breg = {}
    wq_rows = [wq_s[q].rearrange("e p a b -> (e p) (a b)") for q in range(NQ)]
    wd_rows = [wd_s[d_].rearrange("e p a b -> (e p) (a b)") for d_ in range(ND)]
    bg_rows = bguT_d.rearrange("e p a -> (e p) a")
    def xb_load(blk):
        bi = blk % 2
        dma("sp", xb[bi][:], xs_s[blk * BR:(blk + 1) * BR, :].rearrange("(s p) d -> p s d", p=128),
            [], [("xb", bi)], xb_tr[bi])

    xb_load(0)
    for blk in range(NBLK):
        bi = blk % 2
        for q in range(NQ):
            dyn_load(wq[q].rearrange("p a b -> p (a b)"), wq_rows[q], blk, ["widx", "wq_s"], [("wq", q)], wq_tr[q])
        for d_ in range(ND):
            dyn_load(wd[d_].rearrange("p a b -> p (a b)"), wd_rows[d_], blk, ["widx", "wd_s"], [("wd", d_)], wd_tr[d_])
        dyn_load(bg[0][:], bg_rows, blk, ["widx"], [("bg", 0)], bg_tr[0])
        for k in range(KC):
            pt, ptk = next_pst()
            for s_ in range(SB):
                tr(pt[:, s_ * 128:(s_ + 1) * 128], xb[bi][:, s_, k * 128:(k + 1) * 128], ident_bf[:],
                   [("xb", bi), "ident_bf"], [ptk])
            cp("act" if k % 2 == 0 else "dve", XT[bi][:, k, :], pt[:, :BR], [ptk], [("XT", bi)])
        if blk + 1 < NBLK:
            xb_load(blk + 1)
        aT = actT[bi]
        for c in range(CE):
            mi = c % 2
            G_, S_, U_ = mt["g"][mi], mt["s"][mi], mt["u"][mi]
            gk, sk, uk = ("mg", mi), ("ms", mi), ("mu", mi)
            gcol = c * 128
            ucol = DE + c * 128
            pgm, pgmk = next_ps()
            for k in range(KC):
                mm(pgm[:, :BR], wq[gcol // QW][:, k, gcol % QW:gcol % QW + 128], XT[bi][:, k, :],
                   k == 0, k == KC - 1, [("wq", gcol // QW), ("XT", bi)], [pgmk])
            pum, pumk = next_ps()
            for k in range(KC):
                mm(pum[:, :BR], wq[ucol // QW][:, k, ucol % QW:ucol % QW + 128], XT[bi][:, k, :],
                   k == 0, k == KC - 1, [("wq", ucol // QW), ("XT", bi)], [pumk])
            ts("dve", G_[:, :], pgm[:, :BR], bg[0][:, c:c + 1], LIMIT, ALU.add, ALU.min, [pgmk, ("bg", 0)], [gk])
            act(S_[:, :], G_[:, :], AF.Sigmoid, [gk], [sk], scale=ALPHA)
            ts("dve", U_[:, :], pum[:, :BR], bg[0][:, CE + c:CE + c + 1], LIMIT, ALU.add, ALU.min,
               [pumk, ("bg", 0)], [uk])
            ts("dve", U_[:, :], U_[:, :], -LIMIT, 1.0, ALU.max, ALU.add, [uk], [uk])
            tt("dve", S_[:, :], S_[:, :], G_[:, :], ALU.mult, [sk, gk], [sk])
            tt("dve", aT[:, c, :], U_[:, :], S_[:, :], ALU.mult, [uk, sk], [("actT", bi)])
        for s_ in range(SB):
            for cb in range(NCB):
                pd, pdk = next_ps()
                for k in range(CE):
                    mm(pd[:, :CB], aT[:, k, s_ * 128:(s_ + 1) * 128], wd[cb][:, k, :], k == 0, k == CE - 1,
                       [("actT", bi), ("wd", cb)], [pdk])
                cp("act", Yt[bi][:, s_, cb * CB:(cb + 1) * CB], pd[:, :CB], [pdk], [("Yt", bi)])
        dma("sp", ys_s[blk * BR:(blk + 1) * BR, :].rearrange("(s p) d -> p s d", p=128), Yt[bi][:],
            [("Yt", bi)], [("ys_s", blk)], y_tr[bi])

    P.barrier()
    A32.reset()
    A16.reset()
    junk = ar("junk2", [128, D])
    wtsT = sb("wtsT", [NE, 128])
    Gt = [[ar("G%d_%d" % (i, k4), [128, D]) for k4 in range(TOPK)] for i in range(2)]
    g_tr = [[P.dma_track("g%d_%d" % (i, k4)) for k4 in range(TOPK)] for i in range(2)]
    acc = [ar("acc%d" % i, [128, D]) for i in range(2)]
    x1t = [ar("x1t%d" % i, [128, D]) for i in range(2)]
    x1_tr = [P.dma_track("x1t%d" % i) for i in range(2)]
    ot_tr = [P.dma_track("ot%d" % i) for i in range(2)]
    for i in range(NSUBS):
        xi = i % 2
        r0 = i * 128
        b = r0 // SEQ
        dma("sp", x1t[xi][:], x1_s[r0:r0 + 128, :], ["x1_s"], [("x1t", xi)], x1_tr[xi])
        for k4 in range(TOPK):
            P.op("pool", lambda e, xi=xi, i=i, k4=k4: e.indirect_dma_start(
                out=Gt[xi][k4][:, :], out_offset=None, in_=ys_s[:, :],
                in_offset=bass.IndirectOffsetOnAxis(ap=d4i[:, i, k4:k4 + 1], axis=0)),
                ["ys_s", "d4i"], [("G", xi, k4)], track=g_tr[xi][k4])
        pw, pwk = next_ps()
        tr(pw[:NE, :128], wts[:, i, :], ident32[:], ["wts", "ident32"], [pwk])
        cp("dve", wtsT[:, :], pw[:NE, :128], [pwk], ["wtsT"])
        for cb in range(NCB):
            pb, pbk = next_ps()
            mm(pb[:, :CB], wtsT[:, :], bdn[:, cb * CB:(cb + 1) * CB], True, True, ["wtsT", "c_bdn"], [pbk])
            cp("act", acc[xi][:, cb * CB:(cb + 1) * CB], pb[:, :CB], [pbk], [("acc", xi)])
        for k4 in range(TOPK):
            stt(acc[xi][:], Gt[xi][k4][:], w4[:, i, k4:k4 + 1], acc[xi][:], ALU.mult, ALU.add,
                [("G", xi, k4), "w4", ("acc", xi)], [("acc", xi)])
        tt("pool", acc[xi][:], acc[xi][:], gtb[:, 1, b, :], ALU.mult, [("acc", xi), "gtb"], [("acc", xi)])
        tt("dve", x1t[xi][:], x1t[xi][:], acc[xi][:], ALU.add, [("x1t", xi), ("acc", xi)], [("x1t", xi)])
        act(junk[:], x1t[xi][:], AF.Square, [("x1t", xi)], ["junk"], accum_out=ssq[:, 0:1])
        P.last_write[("ssq", 0)] = P.last_write["junk"]
        P.readers[("ssq", 0)] = []
        act(rstd[:, 0:1], ssq[:, 0:1], AF.Sqrt, [("ssq", 0)], [("rstd", 0)], scale=1.0 / D, bias=EPS)
        P.op("dve", lambda e: e.reciprocal(rstd[:, 0:1], rstd[:, 0:1]), [("rstd", 0)], [("rstd", 0)])
        stt(x1t[xi][:], x1t[xi][:], rstd[:, 0:1], fgb[:], ALU.mult, ALU.mult,
            [("x1t", xi), ("rstd", 0), "c_fgb"], [("x1t", xi)])
        dma("sp", out_d[r0:r0 + 128, :], x1t[xi][:], [("x1t", xi)], [("out", i)], ot_tr[xi])
    P.wait_all("sp", ot_tr)
    print("[build] sbuf bytes remaining/partition:", nc.sbuf_bytes_remaining, "A32 hi", A32.hi * 4, "A16 hi", A16.hi * 2,
          "n_ops", sum(len(v) for v in P.issue.values()), flush=True)
    P.emit(st)
    st.close()
    return nc


def _layout(inputs, cfg):
    D, DE, NE, SEQ, NSEQ, NCORES = cfg["D"], cfg["DE"], cfg["NE"], cfg["SEQ"], cfg["NSEQ"], cfg["NCORES"]
    KC, CE = D // 128, DE // 128
    f = lambda a: np.ascontiguousarray(np.asarray(a, dtype=np.float32))
    g = {k: np.asarray(v) for k, v in inputs.items()}

    def fm(v):
        return f(v.reshape(-1, 128).T)

    def km(w):
        return f(w.reshape(-1, 128, w.shape[-1]).transpose(1, 0, 2))

    def bc(v):
        return f(np.broadcast_to(v[None, :], (128, v.shape[0])))
    shared = {}
    shared["ada_w"] = km(g["ada_w"][0])
    ada_b = g["ada_b"][0]
    shared["ada_bT"] = fm(ada_b)
    shared["ada_bg"] = f(np.stack([np.broadcast_to(ada_b[2 * D:3 * D], (128, D)),
                                   np.broadcast_to(ada_b[5 * D:6 * D], (128, D))], axis=1))
    shared["g1T"] = fm(g["norm1_g"][0])
    shared["g2T"] = fm(g["norm2_g"][0])
    shared["w_in"] = km(g["w_in"][0])
    shared["conv_wT"] = f(g["conv_w"][0].reshape(4, KC, 128).transpose(2, 1, 0))
    shared["conv_bT"] = fm(g["conv_b"][0])
    shared["lru_wa"] = f(g["lru_wa"][0].reshape(KC, 2, 64, 64))
    shared["lru_wx"] = f(g["lru_wx"][0].reshape(KC, 2, 64, 64))
    shared["lru_baT"] = fm(g["lru_ba"][0])
    shared["lru_bxT"] = fm(g["lru_bx"][0])
    shared["lamT"] = fm(g["lru_lam"][0])
    shared["ln_g_b"] = bc(g["sg_ln_g"][0])
    shared["ln_b_b"] = bc(g["sg_ln_b"][0])
    shared["sg_wsT"] = f(g["sg_ws"][0].transpose(2, 0, 1))
    shared["sg_bs_b"] = f(np.broadcast_to(g["sg_bs"][0][None], (128, KC, 128)))
    shared["w_br"] = f(np.stack([km(g["w_br_rnn"][0]), km(g["w_br_sg"][0]), km(g["w_out"][0])]))
    shared["w_router"] = km(g["w_router"][0])
    shared["b_router_b"] = bc(g["b_router"][0])
    shared["w_gu"] = f(g["w_gu"][0].reshape(NE, KC, 128, 2 * DE).transpose(0, 2, 1, 3))
    shared["b_guT"] = f(g["b_gu"][0].reshape(NE, 2 * CE, 128).transpose(0, 2, 1))
    shared["w_down"] = f(g["w_down"][0].reshape(NE, CE, 128, D).transpose(0, 2, 1, 3))
    shared["b_down"] = f(g["b_down"][0])
    shared["final_g_b"] = bc(g["final_g"])
    x = g["x"].reshape(NCORES, NSEQ * SEQ, D)
    c = g["c"].reshape(NCORES, NSEQ, KC, 128)
    maps = []
    for i in range(NCORES):
        m = dict(shared)
        m["x"] = f(x[i])
        m["cT"] = f(c[i].transpose(2, 1, 0))
        maps.append(m)
    return maps


_NC_CACHE = {}


def run(inputs, cfg):
    key = tuple(sorted(cfg.items()))
    if key not in _NC_CACHE:
        _NC_CACHE[key] = build_nc(cfg)
    nc = _NC_CACHE[key]
    maps = _layout(inputs, cfg)
    res = run_bass_kernel_spmd(nc, maps, core_ids=list(range(cfg["NCORES"])))
    if cfg.get("DEBUG"):
        global DBG
        DBG = res.results
    out = np.stack([r["out"] for r in res.results], axis=0)
    B = cfg["NCORES"] * cfg["NSEQ"]
    return out.reshape(B, cfg["SEQ"], cfg["D"]).astype(np.float32)


def kernel(**inputs):
    return run(inputs, CFG_FULL)
```

```python
from contextlib import ExitStack
import numpy as np
import concourse.bass as bass
import concourse.mybir as mybir
from concourse.bass_utils import run_bass_kernel_spmd

F32 = mybir.dt.float32
BF16 = mybir.dt.bfloat16
I32 = mybir.dt.int32
AF = mybir.ActivationFunctionType
ALU = mybir.AluOpType
ENGS = ("pe", "act", "dve", "pool", "sp")

CFG_FULL = dict(D=1024, DE=1024, NE=32, SEQ=4096, NSEQ=2, NCORES=8)
EPS = 1e-6
LIMIT = 7.0
ALPHA = 1.702
TOPK = 4
USE_GELU_TANH_LUT = False
BR = 256
USE_COND_SKIP = True


class Prog:
    def __init__(self, nc):
        self.nc = nc
        self.tracks = {}
        self.issue = {e: [] for e in ENGS}
        self.last_write = {}
        self.readers = {}
        self.waited = {e: {} for e in ENGS}
        self.n_dma_tracks = 0

    def dma_track(self, name=""):
        self.n_dma_tracks += 1
        return "dma:%d:%s" % (self.n_dma_tracks, name)

    def op(self, eng, fn, reads=(), writes=(), track=None):
        track = track or eng
        tl = self.tracks.setdefault(track, [])
        idx = len(tl)
        deps = {}

        def add(d):
            t, i = d
            if t == track and t.startswith("dma:"):
                return
            if deps.get(t, -1) < i:
                deps[t] = i
        for k in reads:
            lw = self.last_write.get(k)
            if lw is not None:
                add(lw)
        for k in writes:
            lw = self.last_write.get(k)
            if lw is not None and lw[0] != track:
                add(lw)
            for r in self.readers.get(k, ()):
                if r[0] != track:
                    add(r)
        waits = []
        wd = self.waited[eng]
        for t, i in deps.items():
            if t.startswith("dma:"):
                i = len(self.tracks[t]) - 1
            if wd.get(t, -1) >= i:
                continue
            if t == eng and eng == "pe":
                continue
            wd[t] = i
            self.tracks[t][i]["need"] = True
            waits.append((t, i))
        rec = dict(eng=eng, track=track, fn=fn, waits=waits, need=track.startswith("dma:"))
        tl.append(rec)
        self.issue[eng].append(rec)
        for k in reads:
            self.readers.setdefault(k, []).append((track, idx))
        for k in writes:
            self.last_write[k] = (track, idx)
            self.readers[k] = []
        return rec

    def wait_all(self, eng, tracks):
        waits = []
        for t in tracks:
            tl = self.tracks.get(t)
            if not tl:
                continue
            i = len(tl) - 1
            tl[i]["need"] = True
            waits.append((t, i))
        self.issue[eng].append(dict(eng=eng, track=eng, fn=None, waits=waits, need=False))

    def barrier(self):
        tr = list(self.tracks.keys())
        for e in ENGS:
            self.wait_all(e, tr)
            for t in tr:
                self.waited[e][t] = len(self.tracks[t]) - 1

    def emit(self, stack):
        nc = self.nc
        sems, cum = {}, {}
        for n, (t, tl) in enumerate(self.tracks.items()):
            sems[t] = stack.enter_context(nc.semaphore("s%d" % n))
            c, step, arr = 0, (16 if t.startswith("dma:") else 1), []
            for rec in tl:
                if rec["need"]:
                    c += step
                arr.append(c)
            cum[t] = arr
        block = stack.enter_context(nc.Block())
        engobj = {"pe": "tensor", "act": "scalar", "dve": "vector", "pool": "gpsimd", "sp": "sync"}

        def make(engname):
            recs = self.issue[engname]

            def body(e):
                for rec in recs:
                    for (t, i) in rec["waits"]:
                        e.wait_ge(sems[t], cum[t][i])
                    if rec["fn"] is None:
                        continue
                    ins = rec["fn"](e)
                    if rec["need"]:
                        t = rec["track"]
                        ins.then_inc(sems[t], 16 if t.startswith("dma:") else 1)
            return body
        for engname in ENGS:
            if self.issue[engname]:
                getattr(block, engobj[engname])(make(engname))


class Arena:
    def __init__(self, nc, st, name, dt, nelem):
        self.t = st.enter_context(nc.sbuf_tensor(name, [128, nelem], dt))
        self.n, self.off, self.hi = nelem, 0, 0

    def reset(self):
        self.off = 0

    def alloc(self, shape):
        n = 1
        for d in shape[1:]:
            n *= d
        o = self.off
        self.off += n
        self.hi = max(self.hi, self.off)
        assert self.off <= self.n, ("arena overflow", self.off, self.n)
        ap = self.t[:shape[0], o:o + n]
        if len(shape) == 3:
            ap = ap.rearrange("p (a b) -> p a b", a=shape[1])
        elif len(shape) == 4:
            ap = ap.rearrange("p (a b c) -> p a b c", a=shape[1], b=shape[2])
        return ap


def build_nc(cfg):
    D, DE, NE, SEQ, NSEQ = cfg["D"], cfg["DE"], cfg["NE"], cfg["SEQ"], cfg["NSEQ"]
    KC, CE = D // 128, DE // 128
    NTOK = NSEQ * SEQ
    T1 = 256
    S1 = T1 // 128
    NT1 = SEQ // T1
    T2 = 512
    G2 = min(1024, NTOK)
    NG = NTOK // G2
    CB = min(512, D)
    NCB = D // CB
    D6 = 6 * D
    NSUBS = NTOK // 128
    KCM = max(KC, CE)
    PW = 256

    nc = bass.Bass("TRN2", target_bir_lowering=False)
    st = ExitStack()
    P = Prog(nc)

    def din(name, shape, dt=F32):
        return nc.dram_tensor(name, list(shape), dt, kind="ExternalInput").ap()

    def dscr(name, shape, dt):
        return nc.dram_tensor(name, list(shape), dt, kind="Internal").ap()

    x_d = din("x", [NTOK, D])
    cT_d = din("cT", [128, KC, NSEQ])
    adaw_d = din("ada_w", [128, KC, D6])
    adabT_d = din("ada_bT", [128, 6 * KC])
    adabg_d = din("ada_bg", [128, 2, D])
    g1T_d = din("g1T", [128, KC])
    g2T_d = din("g2T", [128, KC])
    win_d = din("w_in", [128, KC, D6])
    convwT_d = din("conv_wT", [128, KC, 4])
    convbT_d = din("conv_bT", [128, KC])
    lruwa_d = din("lru_wa", [KC, 2, 64, 64])
    lruwx_d = din("lru_wx", [KC, 2, 64, 64])
    baT_d = din("lru_baT", [128, KC])
    bxT_d = din("lru_bxT", [128, KC])
    lamT_d = din("lamT", [128, KC])
    lng_d = din("ln_g_b", [128, D])
    lnb_d = din("ln_b_b", [128, D])
    wsT_d = din("sg_wsT", [128, KC, 128])
    bsb_d = din("sg_bs_b", [128, KC, 128])
    wbr_d = din("w_br", [3, 128, KC, D])
    wr_d = din("w_router", [128, KC, NE])
    brb_d = din("b_router_b", [128, NE])
    wgu_d = din("w_gu", [NE, 128, KC, 2 * DE])
    bguT_d = din("b_guT", [NE, 128, 2 * CE])
    wdn_d = din("w_down", [NE, 128, CE, D])
    bdn_d = din("b_down", [NE, D])
    fgb_d = din("final_g_b", [128, D])
    out_d = nc.dram_tensor("out", [NTOK, D], F32, kind="ExternalOutput").ap()

    wmix_s = dscr("wmix_s", [9, 128, KC, D], BF16)
    QW = min(512, 2 * DE)
    NQ = (2 * DE) // QW
    ND = NCB
    NBLK = (NTOK * TOPK) // BR + NE
    SB = BR // 128
    wq_s = [dscr("wq_s%d" % q, [NE, 128, KC, QW], BF16) for q in range(NQ)]
    wd_s = [dscr("wd_s%d" % d_, [NE, 128, CE, CB], BF16) for d_ in range(ND)]
    x1_s = dscr("x1_s", [NTOK, D], F32)
    h2tm_s = dscr("h2tm_s", [NTOK, D], BF16)
    xs_s = dscr("xs_s", [NBLK * BR, D], BF16)
    ys_s = dscr("ys_s", [NBLK * BR, D], F32)

    def sb(name, shape, dt=F32):
        return st.enter_context(nc.sbuf_tensor(name, list(shape), dt))

    A32 = Arena(nc, st, "arena32", F32, cfg.get("A32", 15104))
    A16 = Arena(nc, st, "arena16", BF16, cfg.get("A16", 40960))

    def ar(name, shape, dt=F32):
        return (A32 if dt == F32 else A16).alloc(list(shape))

    NPS = 5
    ps = [st.enter_context(nc.psum_tensor("ps%d" % i, [128, 512], F32)) for i in range(NPS)]
    pst = [st.enter_context(nc.psum_tensor("pst%d" % i, [128, 512], BF16)) for i in range(2)]
    psm = st.enter_context(nc.psum_tensor("psm", [128, 512], F32))
    psmk = "psm"
    psc = [0]
    pstc = [0]

    def next_ps():
        i = psc[0] % NPS
        psc[0] += 1
        return ps[i], ("ps", i)

    def next_pst():
        i = pstc[0] % 2
        pstc[0] += 1
        return pst[i], ("pst", i)

    def mm(out, lhsT, rhs, start, stop, reads, writes):
        P.op("pe", lambda e: e.matmul(out, lhsT, rhs, start=start, stop=stop), reads, writes)

    def tr(out, in_, ident, reads, writes):
        P.op("pe", lambda e: e.transpose(out, in_, ident), reads, writes)

    def act(out, in_, func, reads, writes, bias=None, scale=None, accum_out=None, eng="act"):
        kw = {}
        if bias is not None:
            kw["bias"] = bias
        if scale is not None:
            kw["scale"] = scale
        if accum_out is not None:
            kw["accum_out"] = accum_out
        P.op("act", lambda e: e.activation(out, in_, func, **kw), reads, writes)

    def ts(eng, out, in0, s1, s2, op0, op1, reads, writes):
        if op1 is None:
            P.op(eng, lambda e: e.tensor_scalar(out, in0, s1, None, op0), reads, writes)
        else:
            P.op(eng, lambda e: e.tensor_scalar(out, in0, s1, s2, op0, op1), reads, writes)

    def tt(eng, out, in0, in1, op, reads, writes):
        P.op(eng, lambda e: e.tensor_tensor(out, in0, in1, op), reads, writes)

    def stt(out, in0, scalar, in1, op0, op1, reads, writes):
        P.op("dve", lambda e: e.scalar_tensor_tensor(out, in0, scalar, in1, op0, op1), reads, writes)

    def cp(eng, out, in_, reads, writes):
        if eng == "act":
            P.op("act", lambda e: e.copy(out, in_), reads, writes)
        else:
            P.op(eng, lambda e: e.tensor_copy(out, in_), reads, writes)

    def dma(q, out, in_, reads, writes, track):
        P.op(q, lambda e: e.dma_start(out=out, in_=in_), reads, writes, track=track)

    ctrack = P.dma_track("const")
    consts = {}

    def cload(name, dram, shape, dt=F32, q="sp", arena=False):
        t = ar("c_" + name, shape, dt) if arena else sb("c_" + name, shape, dt)
        dma(q, t[:], dram, [], ["c_" + name], ctrack)
        consts[name] = t
        return t

    cT = cload("cT", cT_d, [128, KC, NSEQ])
    adabT = cload("adabT", adabT_d, [128, 6 * KC])
    adabg = cload("adabg", adabg_d, [128, 2, D], arena=True)
    g1T = cload("g1T", g1T_d, [128, KC])
    g2T = cload("g2T", g2T_d, [128, KC])
    convwT = cload("convwT", convwT_d, [128, KC, 4])
    convbT = cload("convbT", convbT_d, [128, KC])
    baT = cload("baT", baT_d, [128, KC])
    bxT = cload("bxT", bxT_d, [128, KC])
    lamT = cload("lamT", lamT_d, [128, KC])
    lng = cload("lng", lng_d, [128, D])
    lnb = cload("lnb", lnb_d, [128, D])
    wsT32 = cload("wsT32", wsT_d, [128, KC, 128], arena=True)
    bsb = cload("bsb", bsb_d, [128, KC, 128])
    wr32 = cload("wr32", wr_d, [128, KC, NE], arena=True)
    brb = cload("brb", brb_d, [128, NE])
    bdn = cload("bdn", bdn_d, [NE, D])
    fgb = cload("fgb", fgb_d, [128, D])
    wbd32 = ar("wbd32", [128, 2, KC, 128])
    P.op("pool", lambda e: e.memset(wbd32[:], 0.0), [], ["c_wbd32"])
    for gi, src in enumerate((lruwa_d, lruwx_d)):
        for half in range(2):
            dma("sp", wbd32[half * 64:(half + 1) * 64, gi, :, half * 64:(half + 1) * 64],
                src[:, half].rearrange("k i j -> i k j"), [], ["c_wbd32"], ctrack)

    ident_bf = sb("ident_bf", [128, 128], BF16)
    ident32 = sb("ident32", [128, 128], F32)
    ones32 = ar("ones32", [128, 128], F32)
    P.op("pool", lambda e: e.memset(ones32[:], 1.0), [], ["ones32"])
    P.op("pool", lambda e: e.affine_select(out=ident32[:], in_=ones32[:], pattern=[[-1, 128]],
                                           compare_op=ALU.is_equal, fill=0.0, base=0, channel_multiplier=1),
         ["ones32"], ["ident32"])
    cp("dve", ident_bf[:], ident32[:], ["ident32"], ["ident_bf"])

    wbd = sb("wbd", [128, 2, KC, 128], BF16)
    cp("dve", wbd[:], wbd32[:], ["c_wbd32"], ["wbd"])
    wsT = sb("wsT", [128, KC, 128], BF16)
    wsTm = ar("wsTm", [128, KC, 128], F32)
    P.op("pool", lambda e: e.affine_select(out=wsTm[:], in_=wsT32[:], pattern=[[0, KC], [1, 128]],
                                           compare_op=ALU.is_ge, fill=0.0, base=0, channel_multiplier=-1),
         ["c_wsT32"], ["wsTm"])
    cp("dve", wsT[:], wsTm[:], ["wsTm"], ["wsT"])
    wr = sb("wr", [128, KC, NE], BF16)
    cp("dve", wr[:], wr32[:], ["c_wr32"], ["wr"])

    kneg = sb("kneg", [128, KC])
    k2 = sb("k2", [128, KC])
    ktmp = sb("ktmp", [128, KC])
    act(ktmp[:], lamT[:], AF.Exp, ["c_lamT"], ["ktmp"], scale=-1.0)
    act(ktmp[:], ktmp[:], AF.Ln, ["ktmp"], ["ktmp"], bias=1.0)
    ts("dve", kneg[:], ktmp[:], -8.0, None, ALU.mult, None, ["ktmp"], ["kneg"])
    ts("dve", k2[:], ktmp[:], -16.0, None, ALU.mult, None, ["ktmp"], ["k2"])

    sc = sb("sc", [128, KC, NSEQ])
    sgt = sb("sgt", [128, KC, NSEQ])
    act(sgt[:], cT[:], AF.Sigmoid, ["c_cT"], ["sgt"])
    tt("dve", sc[:], sgt[:], cT[:], ALU.mult, ["sgt", "c_cT"], ["sc"])
    screp = ar("screp", [128, NSEQ, KC, 128])
    for b in range(NSEQ):
        for k in range(KC):
            cp("dve", screp[:, b, k, :], sc[:, k, b:b + 1].to_broadcast([128, 128]), ["sc"], ["screp"])
    modT = sb("modT", [128, NSEQ, 6 * KC])
    gtb = sb("gtb", [128, 2, NSEQ, D])
    stg = [ar("stg%d" % i, [128, KCM, PW]) for i in range(2)]
    stg_tr = [P.dma_track("stg%d" % i) for i in range(2)]
    stgc = [0]

    def stage_load(dram_ap, q="sp"):
        i = stgc[0] % 2
        stgc[0] += 1
        dma(q, stg[i][:, :dram_ap.shape[1], :dram_ap.shape[2]], dram_ap, [], [("stg", i)], stg_tr[i])
        return stg[i], ("stg", i)

    NJB = D6 // PW
    for jb in range(NJB):
        s_t, s_k = stage_load(adaw_d[:, :, jb * PW:(jb + 1) * PW])
        for jj in range(PW // 128):
            j = jb * (PW // 128) + jj
            for b in range(NSEQ):
                for k in range(KC):
                    col = b * 6 * KC + j
                    mm(psm[:, col:col + 1], s_t[:, k, jj * 128:(jj + 1) * 128], sc[:, k, b:b + 1],
                       k == 0, k == KC - 1, [s_k, "sc"], [psmk])
        m = (jb * PW) // D
        if m in (2, 5):
            which = 0 if m == 2 else 1
            c0 = (jb * PW) % D
            for b in range(NSEQ):
                pg, pgk = next_ps()
                for k in range(KC):
                    mm(pg[:, :PW], screp[:, b, k, :], s_t[:, k, :], k == 0, k == KC - 1, [s_k, "screp"], [pgk])
                tt("dve", gtb[:, which, b, c0:c0 + PW], pg[:, :PW], adabg[:, which, c0:c0 + PW], ALU.add,
                   [pgk, "c_adabg"], ["gtb"])
    for b in range(NSEQ):
        tt("dve", modT[:, b, :], psm[:, b * 6 * KC:(b + 1) * 6 * KC], adabT[:], ALU.add,
           [psmk, "c_adabT"], ["modT"])
    s1 = sb("s1", [128, NSEQ, KC])
    s2 = sb("s2", [128, NSEQ, KC])
    for b in range(NSEQ):
        stt(s1[:, b, :], modT[:, b, KC:2 * KC], 1.0, g1T[:], ALU.add, ALU.mult, ["modT", "c_g1T"], ["s1"])
        stt(s2[:, b, :], modT[:, b, 4 * KC:5 * KC], 1.0, g2T[:], ALU.add, ALU.mult, ["modT", "c_g2T"], ["s2"])

    cbf = [ar("cbf%d" % i, [128, KCM, PW], BF16) for i in range(2)]
    cbf_tr = [P.dma_track("cbf%d" % i) for i in range(2)]
    cbc = [0]

    def precast(src_ap, dst_ap, dst_key):
        kc, w = src_ap.shape[1], src_ap.shape[2]
        s_t, s_k = stage_load(src_ap)
        i = cbc[0] % 2
        cbc[0] += 1
        cp("act", cbf[i][:, :kc, :w], s_t[:, :kc, :w], [s_k], [("cbf", i)])
        dma("act", dst_ap, cbf[i][:, :kc, :w], [("cbf", i)], [dst_key], cbf_tr[i])

    for blk in range(9):
        for c0 in range(0, D, PW):
            w = min(PW, D - c0)
            src = win_d[:, :, blk * D + c0: blk * D + c0 + w] if blk < 6 else wbr_d[blk - 6][:, :, c0:c0 + w]
            precast(src, wmix_s[blk][:, :, c0:c0 + w], ("wmix", blk, c0))
    for e_ in range(NE):
        for c0 in range(0, 2 * DE, PW):
            w = min(PW, 2 * DE - c0)
            precast(wgu_d[e_][:, :, c0:c0 + w], wq_s[c0 // QW][e_][:, :, c0 % QW:c0 % QW + w], ("wq_s", e_, c0))
        for c0 in range(0, D, PW):
            w = min(PW, D - c0)
            precast(wdn_d[e_][:, :, c0:c0 + w], wd_s[c0 // CB][e_][:, :, c0 % CB:c0 % CB + w], ("wd_s", e_, c0))

    P.barrier()
    A32.reset()
    A16.reset()
    wts = sb("wts", [128, NSUBS, NE])
    hist = sb("hist", [128, KC, 3])
    hstate = sb("hstate", [128, KC])
    RING = 3
    wring = [ar("wring%d" % i, [128, KC, D], BF16) for i in range(RING)]
    wring_tr = [P.dma_track("wring%d" % i) for i in range(RING)]
    wrc = [0]

    def wload(blk):
        i = wrc[0] % RING
        wrc[0] += 1
        dma("sp", wring[i][:], wmix_s[blk], [("wmix", blk, c0) for c0 in range(0, D, PW)], [("wring", i)], wring_tr[i])
        return wring[i], ("wring", i)

    X = ar("X", [128, S1, D])
    x_tr = P.dma_track("x")
    xn = ar("xn", [128, S1, D], BF16)
    hT = ar("hT", [128, KC, T1], BF16)
    yrT = ar("yrT", [128, KC, T1], BF16)
    ysT = ar("ysT", [128, KC, T1], BF16)
    mT = ar("mT", [128, KC, T1], BF16)
    tmpR = ar("tmpR", [128, KC, T1])
    junk = ar("junk", [128, D])
    ssq = sb("ssq", [128, 4])
    rstd = sb("rstd", [128, 4])
    NB = 2
    tmp = {n: [ar("t_%s%d" % (n, i), [128, T1 + (4 if n == "rx" else 0)]) for i in range(NB)]
           for n in ("rx", "cv", "r", "i", "a", "m", "u", "hs", "g", "g2", "q")}
    cvb = [ar("cvb%d" % i, [128, T1], BF16) for i in range(NB)]
    vtm = [ar("vtm%d" % i, [128, D]) for i in range(2)]
    vt2 = [ar("vt2%d" % i, [128, D]) for i in range(2)]
    bnst = sb("bnst", [128, 2 * max(1, D // 512), 6])
    mv = sb("mv", [128, 2])
    x1st_tr = P.dma_track("x1st")
    h2st_tr = P.dma_track("h2st")
    h2tm = ar("h2tm", [128, S1, D], BF16)
    h2T_o = ar("h2T_o", [128, KC, T1], BF16)
    lg = sb("lg", [128, NE])
    top8 = sb("top8", [128, 8])
    negmx = sb("negmx", [128, 1])
    msk = sb("msk", [128, NE])
    ex = sb("ex", [128, NE])
    den = sb("den", [128, 1])

    def gelu(eng_alt, out, in_, n, reads, writes, tg, tgk):
        if USE_GELU_TANH_LUT:
            act(out, in_, AF.Gelu_apprx_tanh, reads, writes)
            return
        act(tg, in_, AF.Square, reads, [tgk])
        ts("dve", tg, tg, 0.044715, 1.0, ALU.mult, ALU.add, [tgk], [tgk])
        tt("dve", tg, tg, in_, ALU.mult, [tgk] + list(reads), [tgk])
        act(tg, tg, AF.Sigmoid, [tgk], [tgk], scale=1.5957691216057308)
        tt("dve", out, tg, in_, ALU.mult, [tgk] + list(reads), writes)

    def rmsnorm_to_T(src, src_key, nsub, dstT, dstT_key, scale_ap_fn, bias_ap_fn, sk_reads):
        for s in range(nsub):
            act(junk[:], src[:, s, :], AF.Square, [src_key], ["junk"], accum_out=ssq[:, s:s + 1])
            P.issue
            P.last_write[("ssq", s)] = P.last_write["junk"]
            P.readers[("ssq", s)] = []
        for s in range(nsub):
            act(rstd[:, s:s + 1], ssq[:, s:s + 1], AF.Sqrt, [("ssq", s)], [("rstd", s)], scale=1.0 / D, bias=EPS)
            P.op("dve", lambda e, s=s: e.reciprocal(rstd[:, s:s + 1], rstd[:, s:s + 1]), [("rstd", s)], [("rstd", s)])
            ts("dve", xn[:, s, :], src[:, s, :], rstd[:, s:s + 1], None, ALU.mult, None,
               [src_key, ("rstd", s)], [("xn", s)])
        for k in range(KC):
            pt, ptk = next_pst()
            for s in range(nsub):
                tr(pt[:, s * 128:(s + 1) * 128], xn[:, s, k * 128:(k + 1) * 128], ident_bf[:],
                   [("xn", s), "ident_bf"], [ptk])
            act(dstT[:, k, :nsub * 128], pt[:, :nsub * 128], AF.Identity, [ptk] + sk_reads, [dstT_key],
                scale=scale_ap_fn(k), bias=bias_ap_fn(k))

    for b in range(NSEQ):
        P.op("pool", lambda e: e.memset(hist[:], 0.0), [], ["hist"])
        P.op("pool", lambda e: e.memset(hstate[:], 0.0), [], ["hstate"])
        for j in range(NT1):
            tok0 = b * SEQ + j * T1
            dma("sp", X[:], x_d[tok0:tok0 + T1, :].rearrange("(s p) d -> p s d", p=128), [], ["X"], x_tr)
            rmsnorm_to_T(X, "X", S1, hT, "hT",
                         lambda k: s1[:, b, k:k + 1], lambda k: modT[:, b, k:k + 1], ["s1", "modT"])
            w0, w0k = wload(0)
            w1, w1k = wload(1)
            for c in range(KC):
                i_ = c % NB
                T = {n: tmp[n][i_] for n in tmp}
                K = {n: ("t", n, i_) for n in tmp}
                pz, pzk = next_ps()
                for k in range(KC):
                    mm(pz[:, :T1], w0[:, k, c * 128:(c + 1) * 128], hT[:, k, :], k == 0, k == KC - 1,
                       [w0k, "hT"], [pzk])
                cp("act", T["rx"][:, 0:3], hist[:, c, :], ["hist"], [K["rx"]])
                cp("act", T["rx"][:, 3:3 + T1], pz[:, :T1], [pzk], [K["rx"]])
                cp("act", hist[:, c, :], T["rx"][:, T1:T1 + 3], [K["rx"]], ["hist"])
                ts("dve", T["cv"][:, :], T["rx"][:, 0:T1], convwT[:, c, 0:1], convbT[:, c:c + 1], ALU.mult, ALU.add,
                   [K["rx"], "c_convwT", "c_convbT"], [K["cv"]])
                for kk in range(1, 4):
                    stt(T["cv"][:, :], T["rx"][:, kk:kk + T1], convwT[:, c, kk:kk + 1], T["cv"][:, :],
                        ALU.mult, ALU.add, [K["rx"], K["cv"], "c_convwT"], [K["cv"]])
                cp("pool", cvb[i_][:, :], T["cv"][:, :], [K["cv"]], [("cvb", i_)])
                pr, prk = next_ps()
                mm(pr[:, :T1], wbd[:, 0, c, :], cvb[i_][:, :], True, True, ["wbd", ("cvb", i_)], [prk])
                pi, pik = next_ps()
                mm(pi[:, :T1], wbd[:, 1, c, :], cvb[i_][:, :], True, True, ["wbd", ("cvb", i_)], [pik])
                act(T["r"][:, :], pr[:, :T1], AF.Sigmoid, [prk, "c_baT"], [K["r"]], bias=baT[:, c:c + 1])
                act(T["i"][:, :], pi[:, :T1], AF.Sigmoid, [pik, "c_bxT"], [K["i"]], bias=bxT[:, c:c + 1])
                act(T["a"][:, :], T["r"][:, :], AF.Exp, [K["r"], "kneg"], [K["a"]], scale=kneg[:, c:c + 1])
                act(T["m"][:, :], T["r"][:, :], AF.Exp, [K["r"], "k2"], [K["m"]], scale=k2[:, c:c + 1])
                act(T["m"][:, :], T["m"][:, :], AF.Sqrt, [K["m"]], [K["m"]], scale=-1.0, bias=1.0)
                tt("pool", T["u"][:, :], T["i"][:, :], T["cv"][:, :], ALU.mult, [K["i"], K["cv"]], [K["u"]])
                tt("pool", T["u"][:, :], T["u"][:, :], T["m"][:, :], ALU.mult, [K["u"], K["m"]], [K["u"]])
                P.op("dve", lambda e, T=T, c=c: e.tensor_tensor_scan(T["hs"][:, :], T["a"][:, :], T["u"][:, :],
                                                                       hstate[:, c:c + 1], ALU.mult, ALU.add),
                     [K["a"], K["u"], "hstate"], [K["hs"]])
                cp("pool", hstate[:, c:c + 1], T["hs"][:, T1 - 1:T1], [K["hs"]], ["hstate"])
                pg, pgk = next_ps()
                for k in range(KC):
                    mm(pg[:, :T1], w1[:, k, c * 128:(c + 1) * 128], hT[:, k, :], k == 0, k == KC - 1,
                       [w1k, "hT"], [pgk])
                gelu("dve", T["g"][:, :], pg[:, :T1], T1, [pgk], [K["g"]], T["g2"][:, :], K["g2"])
                tt("dve", yrT[:, c, :], T["hs"][:, :], T["g"][:, :], ALU.mult, [K["hs"], K["g"]], ["yrT"])
            w3, w3k = wload(3)
            for s in range(S1):
                vi = s % 2
                for cb in range(NCB):
                    pv, pvk = next_ps()
                    for k in range(KC):
                        mm(pv[:, :CB], hT[:, k, s * 128:(s + 1) * 128], w3[:, k, cb * CB:(cb + 1) * CB],
                           k == 0, k == KC - 1, [w3k, "hT"], [pvk])
                    gelu("dve", vtm[vi][:, cb * CB:(cb + 1) * CB], pv[:, :CB], CB, [pvk], [("vtm", vi)],
                         vt2[vi][:, cb * CB:(cb + 1) * CB], ("vt2", vi))
                nchunk = max(1, D // 512)
                cw = D // nchunk
                for q in range(nchunk):
                    P.op("dve", lambda e, vi=vi, q=q: e.bn_stats(bnst[:, q, :], vtm[vi][:, q * cw:(q + 1) * cw]),
                         [("vtm", vi)], ["bnst"])
                P.op("dve", lambda e: e.bn_aggr(mv[:], bnst[:, :nchunk, :].rearrange("p a b -> p (a b)")),
                     ["bnst"], ["mv"])
                act(mv[:, 1:2], mv[:, 1:2], AF.Sqrt, ["mv"], ["mv"], bias=EPS)
                P.op("dve", lambda e: e.reciprocal(mv[:, 1:2], mv[:, 1:2]), ["mv"], ["mv"])
                ts("dve", vtm[vi][:, :], vtm[vi][:, :], mv[:, 0:1], mv[:, 1:2], ALU.subtract, ALU.mult,
                   [("vtm", vi), "mv"], [("vtm", vi)])
                tt("pool", vtm[vi][:, :], vtm[vi][:, :], lng[:], ALU.mult, [("vtm", vi), "c_lng"], [("vtm", vi)])
                tt("pool", xn[:, s, :], vtm[vi][:, :], lnb[:], ALU.add, [("vtm", vi), "c_lnb"], [("xn", s)])
            w2, w2k = wload(2)
            for g in range(KC):
                i_ = g % NB
                T = {n: tmp[n][i_] for n in tmp}
                K = {n: ("t", n, i_) for n in tmp}
                pu, puk = next_ps()
                for k in range(KC):
                    mm(pu[:, :T1], w2[:, k, g * 128:(g + 1) * 128], hT[:, k, :], k == 0, k == KC - 1,
                       [w2k, "hT"], [puk])
                gelu("dve", T["g"][:, :], pu[:, :T1], T1, [puk], [K["g"]], T["g2"][:, :], K["g2"])
                psv, psvk = next_ps()
                for s in range(S1):
                    mm(psv[:, s * 128:(s + 1) * 128], xn[:, s, g * 128:(g + 1) * 128], wsT[:, g, :], True, True,
                       [("xn", s), "wsT"], [psvk])
                for s in range(S1):
                    tt("dve", T["q"][:, s * 128:(s + 1) * 128], psv[:, s * 128:(s + 1) * 128], bsb[:, g, :], ALU.add,
                       [psvk, "c_bsb"], [K["q"]])
                tt("pool", ysT[:, g, :], T["q"][:, :], T["g"][:, :], ALU.mult, [K["q"], K["g"]], ["ysT"])
            for pas, (ga, gb_, yT, yk) in enumerate(((4, 6, yrT, "yrT"), (5, 7, ysT, "ysT"))):
                wg, wgk = wload(ga)
                wb, wbk = wload(gb_)
                for oc in range(KC):
                    i_ = oc % NB
                    T = {n: tmp[n][i_] for n in tmp}
                    K = {n: ("t", n, i_) for n in tmp}
                    pgt, pgtk = next_ps()
                    for k in range(KC):
                        mm(pgt[:, :T1], wg[:, k, oc * 128:(oc + 1) * 128], hT[:, k, :], k == 0, k == KC - 1,
                           [wgk, "hT"], [pgtk])
                    act(T["g"][:, :], pgt[:, :T1], AF.Sigmoid, [pgtk], [K["g"]])
                    pbr, pbrk = next_ps()
                    for k in range(KC):
                        mm(pbr[:, :T1], wb[:, k, oc * 128:(oc + 1) * 128], yT[:, k, :], k == 0, k == KC - 1,
                           [wbk, yk], [pbrk])
                    if pas == 0:
                        tt("dve", tmpR[:, oc, :], T["g"][:, :], pbr[:, :T1], ALU.mult, [K["g"], pbrk], [("tmpR", oc)])
                    else:
                        tt("dve", T["i"][:, :], T["g"][:, :], pbr[:, :T1], ALU.mult, [K["g"], pbrk], [K["i"]])
                        tt("pool", mT[:, oc, :], tmpR[:, oc, :], T["i"][:, :], ALU.add, [("tmpR", oc), K["i"]], ["mT"])
            w8, w8k = wload(8)
            for s in range(S1):
                for cb in range(NCB):
                    po, pok = next_ps()
                    for k in range(KC):
                        mm(po[:, :CB], mT[:, k, s * 128:(s + 1) * 128], w8[:, k, cb * CB:(cb + 1) * CB],
                           k == 0, k == KC - 1, [w8k, "mT"], [pok])
                    tt("dve", junk[:, cb * CB:(cb + 1) * CB], po[:, :CB], gtb[:, 0, b, cb * CB:(cb + 1) * CB], ALU.mult,
                       [pok, "gtb"], ["junk"])
                    tt("pool", X[:, s, cb * CB:(cb + 1) * CB], X[:, s, cb * CB:(cb + 1) * CB],
                       junk[:, cb * CB:(cb + 1) * CB], ALU.add, ["X", "junk"], ["X"])
            dma("sp", x1_s[tok0:tok0 + T1, :].rearrange("(s p) d -> p s d", p=128), X[:], ["X"], ["x1_s"], x1st_tr)
            rmsnorm_to_T(X, "X", S1, h2T_o, "h2T_o",
                         lambda k: s2[:, b, k:k + 1], lambda k: modT[:, b, 3 * KC + k:3 * KC + k + 1], ["s2", "modT"])
            for s in range(S1):
                for k0 in range(0, KC, 4):
                    pt, ptk = next_pst()
                    nk = min(4, KC - k0)
                    for kk in range(nk):
                        tr(pt[:, kk * 128:(kk + 1) * 128], h2T_o[:, k0 + kk, s * 128:(s + 1) * 128], ident_bf[:],
                           ["h2T_o", "ident_bf"], [ptk])
                    cp("act", h2tm[:, s, k0 * 128:(k0 + nk) * 128], pt[:, :nk * 128], [ptk], ["h2tm"])
            dma("sp", h2tm_s[tok0:tok0 + T1, :].rearrange("(s p) d -> p s d", p=128), h2tm[:], ["h2tm"], ["h2tm_s"], h2st_tr)
            for s in range(S1):
                sub = (tok0 // 128) + s
                pl, plk = next_ps()
                for k in range(KC):
                    mm(pl[:, :NE], h2T_o[:, k, s * 128:(s + 1) * 128], wr[:, k, :], k == 0, k == KC - 1,
                       ["h2T_o", "wr"], [plk])
                tt("dve", lg[:], pl[:, :NE], brb[:], ALU.add, [plk, "c_brb"], ["lg"])
                P.op("dve", lambda e: e.max(top8[:], lg[:]), ["lg"], ["top8"])
                ts("dve", negmx[:], top8[:, 0:1], -1.0, None, ALU.mult, None, ["top8"], ["negmx"])
                ts("dve", msk[:], lg[:], top8[:, TOPK - 1:TOPK], None, ALU.is_ge, None, ["lg", "top8"], ["msk"])
                act(ex[:], lg[:], AF.Exp, ["lg", "negmx"], ["ex"], bias=negmx[:, 0:1])
                tt("dve", ex[:], ex[:], msk[:], ALU.mult, ["ex", "msk"], ["ex"])
                P.op("dve", lambda e: e.reduce_sum(den[:], ex[:], axis=mybir.AxisListType.X), ["ex"], ["den"])
                P.op("dve", lambda e: e.reciprocal(den[:], den[:]), ["den"], ["den"])
                ts("dve", wts[:, sub, :], ex[:], den[:, 0:1], None, ALU.mult, None, ["ex", "den"], ["wts"])

    P.barrier()
    A32.reset()
    A16.reset()
    LOGB = BR.bit_length() - 1
    d4f = sb("d4f", [128, NSUBS, 8])
    d4i = sb("d4i", [128, NSUBS, 4], I32)
    w4 = sb("w4", [128, NSUBS, 4])
    be_i = sb("be_i", [128, NBLK], I32)
    ld_i = sb("ld_i", [128, NBLK], I32)
    widx = sb("widx", [128, NBLK], I32)
    mask = ar("mask", [128, NSUBS, NE])
    dest = ar("dest", [128, NSUBS, NE])
    dkey = ar("dkey", [128, NSUBS, NE])
    ustr = ar("ustr", [128, 128])
    onesq = ar("onesq", [128, 128])
    macc = ar("macc", [128, NE])
    cnt = ar("cnt", [128, NE])
    padded = ar("padded", [128, NE])
    pend = ar("pend", [128, NE])
    pstart = ar("pstart", [128, NE])
    onesne = ar("onesne", [128, NE])
    eqt = ar("eqt", [128, NE])
    bst_i = A32.alloc([128, NBLK]).bitcast(I32)
    bst = ar("bst", [128, NBLK])
    bef = ar("bef", [128, NBLK])
    bet = ar("bet", [128, NBLK])
    P.op("pool", lambda e: e.memset(onesq[:], 1.0), [], ["onesq"])
    P.op("pool", lambda e: e.memset(onesne[:], 1.0), [], ["onesne"])
    P.op("pool", lambda e: e.memset(macc[:], 0.0), [], ["macc"])
    P.op("pool", lambda e: e.affine_select(out=ustr[:], in_=onesq[:], pattern=[[1, 128]], compare_op=ALU.is_ge,
                                           fill=0.0, base=-1, channel_multiplier=-1), ["onesq"], ["ustr"])
    ts("dve", mask[:], wts[:], 0.0, None, ALU.is_gt, None, ["wts"], ["mask"])
    for i in range(NSUBS):
        prk_t, prk = next_ps()
        mm(prk_t[:, :NE], ustr[:], mask[:, i, :], True, False, ["ustr", "mask"], [prk])
        mm(prk_t[:, :NE], onesq[:], macc[:], False, True, ["onesq", "macc"], [prk])
        cp("act", dest[:, i, :], prk_t[:, :NE], [prk], [("dest", i)])
        tt("dve", macc[:], macc[:], mask[:, i, :], ALU.add, ["macc", "mask"], ["macc"])
    pc_t, pck = next_ps()
    mm(pc_t[:, :NE], onesq[:], macc[:], True, True, ["onesq", "macc"], [pck])
    cp("dve", cnt[:], pc_t[:, :NE], [pck], ["cnt"])
    P.op("pool", lambda e: e.memset(padded[:], 0.0), [], ["padded"])
    for j in range(NTOK // BR):
        stt(padded[:], cnt[:], float(j * BR), padded[:], ALU.is_gt, ALU.add, ["cnt", "padded"], ["padded"])
    ts("dve", padded[:], padded[:], float(BR), None, ALU.mult, None, ["padded"], ["padded"])
    P.op("dve", lambda e: e.tensor_tensor_scan(pend[:], onesne[:], padded[:], 0.0, ALU.mult, ALU.add),
         ["onesne", "padded"], ["pend"])
    tt("dve", pstart[:], pend[:], padded[:], ALU.subtract, ["pend", "padded"], ["pstart"])
    for i in range(NSUBS):
        tt("dve", dest[:, i, :], dest[:, i, :], pstart[:], ALU.add, [("dest", i), "pstart"], [("dest", i)])
    dkeys = [("dest", i) for i in range(NSUBS)]
    stt(dkey[:], dest[:], 1.0, mask[:], ALU.add, ALU.mult, dkeys + ["mask"], ["dkey"])
    ts("dve", dkey[:], dkey[:], -1.0, None, ALU.add, None, ["dkey"], ["dkey"])
    for i in range(NSUBS):
        P.op("dve", lambda e, i=i: e.max(d4f[:, i, :], dkey[:, i, :]), ["dkey"], [("d4f", i)])
        for k4 in range(TOPK):
            ts("dve", eqt[:], dkey[:, i, :], d4f[:, i, k4:k4 + 1], None, ALU.is_equal, None, ["dkey", ("d4f", i)], ["eqt"])
            tt("dve", eqt[:], eqt[:], wts[:, i, :], ALU.mult, ["eqt", "wts"], ["eqt"])
            P.op("dve", lambda e, i=i, k4=k4: e.reduce_sum(w4[:, i, k4:k4 + 1], eqt[:], axis=mybir.AxisListType.X),
                 ["eqt"], ["w4"])
        cp("dve", d4i[:, i, :], d4f[:, i, 0:TOPK], [("d4f", i)], ["d4i"])
    P.op("pool", lambda e: e.iota(bst_i, pattern=[[BR, NBLK]], base=0, channel_multiplier=0), [], ["bst_i"])
    cp("dve", bst[:], bst_i, ["bst_i"], ["bst"])
    P.op("pool", lambda e: e.memset(bef[:], 0.0), [], ["bef"])
    for e_ in range(NE):
        ts("dve", bet[:], bst[:], pend[:, e_:e_ + 1], None, ALU.is_ge, None, ["bst", "pend"], ["bet"])
        tt("dve", bef[:], bef[:], bet[:], ALU.add, ["bef", "bet"], ["bef"])
    ts("dve", bef[:], bef[:], float(NE - 1), None, ALU.min, None, ["bef"], ["bef"])
    cp("dve", be_i[:], bef[:], ["bef"], ["be_i"])
    P.op("pool", lambda e: e.memset(bet[:], 1.0), ["bet"], ["bet"])
    if NBLK > 1:
        tt("dve", bet[:, 1:NBLK], bef[:, 1:NBLK], bef[:, 0:NBLK - 1], ALU.not_equal, ["bef", "bet"], ["bet"])
    cp("dve", ld_i[:], bet[:], ["bet"], ["ld_i"])
    pidx_i = A32.alloc([128, 1]).bitcast(I32)
    pidx = ar("pidx", [128, 1])
    widf = ar("widf", [128, NBLK])
    P.op("pool", lambda e: e.iota(pidx_i, pattern=[[0, 1]], base=0, channel_multiplier=1), [], ["pidx_i"])
    cp("dve", pidx[:], pidx_i, ["pidx_i"], ["pidx"])
    ts("dve", widf[:], bef[:], 128.0, pidx[:, 0:1], ALU.mult, ALU.add, ["bef", "pidx"], ["widf"])
    if USE_COND_SKIP:
        ts("dve", bet[:], bet[:], -1.0, -float(2 ** 30), ALU.add, ALU.mult, ["bet"], ["bet"])
        tt("dve", widf[:], widf[:], bet[:], ALU.add, ["widf", "bet"], ["widf"])
    cp("dve", widx[:], widf[:], ["widf"], ["widx"])

    if cfg.get("DEBUG"):
        dbg_tr = P.dma_track("dbg")
        MX = max(NE, NBLK)
        dd = {"dbg_d4f": (d4f, [128, NSUBS, 8], ["d4i"]), "dbg_w4": (w4, [128, NSUBS, 4], ["w4"]),
              "dbg_wts": (wts, [128, NSUBS, NE], ["wts"]), "dbg_cnt": (cnt, [128, NE], ["cnt"]),
              "dbg_pend": (pend, [128, NE], ["pend"]), "dbg_bef": (bef, [128, NBLK], ["bef"]),
              "dbg_widf": (widf, [128, NBLK], ["widx"]), "dbg_dest": (dest, [128, NSUBS, NE], ["dkey"]),
              "dbg_dkey": (dkey, [128, NSUBS, NE], ["dkey", "d4i"])}
        for nm, (t_, shp, rd) in dd.items():
            o_ = nc.dram_tensor(nm, shp, F32, kind="ExternalOutput").ap()
            dma("sp", o_, t_[:], rd, [nm], dbg_tr)
    P.barrier()
    A32.reset()
    A16.reset()
    zt = ar("zt", [128, 4, D], BF16)
    z_tr = P.dma_track("zfill")
    P.op("pool", lambda e: e.memset(zt[:], 0.0), [], ["zt"])
    for r0 in range(0, NBLK * BR, 512):
        dma("sp", xs_s[r0:r0 + 512, :].rearrange("(s p) d -> p s d", p=128), zt[:], ["zt"], ["xs_s"], z_tr)
    hsc = [ar("hsc%d" % i, [128, D], BF16) for i in range(2)]
    hsc_tr = [P.dma_track("hsc%d" % i) for i in range(2)]
    sc_tr = [P.dma_track("scat%d" % i) for i in range(2)]
    for i in range(NSUBS):
        hi = i % 2
        dma("sp", hsc[hi][:], h2tm_s[i * 128:(i + 1) * 128, :], ["h2tm_s"], [("hsc", hi)], hsc_tr[hi])
        for k4 in range(TOPK):
            P.op("pool", lambda e, hi=hi, i=i, k4=k4: e.indirect_dma_start(
                out=xs_s[:, :], out_offset=bass.IndirectOffsetOnAxis(ap=d4i[:, i, k4:k4 + 1], axis=0),
                in_=hsc[hi][:, :], in_offset=None), [("hsc", hi), "d4i", "xs_s"], [("xs_sc", i, k4)], track=sc_tr[hi])
    P.barrier()
    A32.reset()
    A16.reset()

    wq = [ar("wq%d" % i, [128, KC, QW], BF16) for i in range(NQ)]
    wd = [ar("wd%d" % i, [128, CE, CB], BF16) for i in range(ND)]
    wq_tr = [P.dma_track("wq%d" % i) for i in range(NQ)]
    wd_tr = [P.dma_track("wd%d" % i) for i in range(ND)]
    bg = [sb("bg%d" % i, [128, 2 * CE]) for i in range(2)]
    bg_tr = [P.dma_track("bg%d" % i) for i in range(2)]
    xb = [ar("xb%d" % i, [128, SB, D], BF16) for i in range(2)]
    xb_tr = [P.dma_track("xb%d" % i) for i in range(2)]
    XT = [ar("XT%d" % i, [128, KC, BR], BF16) for i in range(2)]
    actT = [ar("actT%d" % i, [128, CE, BR], BF16) for i in range(2)]
    mt = {n: [ar("m_%s%d" % (n, i), [128, BR]) for i in range(2)] for n in ("g", "s", "u")}
    Yt = [ar("Yt%d" % i, [128, SB, D]) for i in range(2)]
    y_tr = [P.dma_track("yst%d" % i) for i in range(2)]
    POOL_ET = mybir.EngineType.Pool

    def dyn_load(dst_ap, src_rows, blk, reads, writes, track):
        def fn(e):
            kw = {}
            if USE_COND_SKIP:
                if "r" not in breg:
                    breg["r"] = e.to_reg(NE * 128 - 1)
                kw = dict(bounds_check=breg["r"], oob_is_err=False)
            return e.indirect_dma_start(out=dst_ap, out_offset=None, in_=src_rows,
                                        in_offset=bass.IndirectOffsetOnAxis(ap=widx[:, blk:blk + 1], axis=0), **kw)
        P.op("pool", fn, reads, writes, track=track)

    breg = {}
    wq_rows = [wq_s[q].rearrange("e p a b -> (e p) (a b)") for q in range(NQ)]
    wd_rows = [wd_s[d_].rearrange("e p a b -> (e p) (a b)") for d_ in range(ND)]
    bg_rows = bguT_d.rearrange("e p a -> (e p) a")
    def xb_load(blk):
        bi = blk % 2
        dma("sp", xb[bi][:], xs_s[blk * BR:(blk + 1) * BR, :].rearrange("(s p) d -> p s d", p=128),
            [], [("xb", bi)], xb_tr[bi])

    xb_load(0)
    for blk in range(NBLK):
        bi = blk % 2
        for q in range(NQ):
            dyn_load(wq[q].rearrange("p a b -> p (a b)"), wq_rows[q], blk, ["widx", "wq_s"], [("wq", q)], wq_tr[q])
        for d_ in range(ND):
            dyn_load(wd[d_].rearrange("p a b -> p (a b)"), wd_rows[d_], blk, ["widx", "wd_s"], [("wd", d_)], wd_tr[d_])
        dyn_load(bg[0][:], bg_rows, blk, ["widx"], [("bg", 0)], bg_tr[0])
        for k in range(KC):
            pt, ptk = next_pst()
            for s_ in range(SB):
                tr(pt[:, s_ * 128:(s_ + 1) * 128], xb[bi][:, s_, k * 128:(k + 1) * 128], ident_bf[:],
                   [("xb", bi), "ident_bf"], [ptk])
            cp("act" if k % 2 == 0 else "dve", XT[bi][:, k, :], pt[:, :BR], [ptk], [("XT", bi)])
        if blk + 1 < NBLK:
            xb_load(blk + 1)
        aT = actT[bi]
        for c in range(CE):
            mi = c % 2
            G_, S_, U_ = mt["g"][mi], mt["s"][mi], mt["u"][mi]
            gk, sk, uk = ("mg", mi), ("ms", mi), ("mu", mi)
            gcol = c * 128
            ucol = DE + c * 128
            pgm, pgmk = next_ps()
            for k in range(KC):
                mm(pgm[:, :BR], wq[gcol // QW][:, k, gcol % QW:gcol % QW + 128], XT[bi][:, k, :],
                   k == 0, k == KC - 1, [("wq", gcol // QW), ("XT", bi)], [pgmk])
            pum, pumk = next_ps()
            for k in range(KC):
                mm(pum[:, :BR], wq[ucol // QW][:, k, ucol % QW:ucol % QW + 128], XT[bi][:, k, :],
                   k == 0, k == KC - 1, [("wq", ucol // QW), ("XT", bi)], [pumk])
            ts("dve", G_[:, :], pgm[:, :BR], bg[0][:, c:c + 1], LIMIT, ALU.add, ALU.min, [pgmk, ("bg", 0)], [gk])
            act(S_[:, :], G_[:, :], AF.Sigmoid, [gk], [sk], scale=ALPHA)
            ts("dve", U_[:, :], pum[:, :BR], bg[0][:, CE + c:CE + c + 1], LIMIT, ALU.add, ALU.min,
               [pumk, ("bg", 0)], [uk])
            ts("dve", U_[:, :], U_[:, :], -LIMIT, 1.0, ALU.max, ALU.add, [uk], [uk])
            tt("dve", S_[:, :], S_[:, :], G_[:, :], ALU.mult, [sk, gk], [sk])
            tt("dve", aT[:, c, :], U_[:, :], S_[:, :], ALU.mult, [uk, sk], [("actT", bi)])
        for s_ in range(SB):
            for cb in range(NCB):
                pd, pdk = next_ps()
                for k in range(CE):
                    mm(pd[:, :CB], aT[:, k, s_ * 128:(s_ + 1) * 128], wd[cb][:, k, :], k == 0, k == CE - 1,
                       [("actT", bi), ("wd", cb)], [pdk])
                cp("act", Yt[bi][:, s_, cb * CB:(cb + 1) * CB], pd[:, :CB], [pdk], [("Yt", bi)])
        dma("sp", ys_s[blk * BR:(blk + 1) * BR, :].rearrange("(s p) d -> p s d", p=128), Yt[bi][:],
            [("Yt", bi)], [("ys_s", blk)], y_tr[bi])

    P.barrier()
    A32.reset()
    A16.reset()
    junk = ar("junk2", [128, D])
    wtsT = sb("wtsT", [NE, 128])
    Gt = [[ar("G%d_%d" % (i, k4), [128, D]) for k4 in range(TOPK)] for i in range(2)]
    g_tr = [[P.dma_track("g%d_%d" % (i, k4)) for k4 in range(TOPK)] for i in range(2)]
    acc = [ar("acc%d" % i, [128, D]) for i in range(2)]
    x1t = [ar("x1t%d" % i, [128, D]) for i in range(2)]
    x1_tr = [P.dma_track("x1t%d" % i) for i in range(2)]
    ot_tr = [P.dma_track("ot%d" % i) for i in range(2)]
    for i in range(NSUBS):
        xi = i % 2
        r0 = i * 128
        b = r0 // SEQ
        dma("sp", x1t[xi][:], x1_s[r0:r0 + 128, :], ["x1_s"], [("x1t", xi)], x1_tr[xi])
        for k4 in range(TOPK):
            P.op("pool", lambda e, xi=xi, i=i, k4=k4: e.indirect_dma_start(
                out=Gt[xi][k4][:, :], out_offset=None, in_=ys_s[:, :],
                in_offset=bass.IndirectOffsetOnAxis(ap=d4i[:, i, k4:k4 + 1], axis=0)),
                ["ys_s", "d4i"], [("G", xi, k4)], track=g_tr[xi][k4])
        pw, pwk = next_ps()
        tr(pw[:NE, :128], wts[:, i, :], ident32[:], ["wts", "ident32"], [pwk])
        cp("dve", wtsT[:, :], pw[:NE, :128], [pwk], ["wtsT"])
        for cb in range(NCB):
            pb, pbk = next_ps()
            mm(pb[:, :CB], wtsT[:, :], bdn[:, cb * CB:(cb + 1) * CB], True, True, ["wtsT", "c_bdn"], [pbk])
            cp("act", acc[xi][:, cb * CB:(cb + 1) * CB], pb[:, :CB], [pbk], [("acc", xi)])
        for k4 in range(TOPK):
            stt(acc[xi][:], Gt[xi][k4][:], w4[:, i, k4:k4 + 1], acc[xi][:], ALU.mult, ALU.add,
                [("G", xi, k4), "w4", ("acc", xi)], [("acc", xi)])
        tt("pool", acc[xi][:], acc[xi][:], gtb[:, 1, b, :], ALU.mult, [("acc", xi), "gtb"], [("acc", xi)])
        tt("dve", x1t[xi][:], x1t[xi][:], acc[xi][:], ALU.add, [("x1t", xi), ("acc", xi)], [("x1t", xi)])
        act(junk[:], x1t[xi][:], AF.Square, [("x1t", xi)], ["junk"], accum_out=ssq[:, 0:1])
        P.last_write[("ssq", 0)] = P.last_write["junk"]
        P.readers[("ssq", 0)] = []
        act(rstd[:, 0:1], ssq[:, 0:1], AF.Sqrt, [("ssq", 0)], [("rstd", 0)], scale=1.0 / D, bias=EPS)
        P.op("dve", lambda e: e.reciprocal(rstd[:, 0:1], rstd[:, 0:1]), [("rstd", 0)], [("rstd", 0)])
        stt(x1t[xi][:], x1t[xi][:], rstd[:, 0:1], fgb[:], ALU.mult, ALU.mult,
            [("x1t", xi), ("rstd", 0), "c_fgb"], [("x1t", xi)])
        dma("sp", out_d[r0:r0 + 128, :], x1t[xi][:], [("x1t", xi)], [("out", i)], ot_tr[xi])
    P.wait_all("sp", ot_tr)
    print("[build] sbuf bytes remaining/partition:", nc.sbuf_bytes_remaining, "A32 hi", A32.hi * 4, "A16 hi", A16.hi * 2,
          "n_ops", sum(len(v) for v in P.issue.values()), flush=True)
    P.emit(st)
    st.close()
    return nc


def _layout(inputs, cfg):
    D, DE, NE, SEQ, NSEQ, NCORES = cfg["D"], cfg["DE"], cfg["NE"], cfg["SEQ"], cfg["NSEQ"], cfg["NCORES"]
    KC, CE = D // 128, DE // 128
    f = lambda a: np.ascontiguousarray(np.asarray(a, dtype=np.float32))
    g = {k: np.asarray(v) for k, v in inputs.items()}

    def fm(v):
        return f(v.reshape(-1, 128).T)

    def km(w):
        return f(w.reshape(-1, 128, w.shape[-1]).transpose(1, 0, 2))

    def bc(v):
        return f(np.broadcast_to(v[None, :], (128, v.shape[0])))
    shared = {}
    shared["ada_w"] = km(g["ada_w"][0])
    ada_b = g["ada_b"][0]
    shared["ada_bT"] = fm(ada_b)
    shared["ada_bg"] = f(np.stack([np.broadcast_to(ada_b[2 * D:3 * D], (128, D)),
                                   np.broadcast_to(ada_b[5 * D:6 * D], (128, D))], axis=1))
    shared["g1T"] = fm(g["norm1_g"][0])
    shared["g2T"] = fm(g["norm2_g"][0])
    shared["w_in"] = km(g["w_in"][0])
    shared["conv_wT"] = f(g["conv_w"][0].reshape(4, KC, 128).transpose(2, 1, 0))
    shared["conv_bT"] = fm(g["conv_b"][0])
    shared["lru_wa"] = f(g["lru_wa"][0].reshape(KC, 2, 64, 64))
    shared["lru_wx"] = f(g["lru_wx"][0].reshape(KC, 2, 64, 64))
    shared["lru_baT"] = fm(g["lru_ba"][0])
    shared["lru_bxT"] = fm(g["lru_bx"][0])
    shared["lamT"] = fm(g["lru_lam"][0])
    shared["ln_g_b"] = bc(g["sg_ln_g"][0])
    shared["ln_b_b"] = bc(g["sg_ln_b"][0])
    shared["sg_wsT"] = f(g["sg_ws"][0].transpose(2, 0, 1))
    shared["sg_bs_b"] = f(np.broadcast_to(g["sg_bs"][0][None], (128, KC, 128)))
    shared["w_br"] = f(np.stack([km(g["w_br_rnn"][0]), km(g["w_br_sg"][0]), km(g["w_out"][0])]))
    shared["w_router"] = km(g["w_router"][0])
    shared["b_router_b"] = bc(g["b_router"][0])
    shared["w_gu"] = f(g["w_gu"][0].reshape(NE, KC, 128, 2 * DE).transpose(0, 2, 1, 3))
    shared["b_guT"] = f(g["b_gu"][0].reshape(NE, 2 * CE, 128).transpose(0, 2, 1))
    shared["w_down"] = f(g["w_down"][0].reshape(NE, CE, 128, D).transpose(0, 2, 1, 3))
    shared["b_down"] = f(g["b_down"][0])
    shared["final_g_b"] = bc(g["final_g"])
    x = g["x"].reshape(NCORES, NSEQ * SEQ, D)
    c = g["c"].reshape(NCORES, NSEQ, KC, 128)
    maps = []
    for i in range(NCORES):
        m = dict(shared)
        m["x"] = f(x[i])
        m["cT"] = f(c[i].transpose(2, 1, 0))
        maps.append(m)
    return maps


_NC_CACHE = {}


def run(inputs, cfg):
    key = tuple(sorted(cfg.items()))
    if key not in _NC_CACHE:
        _NC_CACHE[key] = build_nc(cfg)
    nc = _NC_CACHE[key]
    maps = _layout(inputs, cfg)
    res = run_bass_kernel_spmd(nc, maps, core_ids=list(range(cfg["NCORES"])))
    if cfg.get("DEBUG"):
        global DBG
        DBG = res.results
    out = np.stack([r["out"] for r in res.results], axis=0)
    B = cfg["NCORES"] * cfg["NSEQ"]
    return out.reshape(B, cfg["SEQ"], cfg["D"]).astype(np.float32)


def kernel(**inputs):
    return run(inputs, CFG_FULL)
```

```python
from contextlib import ExitStack
import numpy as np
import concourse.bass as bass
import concourse.mybir as mybir
from concourse.bass_utils import run_bass_kernel_spmd

F32 = mybir.dt.float32
BF16 = mybir.dt.bfloat16
I32 = mybir.dt.int32
AF = mybir.ActivationFunctionType
ALU = mybir.AluOpType
ENGS = ("pe", "act", "dve", "pool", "sp")

CFG_FULL = dict(D=1024, DE=1024, NE=32, SEQ=4096, NSEQ=2, NCORES=8)
EPS = 1e-6
LIMIT = 7.0
ALPHA = 1.702
TOPK = 4
USE_GELU_TANH_LUT = False
BR = 256
USE_COND_SKIP = True


class Prog:
    def __init__(self, nc):
        self.nc = nc
        self.tracks = {}
        self.issue = {e: [] for e in ENGS}
        self.last_write = {}
        self.readers = {}
        self.waited = {e: {} for e in ENGS}
        self.n_dma_tracks = 0

    def dma_track(self, name=""):
        self.n_dma_tracks += 1
        return "dma:%d:%s" % (self.n_dma_tracks, name)

    def op(self, eng, fn, reads=(), writes=(), track=None):
        track = track or eng
        tl = self.tracks.setdefault(track, [])
        idx = len(tl)
        deps = {}

        def add(d):
            t, i = d
            if t == track and t.startswith("dma:"):
                return
            if deps.get(t, -1) < i:
                deps[t] = i
        for k in reads:
            lw = self.last_write.get(k)
            if lw is not None:
                add(lw)
        for k in writes:
            lw = self.last_write.get(k)
            if lw is not None and lw[0] != track:
                add(lw)
            for r in self.readers.get(k, ()):
                if r[0] != track:
                    add(r)
        waits = []
        wd = self.waited[eng]
        for t, i in deps.items():
            if t.startswith("dma:"):
                i = len(self.tracks[t]) - 1
            if wd.get(t, -1) >= i:
                continue
            if t == eng and eng == "pe":
                continue
            wd[t] = i
            self.tracks[t][i]["need"] = True
            waits.append((t, i))
        rec = dict(eng=eng, track=track, fn=fn, waits=waits, need=track.startswith("dma:"))
        tl.append(rec)
        self.issue[eng].append(rec)
        for k in reads:
            self.readers.setdefault(k, []).append((track, idx))
        for k in writes:
            self.last_write[k] = (track, idx)
            self.readers[k] = []
        return rec

    def wait_all(self, eng, tracks):
        waits = []
        for t in tracks:
            tl = self.tracks.get(t)
            if not tl:
                continue
            i = len(tl) - 1
            tl[i]["need"] = True
            waits.append((t, i))
        self.issue[eng].append(dict(eng=eng, track=eng, fn=None, waits=waits, need=False))

    def barrier(self):
        tr = list(self.tracks.keys())
        for e in ENGS:
            self.wait_all(e, tr)
            for t in tr:
                self.waited[e][t] = len(self.tracks[t]) - 1

    def emit(self, stack):
        nc = self.nc
        sems, cum = {}, {}
        for n, (t, tl) in enumerate(self.tracks.items()):
            sems[t] = stack.enter_context(nc.semaphore("s%d" % n))
            c, step, arr = 0, (16 if t.startswith("dma:") else 1), []
            for rec in tl:
                if rec["need"]:
                    c += step
                arr.append(c)
            cum[t] = arr
        block = stack.enter_context(nc.Block())
        engobj = {"pe": "tensor", "act": "scalar", "dve": "vector", "pool": "gpsimd", "sp": "sync"}

        def make(engname):
            recs = self.issue[engname]

            def body(e):
                for rec in recs:
                    for (t, i) in rec["waits"]:
                        e.wait_ge(sems[t], cum[t][i])
                    if rec["fn"] is None:
                        continue
                    ins = rec["fn"](e)
                    if rec["need"]:
                        t = rec["track"]
                        ins.then_inc(sems[t], 16 if t.startswith("dma:") else 1)
            return body
        for engname in ENGS:
            if self.issue[engname]:
                getattr(block, engobj[engname])(make(engname))


class Arena:
    def __init__(self, nc, st, name, dt, nelem):
        self.t = st.enter_context(nc.sbuf_tensor(name, [128, nelem], dt))
        self.n, self.off, self.hi = nelem, 0, 0

    def reset(self):
        self.off = 0

    def alloc(self, shape):
        n = 1
        for d in shape[1:]:
            n *= d
        o = self.off
        self.off += n
        self.hi = max(self.hi, self.off)
        assert self.off <= self.n, ("arena overflow", self.off, self.n)
        ap = self.t[:shape[0], o:o + n]
        if len(shape) == 3:
            ap = ap.rearrange("p (a b) -> p a b", a=shape[1])
        elif len(shape) == 4:
            ap = ap.rearrange("p (a b c) -> p a b c", a=shape[1], b=shape[2])
        return ap


def build_nc(cfg):
    D, DE, NE, SEQ, NSEQ = cfg["D"], cfg["DE"], cfg["NE"], cfg["SEQ"], cfg["NSEQ"]
    KC, CE = D // 128, DE // 128
    NTOK = NSEQ * SEQ
    T1 = 256
    S1 = T1 // 128
    NT1 = SEQ // T1
    T2 = 512
    G2 = min(1024, NTOK)
    NG = NTOK // G2
    CB = min(512, D)
    NCB = D // CB
    D6 = 6 * D
    NSUBS = NTOK // 128
    KCM = max(KC, CE)
    PW = 256

    nc = bass.Bass("TRN2", target_bir_lowering=False)
    st = ExitStack()
    P = Prog(nc)

    def din(name, shape, dt=F32):
        return nc.dram_tensor(name, list(shape), dt, kind="ExternalInput").ap()

    def dscr(name, shape, dt):
        return nc.dram_tensor(name, list(shape), dt, kind="Internal").ap()

    x_d = din("x", [NTOK, D])
    cT_d = din("cT", [128, KC, NSEQ])
    adaw_d = din("ada_w", [128, KC, D6])
    adabT_d = din("ada_bT", [128, 6 * KC])
    adabg_d = din("ada_bg", [128, 2, D])
    g1T_d = din("g1T", [128, KC])
    g2T_d = din("g2T", [128, KC])
    win_d = din("w_in", [128, KC, D6])
    convwT_d = din("conv_wT", [128, KC, 4])
    convbT_d = din("conv_bT", [128, KC])
    lruwa_d = din("lru_wa", [KC, 2, 64, 64])
    lruwx_d = din("lru_wx", [KC, 2, 64, 64])
    baT_d = din("lru_baT", [128, KC])
    bxT_d = din("lru_bxT", [128, KC])
    lamT_d = din("lamT", [128, KC])
    lng_d = din("ln_g_b", [128, D])
    lnb_d = din("ln_b_b", [128, D])
    wsT_d = din("sg_wsT", [128, KC, 128])
    bsb_d = din("sg_bs_b", [128, KC, 128])
    wbr_d = din("w_br", [3, 128, KC, D])
    wr_d = din("w_router", [128, KC, NE])
    brb_d = din("b_router_b", [128, NE])
    wgu_d = din("w_gu", [NE, 128, KC, 2 * DE])
    bguT_d = din("b_guT", [NE, 128, 2 * CE])
    wdn_d = din("w_down", [NE, 128, CE, D])
    bdn_d = din("b_down", [NE, D])
    fgb_d = din("final_g_b", [128, D])
    out_d = nc.dram_tensor("out", [NTOK, D], F32, kind="ExternalOutput").ap()

    wmix_s = dscr("wmix_s", [9, 128, KC, D], BF16)
    QW = min(512, 2 * DE)
    NQ = (2 * DE) // QW
    ND = NCB
    NBLK = (NTOK * TOPK) // BR + NE
    SB = BR // 128
    wq_s = [dscr("wq_s%d" % q, [NE, 128, KC, QW], BF16) for q in range(NQ)]
    wd_s = [dscr("wd_s%d" % d_, [NE, 128, CE, CB], BF16) for d_ in range(ND)]
    x1_s = dscr("x1_s", [NTOK, D], F32)
    h2tm_s = dscr("h2tm_s", [NTOK, D], BF16)
    xs_s = dscr("xs_s", [NBLK * BR, D], BF16)
    ys_s = dscr("ys_s", [NBLK * BR, D], F32)

    def sb(name, shape, dt=F32):
        return st.enter_context(nc.sbuf_tensor(name, list(shape), dt))

    A32 = Arena(nc, st, "arena32", F32, cfg.get("A32", 15104))
    A16 = Arena(nc, st, "arena16", BF16, cfg.get("A16", 40960))

    def ar(name, shape, dt=F32):
        return (A32 if dt == F32 else A16).alloc(list(shape))

    NPS = 5
    ps = [st.enter_context(nc.psum_tensor("ps%d" % i, [128, 512], F32)) for i in range(NPS)]
    pst = [st.enter_context(nc.psum_tensor("pst%d" % i, [128, 512], BF16)) for i in range(2)]
    psm = st.enter_context(nc.psum_tensor("psm", [128, 512], F32))
    psmk = "psm"
    psc = [0]
    pstc = [0]

    def next_ps():
        i = psc[0] % NPS
        psc[0] += 1
        return ps[i], ("ps", i)

    def next_pst():
        i = pstc[0] % 2
        pstc[0] += 1
        return pst[i], ("pst", i)

    def mm(out, lhsT, rhs, start, stop, reads, writes):
        P.op("pe", lambda e: e.matmul(out, lhsT, rhs, start=start, stop=stop), reads, writes)

    def tr(out, in_, ident, reads, writes):
        P.op("pe", lambda e: e.transpose(out, in_, ident), reads, writes)

    def act(out, in_, func, reads, writes, bias=None, scale=None, accum_out=None, eng="act"):
        kw = {}
        if bias is not None:
            kw["bias"] = bias
        if scale is not None:
            kw["scale"] = scale
        if accum_out is not None:
            kw["accum_out"] = accum_out
        P.op("act", lambda e: e.activation(out, in_, func, **kw), reads, writes)

    def ts(eng, out, in0, s1, s2, op0, op1, reads, writes):
        if op1 is None:
            P.op(eng, lambda e: e.tensor_scalar(out, in0, s1, None, op0), reads, writes)
        else:
            P.op(eng, lambda e: e.tensor_scalar(out, in0, s1, s2, op0, op1), reads, writes)

    def tt(eng, out, in0, in1, op, reads, writes):
        P.op(eng, lambda e: e.tensor_tensor(out, in0, in1, op), reads, writes)

    def stt(out, in0, scalar, in1, op0, op1, reads, writes):
        P.op("dve", lambda e: e.scalar_tensor_tensor(out, in0, scalar, in1, op0, op1), reads, writes)

    def cp(eng, out, in_, reads, writes):
        if eng == "act":
            P.op("act", lambda e: e.copy(out, in_), reads, writes)
        else:
            P.op(eng, lambda e: e.tensor_copy(out, in_), reads, writes)

    def dma(q, out, in_, reads, writes, track):
        P.op(q, lambda e: e.dma_start(out=out, in_=in_), reads, writes, track=track)

    ctrack = P.dma_track("const")
    consts = {}

    def cload(name, dram, shape, dt=F32, q="sp", arena=False):
        t = ar("c_" + name, shape, dt) if arena else sb("c_" + name, shape, dt)
        dma(q, t[:], dram, [], ["c_" + name], ctrack)
        consts[name] = t
        return t

    cT = cload("cT", cT_d, [128, KC, NSEQ])
    adabT = cload("adabT", adabT_d, [128, 6 * KC])
    adabg = cload("adabg", adabg_d, [128, 2, D], arena=True)
    g1T = cload("g1T", g1T_d, [128, KC])
    g2T = cload("g2T", g2T_d, [128, KC])
    convwT = cload("convwT", convwT_d, [128, KC, 4])
    convbT = cload("convbT", convbT_d, [128, KC])
    baT = cload("baT", baT_d, [128, KC])
    bxT = cload("bxT", bxT_d, [128, KC])
    lamT = cload("lamT", lamT_d, [128, KC])
    lng = cload("lng", lng_d, [128, D])
    lnb = cload("lnb", lnb_d, [128, D])
    wsT32 = cload("wsT32", wsT_d, [128, KC, 128], arena=True)
    bsb = cload("bsb", bsb_d, [128, KC, 128])
    wr32 = cload("wr32", wr_d, [128, KC, NE], arena=True)
    brb = cload("brb", brb_d, [128, NE])
    bdn = cload("bdn", bdn_d, [NE, D])
    fgb = cload("fgb", fgb_d, [128, D])
    wbd32 = ar("wbd32", [128, 2, KC, 128])
    P.op("pool", lambda e: e.memset(wbd32[:], 0.0), [], ["c_wbd32"])
    for gi, src in enumerate((lruwa_d, lruwx_d)):
        for half in range(2):
            dma("sp", wbd32[half * 64:(half + 1) * 64, gi, :, half * 64:(half + 1) * 64],
                src[:, half].rearrange("k i j -> i k j"), [], ["c_wbd32"], ctrack)

    ident_bf = sb("ident_bf", [128, 128], BF16)
    ident32 = sb("ident32", [128, 128], F32)
    ones32 = ar("ones32", [128, 128], F32)
    P.op("pool", lambda e: e.memset(ones32[:], 1.0), [], ["ones32"])
    P.op("pool", lambda e: e.affine_select(out=ident32[:], in_=ones32[:], pattern=[[-1, 128]],
                                           compare_op=ALU.is_equal, fill=0.0, base=0, channel_multiplier=1),
         ["ones32"], ["ident32"])
    cp("dve", ident_bf[:], ident32[:], ["ident32"], ["ident_bf"])

    wbd = sb("wbd", [128, 2, KC, 128], BF16)
    cp("dve", wbd[:], wbd32[:], ["c_wbd32"], ["wbd"])
    wsT = sb("wsT", [128, KC, 128], BF16)
    wsTm = ar("wsTm", [128, KC, 128], F32)
    P.op("pool", lambda e: e.affine_select(out=wsTm[:], in_=wsT32[:], pattern=[[0, KC], [1, 128]],
                                           compare_op=ALU.is_ge, fill=0.0, base=0, channel_multiplier=-1),
         ["c_wsT32"], ["wsTm"])
    cp("dve", wsT[:], wsTm[:], ["wsTm"], ["wsT"])
    wr = sb("wr", [128, KC, NE], BF16)
    cp("dve", wr[:], wr32[:], ["c_wr32"], ["wr"])

    kneg = sb("kneg", [128, KC])
    k2 = sb("k2", [128, KC])
    ktmp = sb("ktmp", [128, KC])
    act(ktmp[:], lamT[:], AF.Exp, ["c_lamT"], ["ktmp"], scale=-1.0)
    act(ktmp[:], ktmp[:], AF.Ln, ["ktmp"], ["ktmp"], bias=1.0)
    ts("dve", kneg[:], ktmp[:], -8.0, None, ALU.mult, None, ["ktmp"], ["kneg"])
    ts("dve", k2[:], ktmp[:], -16.0, None, ALU.mult, None, ["ktmp"], ["k2"])

    sc = sb("sc", [128, KC, NSEQ])
    sgt = sb("sgt", [128, KC, NSEQ])
    act(sgt[:], cT[:], AF.Sigmoid, ["c_cT"], ["sgt"])
    tt("dve", sc[:], sgt[:], cT[:], ALU.mult, ["sgt", "c_cT"], ["sc"])
    screp = ar("screp", [128, NSEQ, KC, 128])
    for b in range(NSEQ):
        for k in range(KC):
            cp("dve", screp[:, b, k, :], sc[:, k, b:b + 1].to_broadcast([128, 128]), ["sc"], ["screp"])
    modT = sb("modT", [128, NSEQ, 6 * KC])
    gtb = sb("gtb", [128, 2, NSEQ, D])
    stg = [ar("stg%d" % i, [128, KCM, PW]) for i in range(2)]
    stg_tr = [P.dma_track("stg%d" % i) for i in range(2)]
    stgc = [0]

    def stage_load(dram_ap, q="sp"):
        i = stgc[0] % 2
        stgc[0] += 1
        dma(q, stg[i][:, :dram_ap.shape[1], :dram_ap.shape[2]], dram_ap, [], [("stg", i)], stg_tr[i])
        return stg[i], ("stg", i)

    NJB = D6 // PW
    for jb in range(NJB):
        s_t, s_k = stage_load(adaw_d[:, :, jb * PW:(jb + 1) * PW])
        for jj in range(PW // 128):
            j = jb * (PW // 128) + jj
            for b in range(NSEQ):
                for k in range(KC):
                    col = b * 6 * KC + j
                    mm(psm[:, col:col + 1], s_t[:, k, jj * 128:(jj + 1) * 128], sc[:, k, b:b + 1],
                       k == 0, k == KC - 1, [s_k, "sc"], [psmk])
        m = (jb * PW) // D
        if m in (2, 5):
            which = 0 if m == 2 else 1
            c0 = (jb * PW) % D
            for b in range(NSEQ):
                pg, pgk = next_ps()
                for k in range(KC):
                    mm(pg[:, :PW], screp[:, b, k, :], s_t[:, k, :], k == 0, k == KC - 1, [s_k, "screp"], [pgk])
                tt("dve", gtb[:, which, b, c0:c0 + PW], pg[:, :PW], adabg[:, which, c0:c0 + PW], ALU.add,
                   [pgk, "c_adabg"], ["gtb"])
    for b in range(NSEQ):
        tt("dve", modT[:, b, :], psm[:, b * 6 * KC:(b + 1) * 6 * KC], adabT[:], ALU.add,
           [psmk, "c_adabT"], ["modT"])
    s1 = sb("s1", [128, NSEQ, KC])
    s2 = sb("s2", [128, NSEQ, KC])
    for b in range(NSEQ):
        stt(s1[:, b, :], modT[:, b, KC:2 * KC], 1.0, g1T[:], ALU.add, ALU.mult, ["modT", "c_g1T"], ["s1"])
        stt(s2[:, b, :], modT[:, b, 4 * KC:5 * KC], 1.0, g2T[:], ALU.add, ALU.mult, ["modT", "c_g2T"], ["s2"])

    cbf = [ar("cbf%d" % i, [128, KCM, PW], BF16) for i in range(2)]
    cbf_tr = [P.dma_track("cbf%d" % i) for i in range(2)]
    cbc = [0]

    def precast(src_ap, dst_ap, dst_key):
        kc, w = src_ap.shape[1], src_ap.shape[2]
        s_t, s_k = stage_load(src_ap)
        i = cbc[0] % 2
        cbc[0] += 1
        cp("act", cbf[i][:, :kc, :w], s_t[:, :kc, :w], [s_k], [("cbf", i)])
        dma("act", dst_ap, cbf[i][:, :kc, :w], [("cbf", i)], [dst_key], cbf_tr[i])

    for blk in range(9):
        for c0 in range(0, D, PW):
            w = min(PW, D - c0)
            src = win_d[:, :, blk * D + c0: blk * D + c0 + w] if blk < 6 else wbr_d[blk - 6][:, :, c0:c0 + w]
            precast(src, wmix_s[blk][:, :, c0:c0 + w], ("wmix", blk, c0))
    for e_ in range(NE):
        for c0 in range(0, 2 * DE, PW):
            w = min(PW, 2 * DE - c0)
            precast(wgu_d[e_][:, :, c0:c0 + w], wq_s[c0 // QW][e_][:, :, c0 % QW:c0 % QW + w], ("wq_s", e_, c0))
        for c0 in range(0, D, PW):
            w = min(PW, D - c0)
            precast(wdn_d[e_][:, :, c0:c0 + w], wd_s[c0 // CB][e_][:, :, c0 % CB:c0 % CB + w], ("wd_s", e_, c0))

    P.barrier()
    A32.reset()
    A16.reset()
    wts = sb("wts", [128, NSUBS, NE])
    hist = sb("hist", [128, KC, 3])
    hstate = sb("hstate", [128, KC])
    RING = 3
    wring = [ar("wring%d" % i, [128, KC, D], BF16) for i in range(RING)]
    wring_tr = [P.dma_track("wring%d" % i) for i in range(RING)]
    wrc = [0]

    def wload(blk):
        i = wrc[0] % RING
        wrc[0] += 1
        dma("sp", wring[i][:], wmix_s[blk], [("wmix", blk, c0) for c0 in range(0, D, PW)], [("wring", i)], wring_tr[i])
        return wring[i], ("wring", i)

    X = ar("X", [128, S1, D])
    x_tr = P.dma_track("x")
    xn = ar("xn", [128, S1, D], BF16)
    hT = ar("hT", [128, KC, T1], BF16)
    yrT = ar("yrT", [128, KC, T1], BF16)
    ysT = ar("ysT", [128, KC, T1], BF16)
    mT = ar("mT", [128, KC, T1], BF16)
    tmpR = ar("tmpR", [128, KC, T1])
    junk = ar("junk", [128, D])
    ssq = sb("ssq", [128, 4])
    rstd = sb("rstd", [128, 4])
    NB = 2
    tmp = {n: [ar("t_%s%d" % (n, i), [128, T1 + (4 if n == "rx" else 0)]) for i in range(NB)]
           for n in ("rx", "cv", "r", "i", "a", "m", "u", "hs", "g", "g2", "q")}
    cvb = [ar("cvb%d" % i, [128, T1], BF16) for i in range(NB)]
    vtm = [ar("vtm%d" % i, [128, D]) for i in range(2)]
    vt2 = [ar("vt2%d" % i, [128, D]) for i in range(2)]
    bnst = sb("bnst", [128, 2 * max(1, D // 512), 6])
    mv = sb("mv", [128, 2])
    x1st_tr = P.dma_track("x1st")
    h2st_tr = P.dma_track("h2st")
    h2tm = ar("h2tm", [128, S1, D], BF16)
    h2T_o = ar("h2T_o", [128, KC, T1], BF16)
    lg = sb("lg", [128, NE])
    top8 = sb("top8", [128, 8])
    negmx = sb("negmx", [128, 1])
    msk = sb("msk", [128, NE])
    ex = sb("ex", [128, NE])
    den = sb("den", [128, 1])

    def gelu(eng_alt, out, in_, n, reads, writes, tg, tgk):
        if USE_GELU_TANH_LUT:
            act(out, in_, AF.Gelu_apprx_tanh, reads, writes)
            return
        act(tg, in_, AF.Square, reads, [tgk])
        ts("dve", tg, tg, 0.044715, 1.0, ALU.mult, ALU.add, [tgk], [tgk])
        tt("dve", tg, tg, in_, ALU.mult, [tgk] + list(reads), [tgk])
        act(tg, tg, AF.Sigmoid, [tgk], [tgk], scale=1.5957691216057308)
        tt("dve", out, tg, in_, ALU.mult, [tgk] + list(reads), writes)

    def rmsnorm_to_T(src, src_key, nsub, dstT, dstT_key, scale_ap_fn, bias_ap_fn, sk_reads):
        for s in range(nsub):
            act(junk[:], src[:, s, :], AF.Square, [src_key], ["junk"], accum_out=ssq[:, s:s + 1])
            P.issue
            P.last_write[("ssq", s)] = P.last_write["junk"]
            P.readers[("ssq", s)] = []
        for s in range(nsub):
            act(rstd[:, s:s + 1], ssq[:, s:s + 1], AF.Sqrt, [("ssq", s)], [("rstd", s)], scale=1.0 / D, bias=EPS)
            P.op("dve", lambda e, s=s: e.reciprocal(rstd[:, s:s + 1], rstd[:, s:s + 1]), [("rstd", s)], [("rstd", s)])
            ts("dve", xn[:, s, :], src[:, s, :], rstd[:, s:s + 1], None, ALU.mult, None,
               [src_key, ("rstd", s)], [("xn", s)])
        for k in range(KC):
            pt, ptk = next_pst()
            for s in range(nsub):
                tr(pt[:, s * 128:(s + 1) * 128], xn[:, s, k * 128:(k + 1) * 128], ident_bf[:],
                   [("xn", s), "ident_bf"], [ptk])
            act(dstT[:, k, :nsub * 128], pt[:, :nsub * 128], AF.Identity, [ptk] + sk_reads, [dstT_key],
                scale=scale_ap_fn(k), bias=bias_ap_fn(k))

    for b in range(NSEQ):
        P.op("pool", lambda e: e.memset(hist[:], 0.0), [], ["hist"])
        P.op("pool", lambda e: e.memset(hstate[:], 0.0), [], ["hstate"])
        for j in range(NT1):
            tok0 = b * SEQ + j * T1
            dma("sp", X[:], x_d[tok0:tok0 + T1, :].rearrange("(s p) d -> p s d", p=128), [], ["X"], x_tr)
            rmsnorm_to_T(X, "X", S1, hT, "hT",
                         lambda k: s1[:, b, k:k + 1], lambda k: modT[:, b, k:k + 1], ["s1", "modT"])
            w0, w0k = wload(0)
            w1, w1k = wload(1)
            for c in range(KC):
                i_ = c % NB
                T = {n: tmp[n][i_] for n in tmp}
                K = {n: ("t", n, i_) for n in tmp}
                pz, pzk = next_ps()
                for k in range(KC):
                    mm(pz[:, :T1], w0[:, k, c * 128:(c + 1) * 128], hT[:, k, :], k == 0, k == KC - 1,
                       [w0k, "hT"], [pzk])
                cp("act", T["rx"][:, 0:3], hist[:, c, :], ["hist"], [K["rx"]])
                cp("act", T["rx"][:, 3:3 + T1], pz[:, :T1], [pzk], [K["rx"]])
                cp("act", hist[:, c, :], T["rx"][:, T1:T1 + 3], [K["rx"]], ["hist"])
                ts("dve", T["cv"][:, :], T["rx"][:, 0:T1], convwT[:, c, 0:1], convbT[:, c:c + 1], ALU.mult, ALU.add,
                   [K["rx"], "c_convwT", "c_convbT"], [K["cv"]])
                for kk in range(1, 4):
                    stt(T["cv"][:, :], T["rx"][:, kk:kk + T1], convwT[:, c, kk:kk + 1], T["cv"][:, :],
                        ALU.mult, ALU.add, [K["rx"], K["cv"], "c_convwT"], [K["cv"]])
                cp("dve", cvb[i_][:, :], T["cv"][:, :], [K["cv"]], [("cvb", i_)])
                pr, prk = next_ps()
                mm(pr[:, :T1], wbd[:, 0, c, :], cvb[i_][:, :], True, True, ["wbd", ("cvb", i_)], [prk])
                pi, pik = next_ps()
                mm(pi[:, :T1], wbd[:, 1, c, :], cvb[i_][:, :], True, True, ["wbd", ("cvb", i_)], [pik])
                act(T["r"][:, :], pr[:, :T1], AF.Sigmoid, [prk, "c_baT"], [K["r"]], bias=baT[:, c:c + 1])
                act(T["i"][:, :], pi[:, :T1], AF.Sigmoid, [pik, "c_bxT"], [K["i"]], bias=bxT[:, c:c + 1])
                act(T["a"][:, :], T["r"][:, :], AF.Exp, [K["r"], "kneg"], [K["a"]], scale=kneg[:, c:c + 1])
                act(T["m"][:, :], T["r"][:, :], AF.Exp, [K["r"], "k2"], [K["m"]], scale=k2[:, c:c + 1])
                act(T["m"][:, :], T["m"][:, :], AF.Sqrt, [K["m"]], [K["m"]], scale=-1.0, bias=1.0)
                tt("dve", T["u"][:, :], T["i"][:, :], T["cv"][:, :], ALU.mult, [K["i"], K["cv"]], [K["u"]])
                tt("dve", T["u"][:, :], T["u"][:, :], T["m"][:, :], ALU.mult, [K["u"], K["m"]], [K["u"]])
                P.op("dve", lambda e, T=T, c=c: e.tensor_tensor_scan(T["hs"][:, :], T["a"][:, :], T["u"][:, :],
                                                                       hstate[:, c:c + 1], ALU.mult, ALU.add),
                     [K["a"], K["u"], "hstate"], [K["hs"]])
                cp("dve", hstate[:, c:c + 1], T["hs"][:, T1 - 1:T1], [K["hs"]], ["hstate"])
                pg, pgk = next_ps()
                for k in range(KC):
                    mm(pg[:, :T1], w1[:, k, c * 128:(c + 1) * 128], hT[:, k, :], k == 0, k == KC - 1,
                       [w1k, "hT"], [pgk])
                gelu("dve", T["g"][:, :], pg[:, :T1], T1, [pgk], [K["g"]], T["g2"][:, :], K["g2"])
                tt("dve", yrT[:, c, :], T["hs"][:, :], T["g"][:, :], ALU.mult, [K["hs"], K["g"]], ["yrT"])
            w3, w3k = wload(3)
            for s in range(S1):
                vi = s % 2
                for cb in range(NCB):
                    pv, pvk = next_ps()
                    for k in range(KC):
                        mm(pv[:, :CB], hT[:, k, s * 128:(s + 1) * 128], w3[:, k, cb * CB:(cb + 1) * CB],
                           k == 0, k == KC - 1, [w3k, "hT"], [pvk])
                    gelu("dve", vtm[vi][:, cb * CB:(cb + 1) * CB], pv[:, :CB], CB, [pvk], [("vtm", vi)],
                         vt2[vi][:, cb * CB:(cb + 1) * CB], ("vt2", vi))
                nchunk = max(1, D // 512)
                cw = D // nchunk
                for q in range(nchunk):
                    P.op("dve", lambda e, vi=vi, q=q: e.bn_stats(bnst[:, q, :], vtm[vi][:, q * cw:(q + 1) * cw]),
                         [("vtm", vi)], ["bnst"])
                P.op("dve", lambda e: e.bn_aggr(mv[:], bnst[:, :nchunk, :].rearrange("p a b -> p (a b)")),
                     ["bnst"], ["mv"])
                act(mv[:, 1:2], mv[:, 1:2], AF.Sqrt, ["mv"], ["mv"], bias=EPS)
                P.op("dve", lambda e: e.reciprocal(mv[:, 1:2], mv[:, 1:2]), ["mv"], ["mv"])
                ts("dve", vtm[vi][:, :], vtm[vi][:, :], mv[:, 0:1], mv[:, 1:2], ALU.subtract, ALU.mult,
                   [("vtm", vi), "mv"], [("vtm", vi)])
                tt("dve", vtm[vi][:, :], vtm[vi][:, :], lng[:], ALU.mult, [("vtm", vi), "c_lng"], [("vtm", vi)])
                tt("dve", xn[:, s, :], vtm[vi][:, :], lnb[:], ALU.add, [("vtm", vi), "c_lnb"], [("xn", s)])
            w2, w2k = wload(2)
            for g in range(KC):
                i_ = g % NB
                T = {n: tmp[n][i_] for n in tmp}
                K = {n: ("t", n, i_) for n in tmp}
                pu, puk = next_ps()
                for k in range(KC):
                    mm(pu[:, :T1], w2[:, k, g * 128:(g + 1) * 128], hT[:, k, :], k == 0, k == KC - 1,
                       [w2k, "hT"], [puk])
                gelu("dve", T["g"][:, :], pu[:, :T1], T1, [puk], [K["g"]], T["g2"][:, :], K["g2"])
                psv, psvk = next_ps()
                for s in range(S1):
                    mm(psv[:, s * 128:(s + 1) * 128], xn[:, s, g * 128:(g + 1) * 128], wsT[:, g, :], True, True,
                       [("xn", s), "wsT"], [psvk])
                for s in range(S1):
                    tt("dve", T["q"][:, s * 128:(s + 1) * 128], psv[:, s * 128:(s + 1) * 128], bsb[:, g, :], ALU.add,
                       [psvk, "c_bsb"], [K["q"]])
                tt("dve", ysT[:, g, :], T["q"][:, :], T["g"][:, :], ALU.mult, [K["q"], K["g"]], ["ysT"])
            for pas, (ga, gb_, yT, yk) in enumerate(((4, 6, yrT, "yrT"), (5, 7, ysT, "ysT"))):
                wg, wgk = wload(ga)
                wb, wbk = wload(gb_)
                for oc in range(KC):
                    i_ = oc % NB
                    T = {n: tmp[n][i_] for n in tmp}
                    K = {n: ("t", n, i_) for n in tmp}
                    pgt, pgtk = next_ps()
                    for k in range(KC):
                        mm(pgt[:, :T1], wg[:, k, oc * 128:(oc + 1) * 128], hT[:, k, :], k == 0, k == KC - 1,
                           [wgk, "hT"], [pgtk])
                    act(T["g"][:, :], pgt[:, :T1], AF.Sigmoid, [pgtk], [K["g"]])
                    pbr, pbrk = next_ps()
                    for k in range(KC):
                        mm(pbr[:, :T1], wb[:, k, oc * 128:(oc + 1) * 128], yT[:, k, :], k == 0, k == KC - 1,
                           [wbk, yk], [pbrk])
                    if pas == 0:
                        tt("dve", tmpR[:, oc, :], T["g"][:, :], pbr[:, :T1], ALU.mult, [K["g"], pbrk], [("tmpR", oc)])
                    else:
                        tt("dve", T["i"][:, :], T["g"][:, :], pbr[:, :T1], ALU.mult, [K["g"], pbrk], [K["i"]])
                        tt("dve", mT[:, oc, :], tmpR[:, oc, :], T["i"][:, :], ALU.add, [("tmpR", oc), K["i"]], ["mT"])
            w8, w8k = wload(8)
            for s in range(S1):
                for cb in range(NCB):
                    po, pok = next_ps()
                    for k in range(KC):
                        mm(po[:, :CB], mT[:, k, s * 128:(s + 1) * 128], w8[:, k, cb * CB:(cb + 1) * CB],
                           k == 0, k == KC - 1, [w8k, "mT"], [pok])
                    tt("dve", junk[:, cb * CB:(cb + 1) * CB], po[:, :CB], gtb[:, 0, b, cb * CB:(cb + 1) * CB], ALU.mult,
                       [pok, "gtb"], ["junk"])
                    tt("dve", X[:, s, cb * CB:(cb + 1) * CB], X[:, s, cb * CB:(cb + 1) * CB],
                       junk[:, cb * CB:(cb + 1) * CB], ALU.add, ["X", "junk"], ["X"])
            dma("sp", x1_s[tok0:tok0 + T1, :].rearrange("(s p) d -> p s d", p=128), X[:], ["X"], ["x1_s"], x1st_tr)
            rmsnorm_to_T(X, "X", S1, h2T_o, "h2T_o",
                         lambda k: s2[:, b, k:k + 1], lambda k: modT[:, b, 3 * KC + k:3 * KC + k + 1], ["s2", "modT"])
            for s in range(S1):
                for k0 in range(0, KC, 4):
                    pt, ptk = next_pst()
                    nk = min(4, KC - k0)
                    for kk in range(nk):
                        tr(pt[:, kk * 128:(kk + 1) * 128], h2T_o[:, k0 + kk, s * 128:(s + 1) * 128], ident_bf[:],
                           ["h2T_o", "ident_bf"], [ptk])
                    cp("act", h2tm[:, s, k0 * 128:(k0 + nk) * 128], pt[:, :nk * 128], [ptk], ["h2tm"])
            dma("sp", h2tm_s[tok0:tok0 + T1, :].rearrange("(s p) d -> p s d", p=128), h2tm[:], ["h2tm"], ["h2tm_s"], h2st_tr)
            for s in range(S1):
                sub = (tok0 // 128) + s
                pl, plk = next_ps()
                for k in range(KC):
                    mm(pl[:, :NE], h2T_o[:, k, s * 128:(s + 1) * 128], wr[:, k, :], k == 0, k == KC - 1,
                       ["h2T_o", "wr"], [plk])
                tt("dve", lg[:], pl[:, :NE], brb[:], ALU.add, [plk, "c_brb"], ["lg"])
                P.op("dve", lambda e: e.max(top8[:], lg[:]), ["lg"], ["top8"])
                ts("dve", negmx[:], top8[:, 0:1], -1.0, None, ALU.mult, None, ["top8"], ["negmx"])
                ts("dve", msk[:], lg[:], top8[:, TOPK - 1:TOPK], None, ALU.is_ge, None, ["lg", "top8"], ["msk"])
                act(ex[:], lg[:], AF.Exp, ["lg", "negmx"], ["ex"], bias=negmx[:, 0:1])
                tt("dve", ex[:], ex[:], msk[:], ALU.mult, ["ex", "msk"], ["ex"])
                P.op("dve", lambda e: e.reduce_sum(den[:], ex[:], axis=mybir.AxisListType.X), ["ex"], ["den"])
                P.op("dve", lambda e: e.reciprocal(den[:], den[:]), ["den"], ["den"])
                ts("dve", wts[:, sub, :], ex[:], den[:, 0:1], None, ALU.mult, None, ["ex", "den"], ["wts"])

    P.barrier()
    A32.reset()
    A16.reset()
    LOGB = BR.bit_length() - 1
    d4f = sb("d4f", [128, NSUBS, 8])
    d4i = sb("d4i", [128, NSUBS, 4], I32)
    w4 = sb("w4", [128, NSUBS, 4])
    be_i = sb("be_i", [128, NBLK], I32)
    ld_i = sb("ld_i", [128, NBLK], I32)
    widx = sb("widx", [128, NBLK], I32)
    mask = ar("mask", [128, NSUBS, NE])
    dest = ar("dest", [128, NSUBS, NE])
    dkey = ar("dkey", [128, NSUBS, NE])
    ustr = ar("ustr", [128, 128])
    onesq = ar("onesq", [128, 128])
    macc = ar("macc", [128, NE])
    cnt = ar("cnt", [128, NE])
    padded = ar("padded", [128, NE])
    pend = ar("pend", [128, NE])
    pstart = ar("pstart", [128, NE])
    onesne = ar("onesne", [128, NE])
    eqt = ar("eqt", [128, NE])
    bst_i = A32.alloc([128, NBLK]).bitcast(I32)
    bst = ar("bst", [128, NBLK])
    bef = ar("bef", [128, NBLK])
    bet = ar("bet", [128, NBLK])
    P.op("pool", lambda e: e.memset(onesq[:], 1.0), [], ["onesq"])
    P.op("pool", lambda e: e.memset(onesne[:], 1.0), [], ["onesne"])
    P.op("pool", lambda e: e.memset(macc[:], 0.0), [], ["macc"])
    P.op("pool", lambda e: e.affine_select(out=ustr[:], in_=onesq[:], pattern=[[1, 128]], compare_op=ALU.is_ge,
                                           fill=0.0, base=-1, channel_multiplier=-1), ["onesq"], ["ustr"])
    ts("dve", mask[:], wts[:], 0.0, None, ALU.is_gt, None, ["wts"], ["mask"])
    for i in range(NSUBS):
        prk_t, prk = next_ps()
        mm(prk_t[:, :NE], ustr[:], mask[:, i, :], True, False, ["ustr", "mask"], [prk])
        mm(prk_t[:, :NE], onesq[:], macc[:], False, True, ["onesq", "macc"], [prk])
        cp("act", dest[:, i, :], prk_t[:, :NE], [prk], [("dest", i)])
        tt("dve", macc[:], macc[:], mask[:, i, :], ALU.add, ["macc", "mask"], ["macc"])
    pc_t, pck = next_ps()
    mm(pc_t[:, :NE], onesq[:], macc[:], True, True, ["onesq", "macc"], [pck])
    cp("dve", cnt[:], pc_t[:, :NE], [pck], ["cnt"])
    P.op("pool", lambda e: e.memset(padded[:], 0.0), [], ["padded"])
    for j in range(NTOK // BR):
        stt(padded[:], cnt[:], float(j * BR), padded[:], ALU.is_gt, ALU.add, ["cnt", "padded"], ["padded"])
    ts("dve", padded[:], padded[:], float(BR), None, ALU.mult, None, ["padded"], ["padded"])
    P.op("dve", lambda e: e.tensor_tensor_scan(pend[:], onesne[:], padded[:], 0.0, ALU.mult, ALU.add),
         ["onesne", "padded"], ["pend"])
    tt("dve", pstart[:], pend[:], padded[:], ALU.subtract, ["pend", "padded"], ["pstart"])
    for i in range(NSUBS):
        tt("dve", dest[:, i, :], dest[:, i, :], pstart[:], ALU.add, [("dest", i), "pstart"], [("dest", i)])
    dkeys = [("dest", i) for i in range(NSUBS)]
    stt(dkey[:], dest[:], 1.0, mask[:], ALU.add, ALU.mult, dkeys + ["mask"], ["dkey"])
    ts("dve", dkey[:], dkey[:], -1.0, None, ALU.add, None, ["dkey"], ["dkey"])
    for i in range(NSUBS):
        P.op("dve", lambda e, i=i: e.max(d4f[:, i, :], dkey[:, i, :]), ["dkey"], [("d4f", i)])
        for k4 in range(TOPK):
            ts("dve", eqt[:], dkey[:, i, :], d4f[:, i, k4:k4 + 1], None, ALU.is_equal, None, ["dkey", ("d4f", i)], ["eqt"])
            tt("dve", eqt[:], eqt[:], wts[:, i, :], ALU.mult, ["eqt", "wts"], ["eqt"])
            P.op("dve", lambda e, i=i, k4=k4: e.reduce_sum(w4[:, i, k4:k4 + 1], eqt[:], axis=mybir.AxisListType.X),
                 ["eqt"], ["w4"])
        cp("dve", d4i[:, i, :], d4f[:, i, 0:TOPK], [("d4f", i)], ["d4i"])
    P.op("pool", lambda e: e.iota(bst_i, pattern=[[BR, NBLK]], base=0, channel_multiplier=0), [], ["bst_i"])
    cp("dve", bst[:], bst_i, ["bst_i"], ["bst"])
    P.op("pool", lambda e: e.memset(bef[:], 0.0), [], ["bef"])
    for e_ in range(NE):
        ts("dve", bet[:], bst[:], pend[:, e_:e_ + 1], None, ALU.is_ge, None, ["bst", "pend"], ["bet"])
        tt("dve", bef[:], bef[:], bet[:], ALU.add, ["bef", "bet"], ["bef"])
    ts("dve", bef[:], bef[:], float(NE - 1), None, ALU.min, None, ["bef"], ["bef"])
    cp("dve", be_i[:], bef[:], ["bef"], ["be_i"])
    P.op("pool", lambda e: e.memset(bet[:], 1.0), ["bet"], ["bet"])
    if NBLK > 1:
        tt("dve", bet[:, 1:NBLK], bef[:, 1:NBLK], bef[:, 0:NBLK - 1], ALU.not_equal, ["bef", "bet"], ["bet"])
    cp("dve", ld_i[:], bet[:], ["bet"], ["ld_i"])
    pidx_i = A32.alloc([128, 1]).bitcast(I32)
    pidx = ar("pidx", [128, 1])
    widf = ar("widf", [128, NBLK])
    P.op("pool", lambda e: e.iota(pidx_i, pattern=[[0, 1]], base=0, channel_multiplier=1), [], ["pidx_i"])
    cp("dve", pidx[:], pidx_i, ["pidx_i"], ["pidx"])
    ts("dve", widf[:], bef[:], 128.0, pidx[:, 0:1], ALU.mult, ALU.add, ["bef", "pidx"], ["widf"])
    if USE_COND_SKIP:
        ts("dve", bet[:], bet[:], -1.0, -float(2 ** 30), ALU.add, ALU.mult, ["bet"], ["bet"])
        tt("dve", widf[:], widf[:], bet[:], ALU.add, ["widf", "bet"], ["widf"])
    cp("dve", widx[:], widf[:], ["widf"], ["widx"])

    if cfg.get("DEBUG"):
        dbg_tr = P.dma_track("dbg")
        MX = max(NE, NBLK)
        dd = {"dbg_d4f": (d4f, [128, NSUBS, 8], ["d4i"]), "dbg_w4": (w4, [128, NSUBS, 4], ["w4"]),
              "dbg_wts": (wts, [128, NSUBS, NE], ["wts"]), "dbg_cnt": (cnt, [128, NE], ["cnt"]),
              "dbg_pend": (pend, [128, NE], ["pend"]), "dbg_bef": (bef, [128, NBLK], ["bef"]),
              "dbg_widf": (widf, [128, NBLK], ["widx"]), "dbg_dest": (dest, [128, NSUBS, NE], ["dkey"]),
              "dbg_dkey": (dkey, [128, NSUBS, NE], ["dkey", "d4i"])}
        for nm, (t_, shp, rd) in dd.items():
            o_ = nc.dram_tensor(nm, shp, F32, kind="ExternalOutput").ap()
            dma("sp", o_, t_[:], rd, [nm], dbg_tr)
    P.barrier()
    A32.reset()
    A16.reset()
    zt = ar("zt", [128, 4, D], BF16)
    z_tr = P.dma_track("zfill")
    P.op("pool", lambda e: e.memset(zt[:], 0.0), [], ["zt"])
    for r0 in range(0, NBLK * BR, 512):
        dma("sp", xs_s[r0:r0 + 512, :].rearrange("(s p) d -> p s d", p=128), zt[:], ["zt"], ["xs_s"], z_tr)
    hsc = [ar("hsc%d" % i, [128, D], BF16) for i in range(2)]
    hsc_tr = [P.dma_track("hsc%d" % i) for i in range(2)]
    sc_tr = [P.dma_track("scat%d" % i) for i in range(2)]
    for i in range(NSUBS):
        hi = i % 2
        dma("sp", hsc[hi][:], h2tm_s[i * 128:(i + 1) * 128, :], ["h2tm_s"], [("hsc", hi)], hsc_tr[hi])
        for k4 in range(TOPK):
            P.op("pool", lambda e, hi=hi, i=i, k4=k4: e.indirect_dma_start(
                out=xs_s[:, :], out_offset=bass.IndirectOffsetOnAxis(ap=d4i[:, i, k4:k4 + 1], axis=0),
                in_=hsc[hi][:, :], in_offset=None), [("hsc", hi), "d4i", "xs_s"], [("xs_sc", i, k4)], track=sc_tr[hi])
    P.barrier()
    A32.reset()
    A16.reset()

    wq = [ar("wq%d" % i, [128, KC, QW], BF16) for i in range(NQ)]
    wd = [ar("wd%d" % i, [128, CE, CB], BF16) for i in range(ND)]
    wq_tr = [P.dma_track("wq%d" % i) for i in range(NQ)]
    wd_tr = [P.dma_track("wd%d" % i) for i in range(ND)]
    bg = [sb("bg%d" % i, [128, 2 * CE]) for i in range(2)]
    bg_tr = [P.dma_track("bg%d" % i) for i in range(2)]
    xb = [ar("xb%d" % i, [128, SB, D], BF16) for i in range(2)]
    xb_tr = [P.dma_track("xb%d" % i) for i in range(2)]
    XT = [ar("XT%d" % i, [128, KC, BR], BF16) for i in range(2)]
    actT = [ar("actT%d" % i, [128, CE, BR], BF16) for i in range(2)]
    mt = {n: [ar("m_%s%d" % (n, i), [128, BR]) for i in range(2)] for n in ("g", "s", "u")}
    Yt = [ar("Yt%d" % i, [128, SB, D]) for i in range(2)]
    y_tr = [P.dma_track("yst%d" % i) for i in range(2)]
    POOL_ET = mybir.EngineType.Pool

    def dyn_load(dst_ap, src_rows, blk, reads, writes, track):
        def fn(e):
            kw = {}
            if USE_COND_SKIP:
                if "r" not in breg:
                    breg["r"] = e.to_reg(NE * 128 - 1)
                kw = dict(bounds_check=breg["r"], oob_is_err=False)
            return e.indirect_dma_start(out=dst_ap, out_offset=None, in_=src_rows,
                                        in_offset=bass.IndirectOffsetOnAxis(ap=widx[:, blk:blk + 1], axis=0), **kw)
        P.op("pool", fn, reads, writes, track=track)

    breg = {}
    wq_rows = [wq_s[q].rearrange("e p a b -> (e p) (a b)") for q in range(NQ)]
    wd_rows = [wd_s[d_].rearrange("e p a b -> (e p) (a b)") for d_ in range(ND)]
    bg_rows = bguT_d.rearrange("e p a -> (e p) a")
    def xb_load(blk):
        bi = blk % 2
        dma("sp", xb[bi][:], xs_s[blk * BR:(blk + 1) * BR, :].rearrange("(s p) d -> p s d", p=128),
            [], [("xb", bi)], xb_tr[bi])

    xb_load(0)
    for blk in range(NBLK):
        bi = blk % 2
        for q in range(NQ):
            dyn_load(wq[q].rearrange("p a b -> p (a b)"), wq_rows[q], blk, ["widx", "wq_s"], [("wq", q)], wq_tr[q])
        for d_ in range(ND):
            dyn_load(wd[d_].rearrange("p a b -> p (a b)"), wd_rows[d_], blk, ["widx", "wd_s"], [("wd", d_)], wd_tr[d_])
        dyn_load(bg[0][:], bg_rows, blk, ["widx"], [("bg", 0)], bg_tr[0])
        for k in range(KC):
            pt, ptk = next_pst()
            for s_ in range(SB):
                tr(pt[:, s_ * 128:(s_ + 1) * 128], xb[bi][:, s_, k * 128:(k + 1) * 128], ident_bf[:],
                   [("xb", bi), "ident_bf"], [ptk])
            cp("act" if k % 2 == 0 else "dve", XT[bi][:, k, :], pt[:, :BR], [ptk], [("XT", bi)])
        if blk + 1 < NBLK:
            xb_load(blk + 1)
        aT = actT[bi]
        for c in range(CE):
            mi = c % 2
            G_, S_, U_ = mt["g"][mi], mt["s"][mi], mt["u"][mi]
            gk, sk, uk = ("mg", mi), ("ms", mi), ("mu", mi)
            gcol = c * 128
            ucol = DE + c * 128
            pgm, pgmk = next_ps()
            for k in range(KC):
                mm(pgm[:, :BR], wq[gcol // QW][:, k, gcol % QW:gcol % QW + 128], XT[bi][:, k, :],
                   k == 0, k == KC - 1, [("wq", gcol // QW), ("XT", bi)], [pgmk])
            pum, pumk = next_ps()
            for k in range(KC):
                mm(pum[:, :BR], wq[ucol // QW][:, k, ucol % QW:ucol % QW + 128], XT[bi][:, k, :],
                   k == 0, k == KC - 1, [("wq", ucol // QW), ("XT", bi)], [pumk])
            ts("dve", G_[:, :], pgm[:, :BR], bg[0][:, c:c + 1], LIMIT, ALU.add, ALU.min, [pgmk, ("bg", 0)], [gk])
            act(S_[:, :], G_[:, :], AF.Sigmoid, [gk], [sk], scale=ALPHA)
            ts("dve", U_[:, :], pum[:, :BR], bg[0][:, CE + c:CE + c + 1], LIMIT, ALU.add, ALU.min,
               [pumk, ("bg", 0)], [uk])
            ts("dve", U_[:, :], U_[:, :], -LIMIT, 1.0, ALU.max, ALU.add, [uk], [uk])
            tt("dve", S_[:, :], S_[:, :], G_[:, :], ALU.mult, [sk, gk], [sk])
            tt("dve", aT[:, c, :], U_[:, :], S_[:, :], ALU.mult, [uk, sk], [("actT", bi)])
        for s_ in range(SB):
            for cb in range(NCB):
                pd, pdk = next_ps()
                for k in range(CE):
                    mm(pd[:, :CB], aT[:, k, s_ * 128:(s_ + 1) * 128], wd[cb][:, k, :], k == 0, k == CE - 1,
                       [("actT", bi), ("wd", cb)], [pdk])
                cp("act", Yt[bi][:, s_, cb * CB:(cb + 1) * CB], pd[:, :CB], [pdk], [("Yt", bi)])
        dma("sp", ys_s[blk * BR:(blk + 1) * BR, :].rearrange("(s p) d -> p s d", p=128), Yt[bi][:],
            [("Yt", bi)], [("ys_s", blk)], y_tr[bi])

    P.barrier()
    A32.reset()
    A16.reset()
    junk = ar("junk2", [128, D])
    wtsT = sb("wtsT", [NE, 128])
    Gt = [[ar("G%d_%d" % (i, k4), [128, D]) for k4 in range(TOPK)] for i in range(2)]
    g_tr = [[P.dma_track("g%d_%d" % (i, k4)) for k4 in range(TOPK)] for i in range(2)]
    acc = [ar("acc%d" % i, [128, D]) for i in range(2)]
    x1t = [ar("x1t%d" % i, [128, D]) for i in range(2)]
    x1_tr = [P.dma_track("x1t%d" % i) for i in range(2)]
    ot_tr = [P.dma_track("ot%d" % i) for i in range(2)]
    for i in range(NSUBS):
        xi = i % 2
        r0 = i * 128
        b = r0 // SEQ
        dma("sp", x1t[xi][:], x1_s[r0:r0 + 128, :], ["x1_s"], [("x1t", xi)], x1_tr[xi])
        for k4 in range(TOPK):
            P.op("pool", lambda e, xi=xi, i=i, k4=k4: e.indirect_dma_start(
                out=Gt[xi][k4][:, :], out_offset=None, in_=ys_s[:, :],
                in_offset=bass.IndirectOffsetOnAxis(ap=d4i[:, i, k4:k4 + 1], axis=0)),
                ["ys_s", "d4i"], [("G", xi, k4)], track=g_tr[xi][k4])
        pw, pwk = next_ps()
        tr(pw[:NE, :128], wts[:, i, :], ident32[:], ["wts", "ident32"], [pwk])
        cp("dve", wtsT[:, :], pw[:NE, :128], [pwk], ["wtsT"])
        for cb in range(NCB):
            pb, pbk = next_ps()
            mm(pb[:, :CB], wtsT[:, :], bdn[:, cb * CB:(cb + 1) * CB], True, True, ["wtsT", "c_bdn"], [pbk])
            cp("act", acc[xi][:, cb * CB:(cb + 1) * CB], pb[:, :CB], [pbk], [("acc", xi)])
        for k4 in range(TOPK):
            stt(acc[xi][:], Gt[xi][k4][:], w4[:, i, k4:k4 + 1], acc[xi][:], ALU.mult, ALU.add,
                [("G", xi, k4), "w4", ("acc", xi)], [("acc", xi)])
        tt("dve", acc[xi][:], acc[xi][:], gtb[:, 1, b, :], ALU.mult, [("acc", xi), "gtb"], [("acc", xi)])
        tt("dve", x1t[xi][:], x1t[xi][:], acc[xi][:], ALU.add, [("x1t", xi), ("acc", xi)], [("x1t", xi)])
        act(junk[:], x1t[xi][:], AF.Square, [("x1t", xi)], ["junk"], accum_out=ssq[:, 0:1])
        P.last_write[("ssq", 0)] = P.last_write["junk"]
        P.readers[("ssq", 0)] = []
        act(rstd[:, 0:1], ssq[:, 0:1], AF.Sqrt, [("ssq", 0)], [("rstd", 0)], scale=1.0 / D, bias=EPS)
        P.op("dve", lambda e: e.reciprocal(rstd[:, 0:1], rstd[:, 0:1]), [("rstd", 0)], [("rstd", 0)])
        stt(x1t[xi][:], x1t[xi][:], rstd[:, 0:1], fgb[:], ALU.mult, ALU.mult,
            [("x1t", xi), ("rstd", 0), "c_fgb"], [("x1t", xi)])
        dma("sp", out_d[r0:r0 + 128, :], x1t[xi][:], [("x1t", xi)], [("out", i)], ot_tr[xi])
    P.wait_all("sp", ot_tr)
    print("[build] sbuf bytes remaining/partition:", nc.sbuf_bytes_remaining, "A32 hi", A32.hi * 4, "A16 hi", A16.hi * 2,
          "n_ops", sum(len(v) for v in P.issue.values()), flush=True)
    P.emit(st)
    st.close()
    return nc


def _layout(inputs, cfg):
    D, DE, NE, SEQ, NSEQ, NCORES = cfg["D"], cfg["DE"], cfg["NE"], cfg["SEQ"], cfg["NSEQ"], cfg["NCORES"]
    KC, CE = D // 128, DE // 128
    f = lambda a: np.ascontiguousarray(np.asarray(a, dtype=np.float32))
    g = {k: np.asarray(v) for k, v in inputs.items()}

    def fm(v):
        return f(v.reshape(-1, 128).T)

    def km(w):
        return f(w.reshape(-1, 128, w.shape[-1]).transpose(1, 0, 2))

    def bc(v):
        return f(np.broadcast_to(v[None, :], (128, v.shape[0])))
    shared = {}
    shared["ada_w"] = km(g["ada_w"][0])
    ada_b = g["ada_b"][0]
    shared["ada_bT"] = fm(ada_b)
    shared["ada_bg"] = f(np.stack([np.broadcast_to(ada_b[2 * D:3 * D], (128, D)),
                                   np.broadcast_to(ada_b[5 * D:6 * D], (128, D))], axis=1))
    shared["g1T"] = fm(g["norm1_g"][0])
    shared["g2T"] = fm(g["norm2_g"][0])
    shared["w_in"] = km(g["w_in"][0])
    shared["conv_wT"] = f(g["conv_w"][0].reshape(4, KC, 128).transpose(2, 1, 0))
    shared["conv_bT"] = fm(g["conv_b"][0])
    shared["lru_wa"] = f(g["lru_wa"][0].reshape(KC, 2, 64, 64))
    shared["lru_wx"] = f(g["lru_wx"][0].reshape(KC, 2, 64, 64))
    shared["lru_baT"] = fm(g["lru_ba"][0])
    shared["lru_bxT"] = fm(g["lru_bx"][0])
    shared["lamT"] = fm(g["lru_lam"][0])
    shared["ln_g_b"] = bc(g["sg_ln_g"][0])
    shared["ln_b_b"] = bc(g["sg_ln_b"][0])
    shared["sg_wsT"] = f(g["sg_ws"][0].transpose(2, 0, 1))
    shared["sg_bs_b"] = f(np.broadcast_to(g["sg_bs"][0][None], (128, KC, 128)))
    shared["w_br"] = f(np.stack([km(g["w_br_rnn"][0]), km(g["w_br_sg"][0]), km(g["w_out"][0])]))
    shared["w_router"] = km(g["w_router"][0])
    shared["b_router_b"] = bc(g["b_router"][0])
    shared["w_gu"] = f(g["w_gu"][0].reshape(NE, KC, 128, 2 * DE).transpose(0, 2, 1, 3))
    shared["b_guT"] = f(g["b_gu"][0].reshape(NE, 2 * CE, 128).transpose(0, 2, 1))
    shared["w_down"] = f(g["w_down"][0].reshape(NE, CE, 128, D).transpose(0, 2, 1, 3))
    shared["b_down"] = f(g["b_down"][0])
    shared["final_g_b"] = bc(g["final_g"])
    x = g["x"].reshape(NCORES, NSEQ * SEQ, D)
    c = g["c"].reshape(NCORES, NSEQ, KC, 128)
    maps = []
    for i in range(NCORES):
        m = dict(shared)
        m["x"] = f(x[i])
        m["cT"] = f(c[i].transpose(2, 1, 0))
        maps.append(m)
    return maps


_NC_CACHE = {}


def run(inputs, cfg):
    key = tuple(sorted(cfg.items()))
    if key not in _NC_CACHE:
        _NC_CACHE[key] = build_nc(cfg)
    nc = _NC_CACHE[key]
    maps = _layout(inputs, cfg)
    res = run_bass_kernel_spmd(nc, maps, core_ids=list(range(cfg["NCORES"])))
    if cfg.get("DEBUG"):
        global DBG
        DBG = res.results
    out = np.stack([r["out"] for r in res.results], axis=0)
    B = cfg["NCORES"] * cfg["NSEQ"]
    return out.reshape(B, cfg["SEQ"], cfg["D"]).astype(np.float32)


def kernel(**inputs):
    return run(inputs, CFG_FULL)
```
